# Optimizing a Trainium2 kernel written in Bass

```python
import jax
import jax.numpy as jnp
from jax import lax
import numpy as np

D_MODEL = 2048
BATCH = 1
SEQ = 8192
DEPTH = 1
DEC_BATCH = 32
DEC_SEQ = 4
PAST_LEN = 8192
PAGE_SIZE = 128

D_CONV = D_MODEL // 2
D_ATTN = D_MODEL - D_CONV
HEAD_DIM = 64
N_HEADS = D_ATTN // HEAD_DIM
N_KV = 4
GROUP = N_HEADS // N_KV
KV_W = N_KV * HEAD_DIM
CONV_W = 3
CMP_LEN = 32
CMP_STRIDE = 16
CMP_HIDDEN = 2 * HEAD_DIM
SLC_BLOCK = 64
TOP_N = 16
WINDOW = 512
Q_BLOCK = 128
FORCE_SCORE = 1.0e4
ROPE_THETA = 10000.0
N_KV_SLOTS = 4
N_KEYS = 128
N_EXPERTS = N_KEYS * N_KEYS
PEER_HEADS = 8
PEER_TOPK = 16
D_KEY = 256
HALF_KEY = D_KEY // 2
TOK_BLOCK = 128
EPS = 1e-6
SPLITS = [D_CONV, 2 * D_CONV, 3 * D_CONV, 3 * D_CONV + D_ATTN, 3 * D_CONV + D_ATTN + 6 * KV_W]
IN_COLS = 3 * D_CONV + D_ATTN + 6 * KV_W + 3 * N_HEADS

kernel_name = 'hymba_shortconv_nsa_peer_adaln_step'


def rmsnorm(x):
    xf = x.astype(jnp.float32)
    y = xf * lax.rsqrt(jnp.mean(xf * xf, axis=-1, keepdims=True) + EPS)
    return y.astype(x.dtype)


def modulate(x, gain, shift, scale):
    return rmsnorm(x) * gain * (1.0 + scale) + shift


def ada_terms(c, w_ada, b_ada):
    m = jax.nn.silu(c) @ w_ada + b_ada
    return jnp.split(m[:, None, :], 6, axis=-1)


def rope(x, pos):
    half = HEAD_DIM // 2
    inv = ROPE_THETA ** (-jnp.arange(half, dtype=jnp.float32) / half)
    ang = pos.astype(jnp.float32)[:, None] * inv[None, :]
    cos = jnp.cos(ang)[:, None, :]
    sin = jnp.sin(ang)[:, None, :]
    xf = x.astype(jnp.float32)
    x1, x2 = xf[..., :half], xf[..., half:]
    return jnp.concatenate([x1 * cos - x2 * sin, x2 * cos + x1 * sin], axis=-1).astype(x.dtype)


def masked_softmax(s, mask):
    s = jnp.where(mask, s.astype(jnp.float32), -jnp.inf)
    m = jnp.max(s, axis=-1, keepdims=True)
    m = jnp.where(jnp.isfinite(m), m, 0.0)
    p = jnp.exp(s - m)
    return p / jnp.maximum(jnp.sum(p, axis=-1, keepdims=True), 1e-30)


def project(h, pos, w_in):
    B, T, _ = h.shape
    b_g, c_g, xv, q, kv, g = jnp.split(h @ w_in, SPLITS, axis=-1)
    q = rope(q.reshape(B, T, N_HEADS, HEAD_DIM), pos)
    kv = kv.reshape(B, T, 6, N_KV, HEAD_DIM)
    rows = jnp.stack([kv[:, :, 0], kv[:, :, 1], rope(kv[:, :, 2], pos), kv[:, :, 3]], axis=2)
    win = jnp.stack([rope(kv[:, :, 4], pos), kv[:, :, 5]], axis=2)
    gates = jax.nn.sigmoid(g).reshape(B, T, N_HEADS, 3)
    return b_g, c_g * xv, q, gates, rows, win


def short_conv(z_ctx, w, b):
    T = z_ctx.shape[1] - (CONV_W - 1)
    out = b
    for k in range(CONV_W):
        out = out + w[k] * z_ctx[:, k:k + T]
    return out


def compress(raw, pe, w1, b1, w2, b2, n_cmp):
    B = raw.shape[0]
    n_chunk = n_cmp + 1
    ch = raw[:, :n_chunk * CMP_STRIDE].reshape(B, n_chunk, CMP_STRIDE, N_KV, HEAD_DIM)
    ha = jnp.einsum('bcsgd,sdh->bcgh', ch + pe[:CMP_STRIDE, None, :], w1[:CMP_STRIDE])
    hb = jnp.einsum('bcsgd,sdh->bcgh', ch + pe[CMP_STRIDE:, None, :], w1[CMP_STRIDE:])
    hid = jax.nn.gelu(ha[:, :-1] + hb[:, 1:] + b1)
    return jnp.einsum('bjgh,hd->bjgd', hid, w2) + b2


def compressed_kv(k_raw, v_raw, pe, w1, b1, w2, b2):
    T = k_raw.shape[1]
    n_cmp = (T - CMP_LEN) // CMP_STRIDE + 1
    kc = compress(k_raw, pe[0], w1[0], b1[0], w2[0], b2[0], n_cmp)
    vc = compress(v_raw, pe[1], w1[1], b1[1], w2[1], b2[1], n_cmp)
    cpos = CMP_STRIDE * jnp.arange(n_cmp) + (CMP_LEN - 1)
    return rope(kc, cpos), vc


def cmp_to_slc(n_cmp, n_sel):
    j = np.arange(n_cmp)[:, None]
    b = np.arange(n_sel)[None, :]
    ov = (CMP_STRIDE * j < SLC_BLOCK * (b + 1)) & (CMP_STRIDE * j + CMP_LEN > SLC_BLOCK * b)
    return jnp.asarray(ov.astype(np.float32))


def slc_blocks(k, n_sel):
    B, T = k.shape[:2]
    k = jnp.pad(k, ((0, 0), (0, n_sel * SLC_BLOCK - T), (0, 0), (0, 0)))
    return k.reshape(B, n_sel, SLC_BLOCK, N_KV, HEAD_DIM).transpose(0, 3, 1, 2, 4)


def nsa_attend(q, qpos, kc, vc, ks_blk, vs_blk, kw, vw, wpos, gates):
    B, Tq = q.shape[:2]
    scale = HEAD_DIM ** -0.5
    qg = q.reshape(B, Tq, N_KV, GROUP, HEAD_DIM)
    n_cmp = kc.shape[1]
    n_sel = ks_blk.shape[2]
    cpos = CMP_STRIDE * jnp.arange(n_cmp) + (CMP_LEN - 1)
    s_c = jnp.einsum('bqgrd,bjgd->bgrqj', qg, kc) * scale
    p_c = masked_softmax(s_c, cpos[None, :] <= qpos[:, None])
    o_c = jnp.einsum('bgrqj,bjgd->bqgrd', p_c.astype(vc.dtype), vc)
    imp = jnp.einsum('bgrqj,js->bgqs', p_c, cmp_to_slc(n_cmp, n_sel))
    cur = qpos // SLC_BLOCK
    blk = jnp.arange(n_sel)
    forced = (blk[None, :] == 0) | (blk[None, :] == cur[:, None]) | (blk[None, :] == cur[:, None] - 1)
    valid = blk[None, :] <= cur[:, None]
    score = jnp.where(valid, jnp.where(forced, FORCE_SCORE, imp), -jnp.inf)
    n_top = min(TOP_N, n_sel)
    _, idx = lax.top_k(score, n_top)
    sel_ok = idx <= cur[None, None, :, None]
    bi = jnp.arange(B)[:, None, None, None]
    gi = jnp.arange(N_KV)[None, :, None, None]
    ksel = ks_blk[bi, gi, idx]
    vsel = vs_blk[bi, gi, idx]
    kpos = idx[..., None] * SLC_BLOCK + jnp.arange(SLC_BLOCK)
    m_s = (sel_ok[..., None] & (kpos <= qpos[None, None, :, None, None])).reshape(B, N_KV, Tq, -1)[:, :, None]
    s_s = jnp.einsum('bqgrd,bgqkpd->bgrqkp', qg, ksel).reshape(B, N_KV, GROUP, Tq, -1) * scale
    p_s = masked_softmax(s_s, m_s)
    o_s = jnp.einsum('bgrqn,bgqnd->bqgrd', p_s.astype(vsel.dtype), vsel.reshape(B, N_KV, Tq, -1, HEAD_DIM))
    s_w = jnp.einsum('bqgrd,bwgd->bgrqw', qg, kw) * scale
    dist = qpos[:, None] - wpos[None, :]
    p_w = masked_softmax(s_w, (dist >= 0) & (dist < WINDOW) & (wpos[None, :] >= 0))
    o_w = jnp.einsum('bgrqw,bwgd->bqgrd', p_w.astype(vw.dtype), vw)
    g = gates.reshape(B, Tq, N_KV, GROUP, 3)
    o = g[..., 0:1] * o_c + g[..., 1:2] * o_s + g[..., 2:3] * o_w
    return o.reshape(B, Tq, D_ATTN)


def peer(h, wq, keys, u_tab, v_tab):
    N = h.shape[0]
    n_blk = -(-N // TOK_BLOCK)
    hp = jnp.pad(h, ((0, n_blk * TOK_BLOCK - N), (0, 0))).reshape(n_blk, TOK_BLOCK, D_MODEL)

    def one_block(hb):
        q = (hb @ wq).reshape(TOK_BLOCK, PEER_HEADS, 2, HALF_KEY)
        s = jnp.einsum('thcd,hckd->thck', q, keys)
        s1, i1 = lax.top_k(s[:, :, 0], PEER_TOPK)
        s2, i2 = lax.top_k(s[:, :, 1], PEER_TOPK)
        cand = (s1[..., :, None] + s2[..., None, :]).reshape(TOK_BLOCK, PEER_HEADS, PEER_TOPK * PEER_TOPK)
        cidx = (i1[..., :, None] * N_KEYS + i2[..., None, :]).reshape(TOK_BLOCK, PEER_HEADS, PEER_TOPK * PEER_TOPK)
        top_s, sel = lax.top_k(cand, PEER_TOPK)
        e = jnp.take_along_axis(cidx, sel, axis=-1)
        g = jax.nn.softmax(top_s.astype(jnp.float32), axis=-1).astype(hb.dtype)
        a = jax.nn.gelu(jnp.einsum('td,thkd->thk', hb, u_tab[e]))
        return jnp.einsum('thk,thkd->td', g * a, v_tab[e])

    return lax.map(one_block, hp).reshape(n_blk * TOK_BLOCK, D_MODEL)[:N]


def finish_layer(x, conv_y, attn, ga1, sh2, sc2, ga2, p):
    x = x + ga1 * (jnp.concatenate([conv_y, attn], axis=-1) @ p['w_out'])
    B, T, _ = x.shape
    h2 = modulate(x, p['norm2_g'], sh2, sc2)
    f = peer(h2.reshape(B * T, D_MODEL), p['peer_wq'], p['peer_keys'], p['peer_u'], p['peer_v'])
    return x + ga2 * f.reshape(B, T, D_MODEL)


def prompt_layer(x, c, p):
    B, T, _ = x.shape
    sh1, sc1, ga1, sh2, sc2, ga2 = ada_terms(c, p['w_ada'], p['b_ada'])
    h = modulate(x, p['norm1_g'], sh1, sc1)
    pos = jnp.arange(T)
    b_g, zc, q, gates, rows, win = project(h, pos, p['w_in'])
    conv_y = b_g * short_conv(jnp.pad(zc, ((0, 0), (CONV_W - 1, 0), (0, 0))), p['conv_w'], p['conv_b'])
    kc, vc = compressed_kv(rows[:, :, 0], rows[:, :, 1], p['cmp_pe'], p['cmp_w1'], p['cmp_b1'], p['cmp_w2'], p['cmp_b2'])
    n_sel = -(-T // SLC_BLOCK)
    ksb = slc_blocks(rows[:, :, 2], n_sel)
    vsb = slc_blocks(rows[:, :, 3], n_sel)
    win_pad = jnp.pad(win, ((0, 0), (WINDOW, 0), (0, 0), (0, 0), (0, 0)))

    def attend_block(i):
        start = i * Q_BLOCK
        qb = lax.dynamic_slice_in_dim(q, start, Q_BLOCK, axis=1)
        gb = lax.dynamic_slice_in_dim(gates, start, Q_BLOCK, axis=1)
        wb = lax.dynamic_slice_in_dim(win_pad, start, WINDOW + Q_BLOCK, axis=1)
        qpos = start + jnp.arange(Q_BLOCK)
        wpos = start - WINDOW + jnp.arange(WINDOW + Q_BLOCK)
        return nsa_attend(qb, qpos, kc, vc, ksb, vsb, wb[:, :, 0], wb[:, :, 1], wpos, gb)

    attn = lax.map(attend_block, jnp.arange(T // Q_BLOCK))
    attn = attn.transpose(1, 0, 2, 3).reshape(B, T, D_ATTN)
    x = finish_layer(x, conv_y, attn, ga1, sh2, sc2, ga2, p)
    return x, rows, win[:, T - min(WINDOW, T):], zc[:, T - (CONV_W - 1):]


def sample_layer(x, c, cache_kv, page_table, win_buf, conv_buf, p):
    B, T, _ = x.shape
    n_pages = page_table.shape[1]
    past_len = n_pages * cache_kv.shape[1]
    sh1, sc1, ga1, sh2, sc2, ga2 = ada_terms(c, p['w_ada'], p['b_ada'])
    h = modulate(x, p['norm1_g'], sh1, sc1)
    pos = past_len + jnp.arange(T)
    b_g, zc, q, gates, rows, win = project(h, pos, p['w_in'])
    z_ctx = jnp.concatenate([conv_buf, zc], axis=1)
    conv_y = b_g * short_conv(z_ctx, p['conv_w'], p['conv_b'])
    past = cache_kv[page_table].reshape(B, past_len, N_KV_SLOTS, N_KV, HEAD_DIM)
    full = jnp.concatenate([past, rows], axis=1)
    kc, vc = compressed_kv(full[:, :, 0], full[:, :, 1], p['cmp_pe'], p['cmp_w1'], p['cmp_b1'], p['cmp_w2'], p['cmp_b2'])
    n_sel = -(-(past_len + T) // SLC_BLOCK)
    ksb = slc_blocks(full[:, :, 2], n_sel)
    vsb = slc_blocks(full[:, :, 3], n_sel)
    wkeep = win_buf.shape[1]
    win_ctx = jnp.concatenate([win_buf, win], axis=1)
    wpos = past_len - wkeep + jnp.arange(wkeep + T)
    attn = nsa_attend(q, pos, kc, vc, ksb, vsb, win_ctx[:, :, 0], win_ctx[:, :, 1], wpos, gates)
    x = finish_layer(x, conv_y, attn, ga1, sh2, sc2, ga2, p)
    return x, rows, win_ctx[:, T:], z_ctx[:, T:]


def setup_inputs(seed: int = 0) -> dict:
    key = jax.random.key(seed)
    ks = jax.random.split(key, 32)
    n_pages = PAST_LEN // PAGE_SIZE
    n_used = DEC_BATCH * n_pages
    n_pool = n_used + max(1, n_used // 4)
    wkeep = min(WINDOW, PAST_LEN)

    def nrm(k, shape, s):
        return s * jax.random.normal(k, shape, jnp.float32)

    page_table = jax.random.permutation(ks[7], n_pool)[:n_used].reshape(DEC_BATCH, n_pages).astype(jnp.int32)
    return {
        'x_prompt': nrm(ks[0], (BATCH, SEQ, D_MODEL), 1.0),
        'x_sample': nrm(ks[1], (DEC_BATCH, DEC_SEQ, D_MODEL), 1.0),
        'c_prompt': nrm(ks[2], (BATCH, D_MODEL), 1.0),
        'c_sample': nrm(ks[3], (DEC_BATCH, D_MODEL), 1.0),
        'cache_kv': nrm(ks[4], (DEPTH, n_pool, PAGE_SIZE, N_KV_SLOTS, N_KV, HEAD_DIM), 1.0),
        'state_win': nrm(ks[5], (DEPTH, DEC_BATCH, wkeep, 2, N_KV, HEAD_DIM), 1.0),
        'state_conv': nrm(ks[6], (DEPTH, DEC_BATCH, CONV_W - 1, D_CONV), 1.0),
        'page_table': page_table,
        'w_ada': nrm(ks[8], (DEPTH, D_MODEL, 6 * D_MODEL), 0.5 * D_MODEL ** -0.5),
        'b_ada': nrm(ks[9], (DEPTH, 6 * D_MODEL), 0.01),
        'norm1_g': 1.0 + nrm(ks[10], (DEPTH, D_MODEL), 0.02),
        'norm2_g': 1.0 + nrm(ks[11], (DEPTH, D_MODEL), 0.02),
        'w_in': nrm(ks[12], (DEPTH, D_MODEL, IN_COLS), D_MODEL ** -0.5),
        'conv_w': nrm(ks[13], (DEPTH, CONV_W, D_CONV), 0.5),
        'conv_b': nrm(ks[14], (DEPTH, D_CONV), 0.01),
        'cmp_pe': nrm(ks[15], (DEPTH, 2, CMP_LEN, HEAD_DIM), 0.02),
        'cmp_w1': nrm(ks[16], (DEPTH, 2, CMP_LEN, HEAD_DIM, CMP_HIDDEN), (CMP_LEN * HEAD_DIM) ** -0.5),
        'cmp_b1': nrm(ks[17], (DEPTH, 2, CMP_HIDDEN), 0.01),
        'cmp_w2': nrm(ks[18], (DEPTH, 2, CMP_HIDDEN, HEAD_DIM), CMP_HIDDEN ** -0.5),
        'cmp_b2': nrm(ks[19], (DEPTH, 2, HEAD_DIM), 0.01),
        'w_out': nrm(ks[20], (DEPTH, D_MODEL, D_MODEL), D_MODEL ** -0.5),
        'peer_wq': nrm(ks[21], (DEPTH, D_MODEL, PEER_HEADS * D_KEY), D_MODEL ** -0.5),
        'peer_keys': nrm(ks[22], (DEPTH, PEER_HEADS, 2, N_KEYS, HALF_KEY), HALF_KEY ** -0.5),
        'peer_u': nrm(ks[23], (DEPTH, N_EXPERTS, D_MODEL), D_MODEL ** -0.5),
        'peer_v': nrm(ks[24], (DEPTH, N_EXPERTS, D_MODEL), PEER_HEADS ** -0.5),
        'final_g': 1.0 + nrm(ks[25], (D_MODEL,), 0.02),
    }


def reference(x_prompt, x_sample, c_prompt, c_sample, cache_kv, state_win, state_conv, page_table,
              w_ada, b_ada, norm1_g, norm2_g, w_in, conv_w, conv_b, cmp_pe, cmp_w1, cmp_b1, cmp_w2, cmp_b2,
              w_out, peer_wq, peer_keys, peer_u, peer_v, final_g):
    xp, xs = x_prompt, x_sample
    rows_p, rows_s, win_p, win_s, conv_p, conv_s = [], [], [], [], [], []
    for l in range(DEPTH):
        p = {'w_ada': w_ada[l], 'b_ada': b_ada[l], 'norm1_g': norm1_g[l], 'norm2_g': norm2_g[l],
             'w_in': w_in[l], 'conv_w': conv_w[l], 'conv_b': conv_b[l], 'cmp_pe': cmp_pe[l],
             'cmp_w1': cmp_w1[l], 'cmp_b1': cmp_b1[l], 'cmp_w2': cmp_w2[l], 'cmp_b2': cmp_b2[l],
             'w_out': w_out[l], 'peer_wq': peer_wq[l], 'peer_keys': peer_keys[l],
             'peer_u': peer_u[l], 'peer_v': peer_v[l]}
        xp, rp, wp, cp = prompt_layer(xp, c_prompt, p)
        xs, rs, ws, cs = sample_layer(xs, c_sample, cache_kv[l], page_table, state_win[l], state_conv[l], p)
        rows_p.append(rp)
        rows_s.append(rs)
        win_p.append(wp)
        win_s.append(ws)
        conv_p.append(cp)
        conv_s.append(cs)
    y_prompt = rmsnorm(xp) * final_g
    y_sample = rmsnorm(xs) * final_g
    kv_rows_prompt = jnp.stack(rows_p)
    kv_rows_sample = jnp.stack(rows_s)
    win_prompt = jnp.stack(win_p)
    win_sample = jnp.stack(win_s)
    conv_prompt = jnp.stack(conv_p)
    conv_sample = jnp.stack(conv_s)
    return (y_prompt, y_sample, kv_rows_prompt, kv_rows_sample, win_prompt, win_sample, conv_prompt, conv_sample)
```

```python
import contextlib
import numpy as np
import concourse.bass as bass
import concourse.mybir as mybir
from concourse.alu_op_type import AluOpType as ALU
from concourse.bass_utils import run_bass_kernel_spmd

AF = mybir.ActivationFunctionType
AX = mybir.AxisListType
F32 = mybir.dt.float32
BF16 = mybir.dt.bfloat16
I32 = mybir.dt.int32
U32 = mybir.dt.uint32

NCORES = 8
D = 2048
DC = 1024
NT = 8
SEQ = 8192
NS = 4
ST = 4
EPS = 1e-6
IN_COLS = 5680


class Res:
    __slots__ = ("w", "rs", "name")

    def __init__(self, name=""):
        self.w = None
        self.rs = []
        self.name = name


class T:
    def __init__(self, t, name=""):
        self.t = t
        self.r = Res(name)

    def __getitem__(self, k):
        return self.t[k]


class View:
    def __init__(self, ap, r):
        self.v = ap
        self.r = r.r if hasattr(r, "r") else r

    def __getitem__(self, k):
        return self.v[k]


class Ctx:
    NDMA = 8

    def __init__(self, nc, es):
        self.nc = nc
        self.es = es
        self.eng = {"pe": nc.tensor, "dve": nc.vector, "act": nc.scalar, "pool": nc.gpsimd, "sp": nc.sync}
        self.sem = {}
        self.cnt = {}
        for k in self.eng:
            self.sem[k] = es.enter_context(nc.semaphore("s_" + k))
            self.cnt[k] = 0
        self.dsem = {}
        self.dcnt = {}
        for q in ("sp", "pool", "act"):
            self.dsem[q] = [es.enter_context(nc.semaphore("d_%s%d" % (q, i))) for i in range(self.NDMA)]
            self.dcnt[q] = 0
        self.seen = {k: {} for k in self.eng}
        self.out_tokens = []
        self.ninst = 0

    def sb(self, name, shape, dt=F32):
        return T(self.es.enter_context(self.nc.sbuf_tensor(name, list(shape), dt)), name)

    def ps(self, name, shape, dt=F32):
        return T(self.es.enter_context(self.nc.psum_tensor(name, list(shape), dt)), name)

    def _wait(self, e, tok):
        if tok is None:
            return
        sem, val = tok
        key = id(sem)
        if self.seen[e].get(key, 0) >= val:
            return
        self.seen[e][key] = val
        self.eng[e].wait_ge(sem, val)
        self.ninst += 1

    @staticmethod
    def _res(x):
        return x.r if hasattr(x, "r") else x

    def _deps(self, e, reads, writes):
        own = self.sem[e]
        toks = []
        for r in reads:
            r = self._res(r)
            if r.w is not None:
                toks.append(r.w)
        for w in writes:
            w = self._res(w)
            if w.w is not None:
                toks.append(w.w)
            for t in w.rs:
                if t[0] is own:
                    continue
                toks.append(t)
        if e == "pe":
            toks = [t for t in toks if t[0] is not own]
        for t in toks:
            self._wait(e, t)

    def _commit(self, tok, reads, writes):
        for r in reads:
            r = self._res(r)
            r.rs.append(tok)
            if len(r.rs) > 96:
                r.rs = r.rs[-96:]
        for w in writes:
            w = self._res(w)
            w.w = tok
            w.rs = []

    def op(self, e, fn, reads=(), writes=()):
        self._deps(e, reads, writes)
        inst = fn(self.eng[e])
        self.cnt[e] += 1
        inst.then_inc(self.sem[e], 1)
        tok = (self.sem[e], self.cnt[e])
        self._commit(tok, reads, writes)
        self.ninst += 1
        return tok

    def dma(self, q, fn, reads=(), writes=(), is_output=False):
        i = self.dcnt[q]
        self.dcnt[q] += 1
        sem = self.dsem[q][i % self.NDMA]
        rnd = i // self.NDMA
        if rnd > 0:
            self._wait(q, (sem, 16 * rnd))
        self._deps(q, reads, writes)
        inst = fn(self.eng[q])
        inst.then_inc(sem, 16)
        tok = (sem, 16 * (rnd + 1))
        self._commit(tok, reads, writes)
        if is_output:
            self.out_tokens.append(tok)
        self.ninst += 1
        return tok

    def barrier(self):
        toks = [(self.sem[e], self.cnt[e]) for e in self.eng if self.cnt[e] > 0]
        for q in self.dsem:
            n = self.dcnt[q]
            for k in range(min(n, self.NDMA)):
                cntk = (n - k + self.NDMA - 1) // self.NDMA
                toks.append((self.dsem[q][k], 16 * cntk))
        for e in self.eng:
            for t in toks:
                self._wait(e, t)

    def finish(self):
        for tok in self.out_tokens:
            self._wait("sp", tok)
        for q in self.dsem:
            n = self.dcnt[q]
            for k in range(min(n, self.NDMA)):
                cntk = (n - k + self.NDMA - 1) // self.NDMA
                self._wait("sp", (self.dsem[q][k], 16 * cntk))
        for e in self.eng:
            if e != "sp" and self.cnt[e] > 0:
                self._wait("sp", (self.sem[e], self.cnt[e]))


def build_program():
    nc = bass.Bass("TRN2", target_bir_lowering=False)

    import os as _osd
    _DBG = _osd.environ.get("KDBG", "")
    _SKIP = set(_osd.environ.get("KSKIP", "").split(","))
    din_shapes = {}

    def din(name, shape, dt=F32):
        if _DBG and name in ("peer_u", "peer_v", "w_out", "peer_wq", "w_ada", "xo"):
            shape = [2, 2] if len(shape) == 2 else [2, 2, 2]
        din_shapes[name] = tuple(shape)
        return nc.dram_tensor(name, list(shape), dt, kind="ExternalInput").ap()

    def dout(name, shape, dt=F32):
        return nc.dram_tensor(name, list(shape), dt, kind="ExternalOutput").ap()

    xo = din("xo", [NT, 128, D])
    xpv = din("xpv", [NT, 2, D])
    pfl = din("pfl", [128, NT])
    cso = din("cso", [NT, 128, 128])
    css = din("css", [16, 128])
    xs = din("xs", [16, D])
    cc = din("cc", [5, D])
    idn = din("idn", [128, 128])
    scv = din("scv", [8, DC])
    swin = din("swin", [NS, 512, 512])
    w_ada = din("w_ada", [D, 6 * D])
    b_ada = din("b_ada", [1, 6 * D])
    g1 = din("g1", [1, D])
    g2 = din("g2", [1, D])
    fg = din("fg", [1, D])
    w_in = din("w_in", [D, IN_COLS])
    conv_w = din("conv_w", [3, DC])
    conv_b = din("conv_b", [1, DC])
    w_out = din("w_out", [D, D])
    peer_wq = din("peer_wq", [D, D])
    peer_keys = din("peer_keys", [16, 128, 128])
    peer_u = din("peer_u", [16384, D])
    peer_v = din("peer_v", [16384, D])
    xp = din("xp", [64, 128, D])
    csa = din("csa", [64, 128, 128])
    cscmp = din("cscmp", [4, 128, 128])
    ovm = din("ovm", [128, 4, 128])
    e32 = din("e32", [32, 16, 128])
    fmask = din("fmask", [NT, 128, 128])
    cmask = din("cmask", [NT, 128, 4, 128])
    dmask = din("dmask", [NT, 128, 8, 128])
    wmask = din("wmask", [NT, 128, 12, 128])
    cache = din("cache", [2560 * 128, 1024])
    ptab = din("ptab", [NS, 64], I32)
    piota = din("piota", [128, 1])
    cmask_sm = din("cmask_sm", [128, 4, 4])
    fmask_sm = din("fmask_sm", [4, 128])
    wmask_sm = din("wmask_sm", [128, 4, 4])
    nmask = din("nmask", [16, NS, 4])
    cmp_pe = din("cmp_pe", [2, 32, 64])
    cmp_w1 = din("cmp_w1", [2, 32, 64, 128])
    cmp_b1 = din("cmp_b1", [2, 128])
    cmp_w2 = din("cmp_w2", [2, 128, 64])
    cmp_b2 = din("cmp_b2", [2, 64])

    yo = dout("yo", [NT, 128, D])
    ys = dout("ys", [16, D])
    rows_o = dout("rows_o", [NT, 128, 1024])
    rows_s = dout("rows_s", [16, 1024])
    win_o = dout("win_o", [NT, 128, 512])
    win_s = dout("win_s", [NS, 512, 512])
    conv_o = dout("conv_o", [NT, 2, DC])
    conv_s = dout("conv_s", [NS, 2, DC])

    m_dram = nc.dram_tensor("m_dram", [5, 6 * D], F32, kind="Internal").ap()
    m_res = Res("m_dram")
    kTs_d = nc.dram_tensor("kTs_d", [128, 2, SEQ], BF16, kind="Internal").ap()
    kTw_d = nc.dram_tensor("kTw_d", [128, 2, SEQ], BF16, kind="Internal").ap()
    vs_d = nc.dram_tensor("vs_d", [4, 128, 64, 65], BF16, kind="Internal").ap()
    vw_d = nc.dram_tensor("vw_d", [4, 128, 64, 65], BF16, kind="Internal").ap()
    kv_res = Res("kv_scratch")
    kTs_s = nc.dram_tensor("kTs_s", [NS, 128, 2, SEQ], BF16, kind="Internal").ap()
    vs_s = nc.dram_tensor("vs_s", [NS, 4, 128, 64, 65], BF16, kind="Internal").ap()
    kTw_s = nc.dram_tensor("kTw_s", [NS, 128, 2, 512], BF16, kind="Internal").ap()
    vw_s = nc.dram_tensor("vw_s", [NS, 4, 128, 4, 65], BF16, kind="Internal").ap()
    kc_s = nc.dram_tensor("kc_s", [NS, 128, 2, 512], BF16, kind="Internal").ap()
    vc_s = nc.dram_tensor("vc_s", [NS, 128, 4, 4, 65], BF16, kind="Internal").ap()
    w_in_v = w_in.rearrange("(c p) n -> p c n", p=128)
    w_out_v = None if _DBG else w_out.rearrange("(c p) n -> p c n", p=128)
    w_ada_v = None if _DBG else w_ada.rearrange("(c p) n -> p c n", p=128)
    wq_v = None if _DBG else peer_wq.rearrange("(c p) n -> p c n", p=128)
    nc._din_shapes = din_shapes

    with contextlib.ExitStack() as es:
        c = Ctx(nc, es)
        idf = c.sb("idf", [128, 128])
        idb = c.sb("idb", [128, 128], BF16)
        zer = c.sb("zer", [128, 512], BF16)
        pfl_t = c.sb("pfl_t", [128, NT])
        cwT = c.sb("cwT", [128, 8, 3])
        cbT = c.sb("cbT", [128, 8])
        h2f = c.sb("h2f", [128, D])
        facc = c.sb("facc", [128, D])
        t12 = [c.sb("t12_%d" % i, [128, 16]) for i in range(2)]
        i12 = [c.sb("i12_%d" % i, [128, 16], U32) for i in range(2)]
        if12 = [c.sb("if12_%d" % i, [128, 16]) for i in range(2)]
        tmp128 = c.sb("tmp128", [128, 128])
        cand = c.sb("cand", [128, 256])
        cidx = c.sb("cidx", [128, 256])
        tmp256 = c.sb("tmp256", [128, 256])
        c16 = c.sb("c16", [128, 16])
        pst = c.sb("pst", [128, 4])
        ef = c.sb("ef", [128, 128])
        eu = c.sb("eu", [128, 128], U32)
        gw = c.sb("gw", [128, 128])
        adot = c.sb("adot", [128, 128])
        coef = c.sb("coef", [128, 128])
        modA = c.sb("modA", [128, D])
        modB = c.sb("modB", [128, D])
        modG = c.sb("modG", [128, D])
        ccT = c.sb("ccT", [128, 16, 5], BF16)
        xt1 = c.sb("xt", [128, D])
        xt = [xt1, xt1]
        junk = c.sb("junk", [128, D])
        yt = c.sb("yt", [128, D])
        hb = c.sb("hb", [128, D], BF16)
        st1 = c.sb("st1", [128, 4])
        st2 = c.sb("st2", [2, 4])
        hT = c.sb("hT", [128, 16, 130], BF16)
        hT_main = hT
        cs_t = c.sb("cs_t", [128, 128])
        kcT = c.sb("kcT", [128, 2, 512], BF16)
        vc1 = c.sb("vc1", [128, 4, 4, 65], BF16)
        ov_sb = c.sb("ov_sb", [128, 4, 128], BF16)
        m_bk = [View(h2f[0:5, i * 512:(i + 1) * 512], Res("m_bk")) for i in range(2)]
        b_bk = [View(h2f[0:5, 1024 + i * 512:1024 + (i + 1) * 512], Res("b_bk")) for i in range(2)]
        pt_i = c.sb("pt_i", [128, 64], I32)
        idx_f = c.sb("idx_f", [128, 64])
        idx_u = c.sb("idx_u", [128, 64], U32)
        pio_t = c.sb("pio_t", [128, 1])
        ropa = View(h2f[:, 0:1024], h2f)
        ropb = View(h2f[:, 1024:2048], h2f)
        rows_t = View(yt[:, 0:1024], yt)
        win_t = View(yt[:, 1024:1536], yt)
        x1 = xt1
        xp2 = yt
        hb2 = hb
        cc_t = xt1
        cc_s = junk
        scv_t = View(junk[0:8, 0:DC], junk)

        pT = [c.ps("pT%d" % i, [128, 8, 128], BF16) for i in range(2)]
        pc_full = [c.ps("pc%d" % i, [128, 512]) for i in range(2)]

        class _PCV:
            def __init__(self, t):
                self.r = t.r
                self.v = t[:, 0:260].rearrange("p (a b) -> p a b", b=130)

            def __getitem__(self, k):
                return self.v[k]

        pc = [_PCV(t) for t in pc_full]
        pq = [c.ps("pq%d" % i, [128, 512]) for i in range(4)]
        pT1f = View(pT[1][:].rearrange("p a b -> p (a b)").bitcast(F32), pT[1])
        pqb = [View(pq[i][:].bitcast(BF16).rearrange("p (a b) -> p a b", b=128), pq[i]) for i in range(4)]

        state = {"wb": 0, "pq": 0, "pc": 0, "wbufs": None}

        def next_wb():
            wl = state["wbufs"]
            b = wl[state["wb"] % 2]
            state["wb"] += 1
            return b

        def next_pq():
            b = pq[state["pq"] % 4]
            state["pq"] += 1
            return b

        def next_pc():
            b = pc[state["pc"] % 2]
            state["pc"] += 1
            return b

        def load_w(view, c0, ncol):
            b = next_wb()
            c.dma("pool", lambda e: e.dma_start(out=b[:, :, 0:ncol], in_=view[:, :, c0:c0 + ncol]), writes=[b, b.r2])
            return b

        esA = contextlib.ExitStack()
        c.es = esA
        wkv = c.sb("wkv", [128, 16, 1024], BF16)
        wkv2 = c.sb("wkv2", [128, 16, 512], BF16)
        rawT = [c.sb("rawT%d" % i, [128, 4, 2064], BF16) for i in range(2)]
        kTs_st = c.sb("kTs_st", [128, 2, 1024], BF16)
        kTw_st = c.sb("kTw_st", [128, 2, 1024], BF16)
        vs_st = c.sb("vs_st", [128, 4, 8, 65], BF16)
        vw_st = c.sb("vw_st", [128, 4, 8, 65], BF16)
        ks_b = c.sb("ks_b", [128, 256], BF16)
        w1_sb = c.sb("w1_sb", [128, 2, 32, 128], BF16)
        peT = c.sb("peT", [128, 2, 34], BF16)
        w2_sb = c.sb("w2_sb", [128, 2, 64], BF16)
        b1T = c.sb("b1T", [128, 2])
        b2bc = c.sb("b2bc", [128, 2, 64])
        biasH = c.sb("biasH", [128, 2])
        hidT = c.sb("hidT", [128, 128], BF16)
        kc_tok = c.sb("kc_tok", [128, 4, 64])
        kcr = c.sb("kcr", [128, 256], BF16)
        hT_alt = c.sb("hT_alt", [128, 16, 128], BF16)
        c.es = es

        class _WV:
            def __init__(self, t):
                self.v = t[:].rearrange("p a b -> p (a b)")[:, 0:8192].rearrange("p (k n) -> p k n", n=512)
                self.r = t.r
                self.r2 = Res("dummy")

            def __getitem__(self, k):
                return self.v[k]

        state["wbufs"] = [_WV(rawT[0]), _WV(rawT[1])]

        c.dma("sp", lambda e: e.dma_start(out=idf[:], in_=idn), writes=[idf])
        c.dma("sp", lambda e: e.dma_start(out=pfl_t[:], in_=pfl), writes=[pfl_t])
        for k_ in range(3):
            c.dma("sp", lambda e: e.dma_start(out=cwT[:, :, k_], in_=conv_w[k_].rearrange("(c p) -> p c", p=128),
                                              allow_slow_non_contiguous=True), writes=[cwT])
        c.dma("sp", lambda e: e.dma_start(out=cbT[:], in_=conv_b.rearrange("o (c p) -> p (o c)", p=128),
                                          allow_slow_non_contiguous=True), writes=[cbT])
        c.op("pool", lambda e: e.memset(zer[:], 0.0), writes=[zer])
        c.dma("sp", lambda e: e.dma_start(out=cc_t[0:5, :], in_=cc), writes=[cc_t])
        c.op("dve", lambda e: e.tensor_copy(out=idb[:], in_=idf[:]), reads=[idf], writes=[idb])

        c.op("act", lambda e: e.activation(out=cc_s[0:5, :], in_=cc_t[0:5, :], func=AF.Silu), reads=[cc_t], writes=[cc_s])
        pa = next_pq()
        for k in range(16):
            c.op("pe", lambda e: e.transpose(out=pa[:, k * 5:(k + 1) * 5], in_=cc_s[0:5, k * 128:(k + 1) * 128],
                                             identity=idf[0:5, 0:5]), reads=[cc_s, idf], writes=[pa])
        c.op("dve", lambda e: e.tensor_copy(out=ccT[:].rearrange("p a b -> p (a b)"), in_=pa[:, 0:80]),
             reads=[pa], writes=[ccT])
        for n in range(0 if _DBG else 24):
            b = load_w(w_ada_v, n * 512, 512)
            p = next_pq()
            for k in range(16):
                c.op("pe", lambda e: e.matmul(p[0:5, :], lhsT=ccT[:, k, :], rhs=b[:, k, :], start=(k == 0), stop=(k == 15)),
                     reads=[ccT, b, b.r2], writes=[p])
            bb = b_bk[n % 2]
            mb = m_bk[n % 2]
            c.dma("sp", lambda e: e.dma_start(out=bb[:, :], in_=b_ada[:, n * 512:(n + 1) * 512].partition_broadcast(5)), writes=[bb])
            c.op("dve", lambda e: e.tensor_tensor(out=mb[:, :], in0=p[0:5, :], in1=bb[:, :], op=ALU.add), reads=[p, bb], writes=[mb])
            c.dma("sp", lambda e: e.dma_start(out=m_dram[:, n * 512:(n + 1) * 512], in_=mb[:, :]), reads=[mb], writes=[m_res])

        c.barrier()
        import os as _os0
        if _os0.environ.get("KDBG", "") == "0":
            c.finish()
            esA.close()
            return nc

        def gen_mod(dst, j, P, col0, kind, gain=None):
            tgt = dst if kind == "plain" else junk
            if P == 128:
                c.dma("sp", lambda e: e.dma_start(out=tgt[:, :], in_=m_dram[0:1, j * D:(j + 1) * D].partition_broadcast(128)),
                      reads=[m_res], writes=[tgt])
            else:
                for s_ in range(NS):
                    c.dma("sp", lambda e: e.dma_start(out=tgt[4 * s_:4 * s_ + 4, :],
                                                      in_=m_dram[1 + s_:2 + s_, j * D:(j + 1) * D].partition_broadcast(4)),
                          reads=[m_res], writes=[tgt])
            if kind != "plain":
                c.dma("sp", lambda e: e.dma_start(out=facc[0:P, :], in_=gain.partition_broadcast(P)), writes=[facc])
                c.op("dve", lambda e: e.scalar_tensor_tensor(out=dst[0:P, :], in0=junk[0:P, :], scalar=1.0, in1=facc[0:P, :],
                                                              op0=ALU.add, op1=ALU.mult), reads=[junk, facc], writes=[dst])

        def norm_mod(P, xin, hout, stt, A, B):
            c.op("act", lambda e: e.activation(out=junk[0:P, :], in_=xin[0:P, :], func=AF.Square, accum_out=stt[0:P, 0:1]),
                 reads=[xin], writes=[junk, stt])
            c.op("dve", lambda e: e.tensor_scalar(out=stt[0:P, 1:2], in0=stt[0:P, 0:1], scalar1=1.0 / D, scalar2=EPS,
                                                  op0=ALU.mult, op1=ALU.add), reads=[stt], writes=[stt])
            c.op("act", lambda e: e.activation(out=stt[0:P, 2:3], in_=stt[0:P, 1:2], func=AF.Sqrt), reads=[stt], writes=[stt])
            c.op("dve", lambda e: e.reciprocal(out=stt[0:P, 3:4], in_=stt[0:P, 2:3]), reads=[stt], writes=[stt])
            c.op("dve", lambda e: e.scalar_tensor_tensor(out=junk[0:P, :], in0=xin[0:P, :], scalar=stt[0:P, 3:4], in1=A[0:P, :],
                                                          op0=ALU.mult, op1=ALU.mult), reads=[xin, stt, A], writes=[junk])
            c.op("dve", lambda e: e.tensor_tensor(out=hout[0:P, :], in0=junk[0:P, :], in1=B[0:P, :], op=ALU.add),
                 reads=[junk, B], writes=[hout])

        def transpose_h(P, hsrc, col0, dst=None):
            hT = hT_main if dst is None else dst
            for half in range(2):
                for k in range(8):
                    kk = half * 8 + k
                    c.op("pe", lambda e: e.transpose(out=pT[half][:, k, 0:P], in_=hsrc[0:P, kk * 128:(kk + 1) * 128],
                                                     identity=idb[0:P, 0:P]), reads=[hsrc, idb], writes=[pT[half]])
                eng = "act" if half == 0 else "dve"
                if eng == "act":
                    c.op("act", lambda e: e.copy(out=hT[:, half * 8:(half + 1) * 8, col0:col0 + P], in_=pT[half][:, :, 0:P]),
                         reads=[pT[half]], writes=[hT])
                else:
                    c.op("dve", lambda e: e.tensor_copy(out=hT[:, half * 8:(half + 1) * 8, col0:col0 + P], in_=pT[half][:, :, 0:P]),
                         reads=[pT[half]], writes=[hT])

        def rope_into(P, src, nh, cst, dst):
            s3 = src[0:P, 0:nh * 64].rearrange("p (h d) -> p h d", d=64)
            a3 = ropa[0:P, 0:nh * 64].rearrange("p (h d) -> p h d", d=64)
            b3 = ropb[0:P, 0:nh * 64].rearrange("p (h d) -> p h d", d=64)
            d3 = dst.rearrange("p (h d) -> p h d", d=64)
            cos2 = cst[0:P, 0:64].unsqueeze(1).broadcast_to([P, nh, 64])
            nsin = cst[0:P, 64:96].unsqueeze(1).broadcast_to([P, nh, 32])
            psin = cst[0:P, 96:128].unsqueeze(1).broadcast_to([P, nh, 32])
            c.op("dve", lambda e: e.tensor_tensor(out=a3, in0=s3, in1=cos2, op=ALU.mult), reads=[src, cst], writes=[ropa])
            c.op("dve", lambda e: e.tensor_tensor(out=b3[:, :, 0:32], in0=s3[:, :, 32:64], in1=nsin, op=ALU.mult),
                 reads=[src, cst], writes=[ropb])
            c.op("dve", lambda e: e.tensor_tensor(out=b3[:, :, 32:64], in0=s3[:, :, 0:32], in1=psin, op=ALU.mult),
                 reads=[src, cst], writes=[ropb])
            return a3, b3, d3

        def proc_tile(P, NTOK, xin, is_sample, slot):
            off = NTOK - P
            for bank in range(6):
                b = load_w(w_in_v, bank * 512, 512)
                for half in range(2):
                    p = next_pc()
                    for jj in range(2):
                        j4 = half * 2 + jj
                        for k in range(16):
                            c.op("pe", lambda e: e.matmul(p[:, jj, 0:NTOK], lhsT=b[:, k, j4 * 128:(j4 + 1) * 128],
                                                          rhs=hT[:, k, 0:NTOK], start=(k == 0), stop=(k == 15)),
                                 reads=[b, b.r2, hT], writes=[p])
                    j0 = (bank % 2) * 4 + half * 2
                    if bank < 2:
                        c.op("act", lambda e: e.copy(out=bgT[:, j0:j0 + 2, 0:P], in_=p[:, :, off:NTOK]), reads=[p], writes=[bgT])
                    elif bank < 4:
                        c.op("act", lambda e: e.copy(out=cgT[:, j0:j0 + 2, 0:NTOK], in_=p[:, :, 0:NTOK]), reads=[p], writes=[cgT])
                    else:
                        c.op("dve", lambda e: e.tensor_tensor(out=zcT[:, j0:j0 + 2, 0:NTOK], in0=p[:, :, 0:NTOK],
                                                              in1=cgT[:, j0:j0 + 2, 0:NTOK], op=ALU.mult),
                             reads=[p, cgT], writes=[zcT])
            if not is_sample:
                c.op("dve", lambda e: e.tensor_scalar(out=zcT[:, :, 0:2], in0=zcT[:, :, 0:2], scalar1=pfl_t[:, slot:slot + 1],
                                                      scalar2=None, op0=ALU.mult), reads=[zcT, pfl_t], writes=[zcT])
                for j in range(8):
                    c.op("dve", lambda e: e.tensor_scalar(out=acc[:, :], in0=zcT[:, j, 2:130], scalar1=cwT[:, j, 2:3],
                                                          scalar2=cbT[:, j:j + 1], op0=ALU.mult, op1=ALU.add),
                         reads=[zcT, cwT, cbT], writes=[acc])
                    c.op("dve", lambda e: e.scalar_tensor_tensor(out=acc[:, :], in0=zcT[:, j, 1:129], scalar=cwT[:, j, 1:2],
                                                                  in1=acc[:, :], op0=ALU.mult, op1=ALU.add),
                         reads=[zcT, cwT, acc], writes=[acc])
                    c.op("dve", lambda e: e.scalar_tensor_tensor(out=acc[:, :], in0=zcT[:, j, 0:128], scalar=cwT[:, j, 0:1],
                                                                  in1=acc[:, :], op0=ALU.mult, op1=ALU.add),
                         reads=[zcT, cwT, acc], writes=[acc])
                    c.op("dve", lambda e: e.tensor_tensor(out=catT[:, j, :], in0=acc[:, :], in1=bgT[:, j, :], op=ALU.mult),
                         reads=[acc, bgT], writes=[catT])
                for t_ in range(2):
                    c.dma("sp", lambda e: e.dma_start(out=conv_o[slot, t_].rearrange("(c p) -> p c", p=128), in_=zcT[:, :, 128 + t_],
                                                      allow_slow_non_contiguous=True), reads=[zcT], is_output=True)
            else:
                c.op("dve", lambda e: e.tensor_copy(out=sctx[:, :, :, 2:6],
                                                    in_=zcT[:, :, 0:16].rearrange("p c (s t) -> p c s t", t=4)),
                     reads=[zcT], writes=[sctx])
                for j in range(8):
                    c.op("dve", lambda e: e.tensor_scalar(out=sacc[:, :, :], in0=sctx[:, j, :, 2:6], scalar1=cwT[:, j, 2:3],
                                                          scalar2=cbT[:, j:j + 1], op0=ALU.mult, op1=ALU.add),
                         reads=[sctx, cwT, cbT], writes=[sacc])
                    c.op("dve", lambda e: e.scalar_tensor_tensor(out=sacc[:, :, :], in0=sctx[:, j, :, 1:5], scalar=cwT[:, j, 1:2],
                                                                  in1=sacc[:, :, :], op0=ALU.mult, op1=ALU.add),
                         reads=[sctx, cwT, sacc], writes=[sacc])
                    c.op("dve", lambda e: e.scalar_tensor_tensor(out=sacc[:, :, :], in0=sctx[:, j, :, 0:4], scalar=cwT[:, j, 0:1],
                                                                  in1=sacc[:, :, :], op0=ALU.mult, op1=ALU.add),
                         reads=[sctx, cwT, sacc], writes=[sacc])
                    c.op("dve", lambda e: e.tensor_tensor(out=catT[:, j, 0:16].rearrange("p (s t) -> p s t", t=4), in0=sacc[:, :, :],
                                                          in1=bgT[:, j, 0:16].rearrange("p (s t) -> p s t", t=4), op=ALU.mult),
                         reads=[sacc, bgT], writes=[catT])
                for s_ in range(NS):
                    for t_ in range(2):
                        c.dma("sp", lambda e: e.dma_start(out=conv_s[s_, t_].rearrange("(c p) -> p c", p=128), in_=sctx[:, :, s_, 4 + t_],
                                                          allow_slow_non_contiguous=True), reads=[sctx], is_output=True)

            cst = cs_t
            for bank in range(2):
                b = load_w(w_in_v, 3072 + bank * 512, 512)
                p = next_pq()
                for k in range(16):
                    c.op("pe", lambda e: e.matmul(p[0:P, :], lhsT=hT[:, k, off:NTOK], rhs=b[:, k, :], start=(k == 0), stop=(k == 15)),
                         reads=[hT, b, b.r2], writes=[p])
                a3, b3, d3 = rope_into(P, p, 8, cst, q_r[0:P, bank * 512:(bank + 1) * 512])
                d4 = q_r[0:P, bank * 512:(bank + 1) * 512].rearrange("p (r f d) -> p f r d", r=4, f=2, d=64)
                c.op("dve", lambda e: e.tensor_tensor(out=d4, in0=a3.rearrange("p (f r) d -> p f r d", f=2),
                                                      in1=b3.rearrange("p (f r) d -> p f r d", f=2), op=ALU.add),
                     reads=[ropa, ropb], writes=[q_r])
            for bank in range(3):
                b = load_w(w_in_v, 4096 + bank * 512, 512)
                p = next_pq()
                for k in range(16):
                    c.op("pe", lambda e: e.matmul(p[0:P, :], lhsT=hT[:, k, off:NTOK], rhs=b[:, k, :], start=(k == 0), stop=(k == 15)),
                         reads=[hT, b, b.r2], writes=[p])
                if bank == 0:
                    c.op("act", lambda e: e.copy(out=rows_t[0:P, 0:512], in_=p[0:P, :]), reads=[p], writes=[rows_t])
                elif bank == 1:
                    a3, b3, d3 = rope_into(P, p, 4, cst, rows_t[0:P, 512:768])
                    c.op("dve", lambda e: e.tensor_tensor(out=d3, in0=a3, in1=b3, op=ALU.add), reads=[ropa, ropb], writes=[rows_t])
                    c.op("act", lambda e: e.copy(out=rows_t[0:P, 768:1024], in_=p[0:P, 256:512]), reads=[p], writes=[rows_t])
                else:
                    a3, b3, d3 = rope_into(P, p, 4, cst, win_t[0:P, 0:256])
                    c.op("dve", lambda e: e.tensor_tensor(out=d3, in0=a3, in1=b3, op=ALU.add), reads=[ropa, ropb], writes=[win_t])
                    c.op("act", lambda e: e.copy(out=win_t[0:P, 256:512], in_=p[0:P, 256:512]), reads=[p], writes=[win_t])
            if not is_sample:
                c.dma("sp", lambda e: e.dma_start(out=rows_o[slot], in_=rows_t[:]), reads=[rows_t], is_output=True)
                c.dma("sp", lambda e: e.dma_start(out=win_o[slot], in_=win_t[:]), reads=[win_t], is_output=True)
            else:
                c.dma("sp", lambda e: e.dma_start(out=rows_s, in_=rows_t[0:16, :]), reads=[rows_t], is_output=True)
                for s_ in range(NS):
                    c.dma("sp", lambda e: e.dma_start(out=win_s[s_, 508:512, :], in_=win_t[4 * s_:4 * s_ + 4, :]),
                          reads=[win_t], is_output=True)

            b = load_w(w_in_v, 5632, 48)
            p = next_pq()
            for k in range(16):
                c.op("pe", lambda e: e.matmul(p[0:P, 0:48], lhsT=hT[:, k, off:NTOK], rhs=b[:, k, 0:48], start=(k == 0), stop=(k == 15)),
                     reads=[hT, b, b.r2], writes=[p])
            c.op("act", lambda e: e.activation(out=gates_t[0:P, :], in_=p[0:P, 0:48], func=AF.Sigmoid), reads=[p], writes=[gates_t])
            if is_sample:
                if "phaseS" in _SKIP:
                    c.op("pool", lambda e: e.memset(catT[:, 8:16, :], 0.0), writes=[catT])
                else:
                    attention_sample()
            else:
                attention_prompt(slot)

            for n in range(4):
                b = load_w(w_out_v, n * 512, 512)
                p = next_pq()
                for k in range(16):
                    c.op("pe", lambda e: e.matmul(p[0:P, :], lhsT=catT[:, k, 0:P], rhs=b[:, k, :], start=(k == 0), stop=(k == 15)),
                         reads=[catT, b, b.r2], writes=[p])
                c.op("dve", lambda e: e.tensor_tensor(out=junk[0:P, n * 512:(n + 1) * 512], in0=p[0:P, :],
                                                      in1=modG[0:P, n * 512:(n + 1) * 512], op=ALU.mult), reads=[p, modG], writes=[junk])
                c.op("dve", lambda e: e.tensor_tensor(out=x1[0:P, n * 512:(n + 1) * 512], in0=junk[0:P, n * 512:(n + 1) * 512],
                                                       in1=xin[0:P, n * 512:(n + 1) * 512], op=ALU.add), reads=[junk, xin], writes=[x1])
            col0 = 128 if is_sample else 0
            gen_mod(modA, 4, P, col0, "scale", gain=g2)
            gen_mod(modB, 3, P, col0, "plain")
            gen_mod(modG, 5, P, col0, "plain")
            norm_mod(P, x1, h2f, st1, modA, modB)
            c.op("act", lambda e: e.copy(out=hb[0:P, :], in_=h2f[0:P, :]), reads=[h2f], writes=[hb])
            transpose_h(P, hb, 0)
            qT = catT
            for n in range(4):
                b = load_w(wq_v, n * 512, 512)
                for half in range(2):
                    p = next_pc()
                    for jj in range(2):
                        j4 = half * 2 + jj
                        for k in range(16):
                            c.op("pe", lambda e: e.matmul(p[:, jj, 0:P], lhsT=b[:, k, j4 * 128:(j4 + 1) * 128],
                                                          rhs=hT[:, k, 0:P], start=(k == 0), stop=(k == 15)),
                                 reads=[b, b.r2, hT], writes=[p])
                    j0 = n * 4 + half * 2
                    c.op("act", lambda e: e.copy(out=qT[:, j0:j0 + 2, 0:P], in_=p[:, :, 0:P]), reads=[p], writes=[qT])
            S = junk
            S3 = junk[:].rearrange("p (j k) -> p j k", k=128)
            for n in range(4):
                p = next_pq()
                for jj in range(4):
                    j = n * 4 + jj
                    c.op("pe", lambda e: e.matmul(p[0:P, jj * 128:(jj + 1) * 128], lhsT=qT[:, j, 0:P], rhs=keysT[:, j, :],
                                                  start=True, stop=True), reads=[qT, keysT], writes=[p])
                c.op("act", lambda e: e.copy(out=junk[0:P, n * 512:(n + 1) * 512], in_=p[0:P, :]), reads=[p], writes=[S])
            for hp in range(8):
                for ci in range(2):
                    tt, ii = t12[ci], i12[ci]
                    src = S3[0:P, 2 * hp + ci, :]
                    c.op("dve", lambda e: e.max(out=tt[0:P, 0:8], in_=src), reads=[S], writes=[tt])
                    c.op("dve", lambda e: e.max_index(out=ii[0:P, 0:8], in_max=tt[0:P, 0:8], in_values=src), reads=[S, tt], writes=[ii])
                    c.op("dve", lambda e: e.match_replace(out=tmp128[0:P, :], in_to_replace=tt[0:P, 0:8], in_values=src, imm_value=-1e30),
                         reads=[S, tt], writes=[tmp128])
                    c.op("dve", lambda e: e.max(out=tt[0:P, 8:16], in_=tmp128[0:P, :]), reads=[tmp128], writes=[tt])
                    c.op("dve", lambda e: e.max_index(out=ii[0:P, 8:16], in_max=tt[0:P, 8:16], in_values=tmp128[0:P, :]),
                         reads=[tmp128, tt], writes=[ii])
                c.op("dve", lambda e: e.tensor_scalar(out=if12[0][0:P, :], in0=i12[0][0:P, :], scalar1=128.0, scalar2=None, op0=ALU.mult),
                     reads=[i12[0]], writes=[if12[0]])
                c.op("dve", lambda e: e.tensor_copy(out=if12[1][0:P, :], in_=i12[1][0:P, :]), reads=[i12[1]], writes=[if12[1]])
                cand3 = cand[0:P, :].rearrange("p (a b) -> p a b", b=16)
                cidx3 = cidx[0:P, :].rearrange("p (a b) -> p a b", b=16)
                c.op("dve", lambda e: e.tensor_tensor(out=cand3, in0=t12[0][0:P, :].unsqueeze(2).broadcast_to([P, 16, 16]),
                                                      in1=t12[1][0:P, :].unsqueeze(1).broadcast_to([P, 16, 16]), op=ALU.add),
                     reads=[t12[0], t12[1]], writes=[cand])
                c.op("dve", lambda e: e.tensor_tensor(out=cidx3, in0=if12[0][0:P, :].unsqueeze(2).broadcast_to([P, 16, 16]),
                                                      in1=if12[1][0:P, :].unsqueeze(1).broadcast_to([P, 16, 16]), op=ALU.add),
                     reads=[if12[0], if12[1]], writes=[cidx])
                c.op("dve", lambda e: e.max(out=c16[0:P, 0:8], in_=cand[0:P, :]), reads=[cand], writes=[c16])
                c.op("dve", lambda e: e.match_replace(out=tmp256[0:P, :], in_to_replace=c16[0:P, 0:8], in_values=cand[0:P, :], imm_value=-1e30),
                     reads=[cand, c16], writes=[tmp256])
                c.op("dve", lambda e: e.max(out=c16[0:P, 8:16], in_=tmp256[0:P, :]), reads=[tmp256], writes=[c16])
                for k in range(16):
                    c.op("dve", lambda e: e.scalar_tensor_tensor(out=tmp256[0:P, :], in0=cand[0:P, :], scalar=c16[0:P, k:k + 1],
                                                                  in1=cidx[0:P, :], op0=ALU.is_equal, op1=ALU.mult),
                         reads=[cand, c16, cidx], writes=[tmp256])
                    c.op("dve", lambda e: e.tensor_reduce(out=ef[0:P, hp * 16 + k:hp * 16 + k + 1], in_=tmp256[0:P, :], axis=AX.X, op=ALU.add),
                         reads=[tmp256], writes=[ef])
                c.op("dve", lambda e: e.tensor_scalar(out=pst[0:P, 0:1], in0=c16[0:P, 0:1], scalar1=-1.0, scalar2=None, op0=ALU.mult),
                     reads=[c16], writes=[pst])
                c.op("act", lambda e: e.activation(out=gw[0:P, hp * 16:(hp + 1) * 16], in_=c16[0:P, :], func=AF.Exp,
                                                   bias=pst[0:P, 0:1], scale=1.0, accum_out=pst[0:P, 1:2]),
                     reads=[c16, pst], writes=[gw, pst])
                c.op("dve", lambda e: e.reciprocal(out=pst[0:P, 2:3], in_=pst[0:P, 1:2]), reads=[pst], writes=[pst])
                c.op("dve", lambda e: e.tensor_scalar(out=gw[0:P, hp * 16:(hp + 1) * 16], in0=gw[0:P, hp * 16:(hp + 1) * 16],
                                                      scalar1=pst[0:P, 2:3], scalar2=None, op0=ALU.mult), reads=[gw, pst], writes=[gw])
            c.op("dve", lambda e: e.tensor_scalar(out=ef[0:P, :], in0=ef[0:P, :], scalar1=0.0, scalar2=16383.0, op0=ALU.max, op1=ALU.min),
                 reads=[ef], writes=[ef])
            c.op("dve", lambda e: e.tensor_copy(out=eu[0:P, :], in_=ef[0:P, :]), reads=[ef], writes=[eu])
            for j in range(128):
                U = (Ub + Vb)[j % 4]
                c.dma("pool", lambda e: e.indirect_dma_start(out=U[0:P, :], out_offset=None, in_=peer_u[:, :],
                                                             in_offset=bass.IndirectOffsetOnAxis(ap=eu[0:P, j:j + 1], axis=0)),
                      reads=[eu], writes=[U])
                prod = yt if j % 2 == 0 else junk
                c.op("dve", lambda e: e.tensor_tensor(out=prod[0:P, :], in0=U[0:P, :], in1=h2f[0:P, :], op=ALU.mult),
                     reads=[U, h2f], writes=[prod])
                c.op("act", lambda e: e.activation(out=prod[0:P, :], in_=prod[0:P, :], func=AF.Identity, accum_out=adot[0:P, j:j + 1]),
                     reads=[prod], writes=[prod, adot])
            c.op("act", lambda e: e.activation(out=coef[0:P, :], in_=adot[0:P, :], func=AF.Gelu_apprx_tanh), reads=[adot], writes=[coef])
            c.op("dve", lambda e: e.tensor_tensor(out=coef[0:P, :], in0=coef[0:P, :], in1=gw[0:P, :], op=ALU.mult),
                 reads=[coef, gw], writes=[coef])
            for j in range(128):
                V = (Vb + Ub)[j % 4]
                c.dma("pool", lambda e: e.indirect_dma_start(out=V[0:P, :], out_offset=None, in_=peer_v[:, :],
                                                             in_offset=bass.IndirectOffsetOnAxis(ap=eu[0:P, j:j + 1], axis=0)),
                      reads=[eu], writes=[V])
                if j == 0:
                    c.op("dve", lambda e: e.tensor_scalar(out=facc[0:P, :], in0=V[0:P, :], scalar1=coef[0:P, 0:1], scalar2=None, op0=ALU.mult),
                         reads=[V, coef], writes=[facc])
                else:
                    c.op("dve", lambda e: e.scalar_tensor_tensor(out=facc[0:P, :], in0=V[0:P, :], scalar=coef[0:P, j:j + 1], in1=facc[0:P, :],
                                                                  op0=ALU.mult, op1=ALU.add), reads=[V, coef, facc], writes=[facc])
            c.op("dve", lambda e: e.tensor_tensor(out=facc[0:P, :], in0=facc[0:P, :], in1=modG[0:P, :], op=ALU.mult),
                 reads=[facc, modG], writes=[facc])
            c.op("dve", lambda e: e.tensor_tensor(out=x1[0:P, :], in0=x1[0:P, :], in1=facc[0:P, :], op=ALU.add),
                 reads=[x1, facc], writes=[x1])
            stt = st1
            c.op("act", lambda e: e.activation(out=junk[0:P, :], in_=x1[0:P, :], func=AF.Square, accum_out=stt[0:P, 0:1]),
                 reads=[x1], writes=[junk, stt])
            c.op("dve", lambda e: e.tensor_scalar(out=stt[0:P, 1:2], in0=stt[0:P, 0:1], scalar1=1.0 / D, scalar2=EPS,
                                                  op0=ALU.mult, op1=ALU.add), reads=[stt], writes=[stt])
            c.op("act", lambda e: e.activation(out=stt[0:P, 2:3], in_=stt[0:P, 1:2], func=AF.Sqrt), reads=[stt], writes=[stt])
            c.op("dve", lambda e: e.reciprocal(out=stt[0:P, 3:4], in_=stt[0:P, 2:3]), reads=[stt], writes=[stt])
            c.dma("sp", lambda e: e.dma_start(out=facc[0:P, :], in_=fg.partition_broadcast(P)), writes=[facc])
            c.op("dve", lambda e: e.scalar_tensor_tensor(out=yt[0:P, :], in0=x1[0:P, :], scalar=stt[0:P, 3:4], in1=facc[0:P, :],
                                                          op0=ALU.mult, op1=ALU.mult), reads=[x1, stt, facc], writes=[yt])
            if not is_sample:
                c.dma("sp", lambda e: e.dma_start(out=yo[slot], in_=yt[:]), reads=[yt], is_output=True)
            else:
                c.dma("sp", lambda e: e.dma_start(out=ys, in_=yt[0:16, :]), reads=[yt], is_output=True)

        gen_mod(modA, 1, 128, 0, "scale", gain=g1)
        gen_mod(modB, 0, 128, 0, "plain")
        c.dma("pool", lambda e: e.dma_start(out=wkv[:], in_=w_in_v[:, :, 4096:5120]), writes=[wkv, rawT[0], rawT[1]])
        c.dma("pool", lambda e: e.dma_start(out=wkv2[:], in_=w_in_v[:, :, 5120:5632]), writes=[wkv2])
        c.op("pool", lambda e: e.memset(peT[:], 0.0), writes=[peT])
        for hh in range(2):
            for a_ in range(2):
                c.dma("pool", lambda e: e.dma_start(out=w1_sb[64 * hh:64 * hh + 64, a_, :, :], in_=cmp_w1[a_].rearrange("s d h -> d s h")),
                      writes=[w1_sb])
        c.dma("sp", lambda e: e.dma_start(out=tmp256[0:32, 0:128].rearrange("p (a d) -> p a d", a=2), in_=cmp_pe.rearrange("a s d -> s a d")),
              writes=[tmp256])
        ppe = next_pq()
        for a_ in range(2):
            c.op("pe", lambda e: e.transpose(out=ppe[0:64, a_ * 32:(a_ + 1) * 32], in_=tmp256[0:32, a_ * 64:(a_ + 1) * 64], identity=idf[0:32, 0:32]),
                 reads=[tmp256, idf], writes=[ppe])
        c.op("dve", lambda e: e.tensor_copy(out=peT[0:64, :, 0:32], in_=ppe[0:64, 0:64].rearrange("p (a s) -> p a s", a=2)),
             reads=[ppe], writes=[peT])
        c.dma("pool", lambda e: e.dma_start(out=w2_sb[:], in_=cmp_w2.rearrange("a h d -> h a d")), writes=[w2_sb])
        c.dma("sp", lambda e: e.dma_start(out=b1T[:], in_=cmp_b1.rearrange("a h -> h a"), allow_slow_non_contiguous=True), writes=[b1T])
        for a_ in range(2):
            c.dma("sp", lambda e: e.dma_start(out=b2bc[:, a_, :], in_=cmp_b2[a_:a_ + 1, :].partition_broadcast(128)), writes=[b2bc])
        c.dma("pool", lambda e: e.dma_start(out=ov_sb[:], in_=ovm), writes=[ov_sb])
        c.op("pool", lambda e: e.memset(vs_st[:], 1.0), writes=[vs_st])
        c.op("pool", lambda e: e.memset(vw_st[:], 1.0), writes=[vw_st])
        c.op("pool", lambda e: e.memset(vc1[:], 1.0), writes=[vc1])
        for a_ in range(2):
            p = next_pq()
            for s_ in range(32):
                c.op("pe", lambda e: e.matmul(p[:, 0:2], lhsT=w1_sb[0:64, a_, s_, :], rhs=peT[0:64, a_, s_:s_ + 2],
                                              start=(s_ == 0), stop=(s_ == 31)), reads=[w1_sb, peT], writes=[p])
            c.op("dve", lambda e: e.tensor_tensor(out=biasH[:, a_:a_ + 1], in0=p[:, 0:1], in1=b1T[:, a_:a_ + 1], op=ALU.add),
                 reads=[p, b1T], writes=[biasH])

        def compress_group(H, kdst=None, vdst=None):
            if "compress" in _SKIP:
                return
            kdst = kcT if kdst is None else kdst
            vdst = vc1 if vdst is None else vdst
            rt = rawT[H % 2]
            c.dma("sp", lambda e: e.dma_start(out=cs_t[:], in_=cscmp[H]), writes=[cs_t])
            for a_ in range(2):
                for g in range(4):
                    p0 = 64 * (g % 2)
                    blk = a_ * 2 + g // 2
                    ph = next_pq()
                    for s_ in range(32):
                        c.op("pe", lambda e: e.matmul(ph[:, 0:128], lhsT=w1_sb[p0:p0 + 64, a_, s_, :],
                                                      rhs=rt[p0:p0 + 64, blk, s_:s_ + 16 * 127 + 1:16],
                                                      start=(s_ == 0), stop=(s_ == 31)), reads=[w1_sb, rt], writes=[ph])
                    c.op("act", lambda e: e.activation(out=hidT[:], in_=ph[:, 0:128], func=AF.Gelu_apprx_tanh, bias=biasH[:, a_:a_ + 1]),
                         reads=[ph, biasH], writes=[hidT])
                    po = next_pq()
                    c.op("pe", lambda e: e.matmul(po[:, 0:64], lhsT=hidT[:], rhs=w2_sb[:, a_, :], start=True, stop=True),
                         reads=[hidT, w2_sb], writes=[po])
                    if a_ == 0:
                        c.op("dve", lambda e: e.tensor_tensor(out=kc_tok[:, g, :], in0=po[:, 0:64], in1=b2bc[:, 0, :], op=ALU.add),
                             reads=[po, b2bc], writes=[kc_tok])
                    else:
                        c.op("dve", lambda e: e.tensor_tensor(out=vdst[:, H, g, 0:64], in0=po[:, 0:64], in1=b2bc[:, 1, :], op=ALU.add),
                             reads=[po, b2bc], writes=[vdst])
            kct = View(kc_tok[:].rearrange("p g d -> p (g d)"), kc_tok)
            a3, b3, d3 = rope_into(128, kct, 4, cs_t, kcr[:, :])
            c.op("dve", lambda e: e.tensor_tensor(out=d3, in0=a3, in1=b3, op=ALU.add), reads=[ropa, ropb], writes=[kcr])
            for gp in range(2):
                c.op("pe", lambda e: e.transpose(out=pT[0][:, gp, :], in_=kcr[:, gp * 128:(gp + 1) * 128], identity=idb[:]),
                     reads=[kcr, idb], writes=[pT[0]])
            c.op("act", lambda e: e.copy(out=kdst[:, :, H * 128:(H + 1) * 128], in_=pT[0][:, 0:2, :]), reads=[pT[0]], writes=[kdst])

        def flush_stage(G8):
            if "flush" in _SKIP:
                return
            c.dma("sp", lambda e: e.dma_start(out=kTs_d[:, :, G8 * 1024:(G8 + 1) * 1024], in_=kTs_st[:]), reads=[kTs_st], writes=[kv_res])
            c.dma("sp", lambda e: e.dma_start(out=kTw_d[:, :, G8 * 1024:(G8 + 1) * 1024], in_=kTw_st[:]), reads=[kTw_st], writes=[kv_res])
            for g in range(4):
                c.dma("sp", lambda e: e.dma_start(out=vs_d[g, :, G8 * 8:(G8 + 1) * 8, :], in_=vs_st[:, g, :, :]), reads=[vs_st], writes=[kv_res])
                c.dma("sp", lambda e: e.dma_start(out=vw_d[g, :, G8 * 8:(G8 + 1) * 8, :], in_=vw_st[:, g, :, :]), reads=[vw_st], writes=[kv_res])

        import os as _os1
        _ktiles = int(_os1.environ.get("KTILES", "64"))
        for t in range(_ktiles):
            xin = xt1
            c.dma("sp", lambda e: e.dma_start(out=xin[:], in_=xp[t]), writes=[xin])
            c.dma("sp", lambda e: e.dma_start(out=cs_t[:], in_=csa[t]), writes=[cs_t])
            hcur = hT_main if t % 2 == 0 else hT_alt
            hbc = hb
            norm_mod(128, xin, hbc, st1, modA, modB)
            transpose_h(128, hbc, 0, dst=hcur)
            G16, t16 = t // 16, t % 16
            t8 = t % 8
            p = next_pq()
            for blk in range(0 if "raw" in _SKIP else 4):
                for k in range(16):
                    c.op("pe", lambda e: e.matmul(p[:, blk * 128:(blk + 1) * 128], lhsT=wkv[:, k, blk * 128:(blk + 1) * 128],
                                                  rhs=hcur[:, k, 0:128], start=(k == 0), stop=(k == 15)), reads=[wkv, hcur], writes=[p])
            c.op("act", lambda e: e.copy(out=rawT[G16 % 2][:, :, t16 * 128:(t16 + 1) * 128],
                                         in_=p[:, :].rearrange("p (b t) -> p b t", t=128)), reads=[p], writes=[rawT[G16 % 2]])
            if t16 == 0 and G16 >= 1:
                prev = rawT[(G16 - 1) % 2]
                c.op("act", lambda e: e.copy(out=prev[:, :, 2048:2064], in_=p[:, :].rearrange("p (b t) -> p b t", t=128)[:, :, 0:16]),
                     reads=[p], writes=[prev])
                compress_group(G16 - 1)
                c.dma("sp", lambda e: e.dma_start(out=cs_t[:], in_=csa[t]), writes=[cs_t])
            for which in range(0 if "which" in _SKIP else 2):
                wsrc = wkv[:, :, 512:1024] if which == 0 else wkv2[:, :, :]
                wres = wkv if which == 0 else wkv2
                kst = kTs_st if which == 0 else kTw_st
                vst = vs_st if which == 0 else vw_st
                p = next_pq()
                for k in range(16):
                    c.op("pe", lambda e: e.matmul(p[:, :], lhsT=hcur[:, k, 0:128], rhs=wsrc[:, k, :], start=(k == 0), stop=(k == 15)),
                         reads=[hcur, wres], writes=[p])
                a3, b3, d3 = rope_into(128, p, 4, cs_t, ks_b[:, :])
                c.op("dve", lambda e: e.tensor_tensor(out=d3, in0=a3, in1=b3, op=ALU.add), reads=[ropa, ropb], writes=[ks_b])
                if "vcopy" not in _SKIP:
                    c.op("dve", lambda e: e.tensor_copy(out=vst[:, :, t8, 0:64], in_=p[:, 256:512].rearrange("p (g d) -> p g d", d=64)),
                         reads=[p], writes=[vst])
                if "ktr" in _SKIP:
                    continue
                for gp in range(2):
                    c.op("pe", lambda e: e.transpose(out=pT[0][:, gp, :], in_=ks_b[:, gp * 128:(gp + 1) * 128], identity=idb[:]),
                         reads=[ks_b, idb], writes=[pT[0]])
                c.op("dve", lambda e: e.tensor_copy(out=kst[:, :, t8 * 128:(t8 + 1) * 128], in_=pT[0][:, 0:2, :]),
                     reads=[pT[0]], writes=[kst])
            if t8 == 7:
                flush_stage(t // 8)
        last = rawT[3 % 2]
        c.op("pool", lambda e: e.memset(last[:, :, 2048:2064], 0.0), writes=[last])
        compress_group(3)
        c.barrier()

        if "phaseS" not in _SKIP:
            wkv_f = wkv[:].rearrange("p a b -> p (a b)").bitcast(F32)
            wkv2_b = wkv2[:].rearrange("p a b -> p (a b)")
            pgb = [View(wkv_f[:, i * 1024:(i + 1) * 1024], Res("pgb")) for i in range(2)]
            pgh2 = [View(wkv2_b[:, 0:1024], Res("pgh0")), View(wkv2_b[:, 4096:5120], Res("pgh1"))]
            kcT_s = View(wkv2_b[:, 1024:2048].rearrange("p (a b) -> p a b", a=2), Res("kcT_s"))
            vc1_s = View(wkv2_b[:, 2048:2048 + 1040].rearrange("p (a g d) -> p a g d", a=4, g=4), Res("vc1_s"))
            c.dma("sp", lambda e: e.dma_start(out=pio_t[:], in_=piota), writes=[pio_t])
            c.op("pool", lambda e: e.memset(vc1_s[:, :, :, :], 1.0), writes=[vc1_s])
            for b in range(NS):
                c.dma("sp", lambda e: e.dma_start(out=pt_i[:], in_=ptab[b:b + 1, :].partition_broadcast(128)), writes=[pt_i])
                c.op("dve", lambda e: e.tensor_scalar(out=idx_f[:], in0=pt_i[:], scalar1=128.0, scalar2=pio_t[:, 0:1], op0=ALU.mult, op1=ALU.add),
                     reads=[pt_i, pio_t], writes=[idx_f])
                c.op("dve", lambda e: e.tensor_scalar(out=idx_f[:], in0=idx_f[:], scalar1=0.0, scalar2=float(2560 * 128 - 1), op0=ALU.max, op1=ALU.min),
                     reads=[idx_f], writes=[idx_f])
                c.op("dve", lambda e: e.tensor_copy(out=idx_u[:], in_=idx_f[:]), reads=[idx_f], writes=[idx_u])
                for pg in range(64):
                    pb_ = pgb[pg % 2]
                    pgh = pgh2[pg % 2]
                    c.dma("pool", lambda e: e.indirect_dma_start(out=pb_[:, :], out_offset=None, in_=cache[:, :],
                                                                 in_offset=bass.IndirectOffsetOnAxis(ap=idx_u[:, pg:pg + 1], axis=0)),
                          reads=[idx_u], writes=[pb_])
                    c.op("act", lambda e: e.copy(out=pgh[:, :], in_=pb_[:, :]), reads=[pb_], writes=[pgh])
                    G16, t16, t8 = pg // 16, pg % 16, pg % 8
                    bA, bB = (pT[0], pT[1]) if pg % 2 == 0 else (pqb[0], pqb[1])
                    for blk in range(4):
                        c.op("pe", lambda e: e.transpose(out=bA[:, blk, :], in_=pgh[:, blk * 128:(blk + 1) * 128], identity=idb[:]),
                             reads=[pgh, idb], writes=[bA])
                    for gp in range(2):
                        c.op("pe", lambda e: e.transpose(out=bB[:, gp, :], in_=pgh[:, 512 + gp * 128:512 + (gp + 1) * 128], identity=idb[:]),
                             reads=[pgh, idb], writes=[bB])
                    c.op("dve", lambda e: e.tensor_copy(out=rawT[G16 % 2][:, :, t16 * 128:(t16 + 1) * 128], in_=bA[:, 0:4, :]),
                         reads=[bA], writes=[rawT[G16 % 2]])
                    c.op("act", lambda e: e.copy(out=kTs_st[:, :, t8 * 128:(t8 + 1) * 128], in_=bB[:, 0:2, :]),
                         reads=[bB], writes=[kTs_st])
                    if t16 == 0 and G16 >= 1:
                        prev = rawT[(G16 - 1) % 2]
                        c.op("dve", lambda e: e.tensor_copy(out=prev[:, :, 2048:2064], in_=bA[:, 0:4, 0:16]), reads=[bA], writes=[prev])
                        compress_group(G16 - 1, kcT_s, vc1_s)
                    c.op("dve", lambda e: e.tensor_copy(out=vs_st[:, :, t8, 0:64], in_=pgh[:, 768:1024].rearrange("p (g d) -> p g d", d=64)),
                         reads=[pgh], writes=[vs_st])
                    if t8 == 7:
                        G8 = pg // 8
                        c.dma("sp", lambda e: e.dma_start(out=kTs_s[b, :, :, G8 * 1024:(G8 + 1) * 1024], in_=kTs_st[:]), reads=[kTs_st], writes=[kv_res])
                        for g in range(4):
                            c.dma("sp", lambda e: e.dma_start(out=vs_s[b, g, :, G8 * 8:(G8 + 1) * 8, :], in_=vs_st[:, g, :, :]), reads=[vs_st], writes=[kv_res])
                lastS = rawT[3 % 2]
                c.op("pool", lambda e: e.memset(lastS[:, :, 2048:2064], 0.0), writes=[lastS])
                compress_group(3, kcT_s, vc1_s)
                c.dma("sp", lambda e: e.dma_start(out=kc_s[b], in_=kcT_s[:, :, :]), reads=[kcT_s], writes=[kv_res])
                c.dma("sp", lambda e: e.dma_start(out=vc_s[b], in_=vc1_s[:, :, :, :]), reads=[vc1_s], writes=[kv_res])
                for wc in range(4):
                    pb_ = pgb[wc % 2]
                    pgh = pgh2[wc % 2]
                    c.dma("sp", lambda e: e.dma_start(out=pb_[:, 0:512], in_=swin[b, wc * 128:(wc + 1) * 128, :]), writes=[pb_])
                    c.op("act", lambda e: e.copy(out=pgh[:, 0:512], in_=pb_[:, 0:512]), reads=[pb_], writes=[pgh])
                    for gp in range(2):
                        c.op("pe", lambda e: e.transpose(out=pT[1][:, gp, :], in_=pgh[:, gp * 128:(gp + 1) * 128], identity=idb[:]),
                             reads=[pgh, idb], writes=[pT[1]])
                    c.op("dve", lambda e: e.tensor_copy(out=kTw_st[:, :, wc * 128:(wc + 1) * 128], in_=pT[1][:, 0:2, :]),
                         reads=[pT[1]], writes=[kTw_st])
                    c.op("dve", lambda e: e.tensor_copy(out=vw_st[:, :, wc, 0:64], in_=pgh[:, 256:512].rearrange("p (g d) -> p g d", d=64)),
                         reads=[pgh], writes=[vw_st])
                c.dma("sp", lambda e: e.dma_start(out=kTw_s[b], in_=kTw_st[:, :, 0:512]), reads=[kTw_st], writes=[kv_res])
                for g in range(4):
                    c.dma("sp", lambda e: e.dma_start(out=vw_s[b, g], in_=vw_st[:, g, 0:4, :]), reads=[vw_st], writes=[kv_res])
            c.barrier()
        esA.close()
        import os as _os
        _dbg = _os.environ.get("KDBG", "")
        if _dbg == "A":
            c.finish()
            return nc

        wb = [c.sb("wb%d" % i, [128, 16, 512], BF16) for i in range(2)]
        for b_ in wb:
            b_.r2 = Res("wb_hi")
        state["wbufs"] = wb

        def _f32v(b_):
            return b_[:].rearrange("p a b -> p (a b)").bitcast(F32)

        Ub = [View(_f32v(wb[0])[:, 0:D], wb[0].r), View(_f32v(wb[0])[:, D:2 * D], wb[0].r2)]
        Vb = [View(_f32v(wb[1])[:, 0:D], wb[1].r), View(_f32v(wb[1])[:, D:2 * D], wb[1].r2)]
        keysT = c.sb("keysT", [128, 16, 128], BF16)
        bgT = c.sb("bgT", [128, 8, 128])
        cgT = c.sb("cgT", [128, 8, 130])
        zcT = c.sb("zcT", [128, 8, 130])
        acc = c.sb("acc", [128, 128])
        catT = c.sb("catT", [128, 16, 128], BF16)
        q_r = c.sb("q_r", [128, 1024], BF16)
        sctx = c.sb("sctx", [128, 8, NS, 6])
        sacc = c.sb("sacc", [128, NS, 4])
        qTp = c.sb("qTp", [128, 8, 128], BF16)
        gates_t = c.sb("gates_t", [128, 48])
        e32_t = c.sb("e32_t", [32, 16, 128], BF16)
        fmask_t = c.sb("fmask_t", [128, 128])
        cmask_t = c.sb("cmask_t", [128, 4, 128], BF16)
        dmask_t = c.sb("dmask_t", [128, 8, 128], BF16)
        wmask_t = c.sb("wmask_t", [128, 12, 128], BF16)
        kbuf = [c.sb("kbuf%d" % i, [128, 1536], BF16) for i in range(2)]
        vbuf = [c.sb("vbuf%d" % i, [128, 12, 65], BF16) for i in range(2)]
        ebuf = [c.sb("ebuf%d" % i, [128, 4, 128], BF16) for i in range(2)]
        pTb = [c.sb("pTb%d" % i, [128, 4, 128], BF16) for i in range(2)]
        attn_f = c.sb("attn_f", [128, 16, 64])
        attn_b = c.sb("attn_b", [128, 1024], BF16)
        imp_t = c.sb("imp_t", [128, 128])
        sc_t = c.sb("sc_t", [128, 128])
        sc2_t = c.sb("sc2_t", [128, 128])
        sel_b = c.sb("sel_b", [128, 128], BF16)
        selT = c.sb("selT", [32, 4, 128], BF16)
        negT = c.sb("negT", [32, 4, 4, 128], BF16)
        m16 = c.sb("m16", [128, 16])
        rs4 = c.sb("rs4", [128, 8])
        c.dma("pool", lambda e: e.dma_start(out=e32_t[:], in_=e32), writes=[e32_t])
        kcT2 = c.sb("kcT2", [128, 2, 512], BF16)
        vc2 = c.sb("vc2", [128, 4, 4, 65], BF16)
        kn_b = c.sb("kn_b", [16, 512], BF16)
        kTn = c.sb("kTn", [128, 4, 16], BF16)
        vn = c.sb("vn", [16, 2, 4, 65], BF16)
        qs = c.sb("qs", [128, 8, 4], BF16)
        gq = c.sb("gq", [4, 48])
        cm_s = c.sb("cm_s", [128, 4, 4], BF16)
        fm_s = c.sb("fm_s", [4, 128])
        wm_s = c.sb("wm_s", [128, 4, 4], BF16)
        nm_s = c.sb("nm_s", [16, NS, 4], BF16)
        c.dma("pool", lambda e: e.dma_start(out=cm_s[:], in_=cmask_sm), writes=[cm_s])
        c.dma("pool", lambda e: e.dma_start(out=wm_s[:], in_=wmask_sm), writes=[wm_s])
        c.dma("pool", lambda e: e.dma_start(out=nm_s[:], in_=nmask), writes=[nm_s])
        c.dma("sp", lambda e: e.dma_start(out=fm_s[:], in_=fmask_sm), writes=[fm_s])

        c.dma("sp", lambda e: e.dma_start(out=junk[:].rearrange("p (j d) -> p j d", d=128), in_=peer_keys.rearrange("j k d -> k j d")),
              writes=[junk])
        for n in range(4):
            p = next_pq()
            for jj in range(4):
                j = n * 4 + jj
                c.op("pe", lambda e: e.transpose(out=p[:, jj * 128:(jj + 1) * 128], in_=junk[:, j * 128:(j + 1) * 128], identity=idf[:]),
                     reads=[junk, idf], writes=[p])
            c.op("dve", lambda e: e.tensor_copy(out=keysT[:, n * 4:(n + 1) * 4, :].rearrange("p a b -> p (a b)"), in_=p[:, :]),
                 reads=[p], writes=[keysT])


        oacc = View(pT1f[:, 0:260].rearrange("p (r d) -> p r d", d=65), pT1f)
        iacc = View(pc_full[1][:, :].rearrange("p (r s) -> p r s", s=128), pc_full[1])
        astate = {"e": 0}

        def zero_bank(bank_ap, res):
            c.op("pe", lambda e: e.matmul(bank_ap, lhsT=zer[:, 0:128], rhs=zer[:, 0:512], start=True, stop=False),
                 reads=[zer], writes=[res])

        def attn_unit(P, kT_ap, kres, qrhs, v_ap, vres, mask_ap, mres, mask2_ap, m2res, last, want_imp=None, NK=128, bias=None):
            ps = next_pq()
            W = 4 * P
            c.op("pe", lambda e: e.matmul(ps[0:NK, 0:W], lhsT=kT_ap, rhs=qrhs, start=True, stop=(bias is None)), reads=[kres, qTp], writes=[ps])
            if bias is not None:
                bl, br_, bres = bias
                c.op("pe", lambda e: e.matmul(ps[0:NK, 0:W], lhsT=bl, rhs=br_, start=False, stop=True), reads=[e32_t, bres], writes=[ps])
            eb = ebuf[astate["e"] % 2]
            pb = pTb[astate["e"] % 2]
            astate["e"] += 1
            ebf = eb[:].rearrange("p r q -> p (r q)")
            pbf = pb[:].rearrange("p r q -> p (r q)")
            masks = [(m, r_) for m, r_ in ((mask_ap, mres), (mask2_ap, m2res)) if m is not None]
            if not masks:
                c.op("act", lambda e: e.activation(out=pbf[0:NK, 0:W], in_=ps[0:NK, 0:W], func=AF.Exp, scale=0.125), reads=[ps], writes=[pb])
            else:
                c.op("act", lambda e: e.activation(out=ebf[0:NK, 0:W], in_=ps[0:NK, 0:W], func=AF.Exp, scale=0.125), reads=[ps], writes=[eb])
                src, sres = ebf, eb
                for m, r_ in masks:
                    c.op("dve", lambda e: e.tensor_tensor(out=pbf[0:NK, 0:W].rearrange("p (r q) -> p r q", q=P),
                                                          in0=src[0:NK, 0:W].rearrange("p (r q) -> p r q", q=P),
                                                          in1=m.unsqueeze(1).broadcast_to([NK, 4, P]), op=ALU.mult),
                         reads=[sres, r_], writes=[pb])
                    src, sres = pbf, pb
            def stage2():
                for r in range(4):
                    c.op("pe", lambda e: e.matmul(oacc[0:P, r, :], lhsT=pbf[0:NK, r * P:(r + 1) * P], rhs=v_ap, start=False, stop=last),
                         reads=[pb, vres], writes=[oacc])
                    if want_imp is not None:
                        c.op("pe", lambda e: e.matmul(iacc[0:P, r, :], lhsT=pbf[0:NK, r * P:(r + 1) * P], rhs=want_imp, start=False, stop=last),
                             reads=[pb, ov_sb], writes=[iacc])
            prev = astate.get("pending")
            astate["pending"] = stage2
            if prev is not None:
                prev()

        def flush_units():
            prev = astate.get("pending")
            astate["pending"] = None
            if prev is not None:
                prev()

        def finish_branch(P, g, br, first_branch, gsrc=None):
            flush_units()
            c.op("dve", lambda e: e.tensor_scalar(out=rs4[0:P, 0:4], in0=oacc[0:P, :, 64], scalar1=1e-30, scalar2=None, op0=ALU.max),
                 reads=[oacc], writes=[rs4])
            c.op("dve", lambda e: e.reciprocal(out=rs4[0:P, 0:4], in_=rs4[0:P, 0:4]), reads=[rs4], writes=[rs4])
            gsrc = gates_t if gsrc is None else gsrc
            g3 = gsrc[0:P, :].rearrange("p (h b) -> p h b", b=3)
            c.op("dve", lambda e: e.tensor_tensor(out=rs4[0:P, 4:8], in0=rs4[0:P, 0:4], in1=g3[:, 4 * g:4 * g + 4, br], op=ALU.mult),
                 reads=[rs4, gsrc], writes=[rs4])
            for r in range(4):
                h = 4 * g + r
                if first_branch:
                    c.op("dve", lambda e: e.tensor_scalar(out=attn_f[0:P, h, :], in0=oacc[0:P, r, 0:64], scalar1=rs4[0:P, 4 + r:5 + r],
                                                          scalar2=None, op0=ALU.mult), reads=[oacc, rs4], writes=[attn_f])
                else:
                    c.op("dve", lambda e: e.scalar_tensor_tensor(out=attn_f[0:P, h, :], in0=oacc[0:P, r, 0:64], scalar=rs4[0:P, 4 + r:5 + r],
                                                                  in1=attn_f[0:P, h, :], op0=ALU.mult, op1=ALU.add),
                         reads=[oacc, rs4, attn_f], writes=[attn_f])

        def attention_prompt(i):
            P = 128
            c.dma("sp", lambda e: e.dma_start(out=fmask_t[:], in_=fmask[i]), writes=[fmask_t])
            c.dma("pool", lambda e: e.dma_start(out=cmask_t[:], in_=cmask[i]), writes=[cmask_t])
            c.dma("pool", lambda e: e.dma_start(out=dmask_t[:], in_=dmask[i]), writes=[dmask_t])
            c.dma("pool", lambda e: e.dma_start(out=wmask_t[:], in_=wmask[i]), writes=[wmask_t])
            for pb_ in range(8):
                c.op("pe", lambda e: e.transpose(out=pT[0][:, pb_, :], in_=q_r[:, pb_ * 128:(pb_ + 1) * 128], identity=idb[:]),
                     reads=[q_r, idb], writes=[pT[0]])
            c.op("act", lambda e: e.copy(out=qTp[:], in_=pT[0][:]), reads=[pT[0]], writes=[qTp])
            njc = min(4, (64 * i + 62) // 128 + 1)
            nkc = 8 * i + 8
            for g in range(4):
                p0, gp = 64 * (g % 2), g // 2
                qrhs = qTp[:].rearrange("p a b -> p (a b)")[p0:p0 + 64, gp * 512:(gp + 1) * 512]
                zero_bank(pT1f[:, :], pT1f)
                zero_bank(pc_full[1][:, :], pc_full[1])
                for jc in range(njc):
                    attn_unit(P, kcT[p0:p0 + 64, gp, jc * 128:(jc + 1) * 128], kcT, qrhs, vc1[:, jc, g, :], vc1,
                              cmask_t[:, jc, :], cmask_t, None, None, jc == njc - 1, want_imp=ov_sb[:, jc, :])
                finish_branch(P, g, 0, True)
                for r in range(4):
                    if r == 0:
                        c.op("dve", lambda e: e.tensor_scalar(out=imp_t[:], in0=iacc[:, 0, :], scalar1=rs4[:, 0:1], scalar2=None, op0=ALU.mult),
                             reads=[iacc, rs4], writes=[imp_t])
                    else:
                        c.op("dve", lambda e: e.scalar_tensor_tensor(out=imp_t[:], in0=iacc[:, r, :], scalar=rs4[:, r:r + 1], in1=imp_t[:],
                                                                      op0=ALU.mult, op1=ALU.add), reads=[iacc, rs4, imp_t], writes=[imp_t])
                c.op("dve", lambda e: e.tensor_tensor(out=sc_t[:], in0=imp_t[:], in1=fmask_t[:], op=ALU.add), reads=[imp_t, fmask_t], writes=[sc_t])
                c.op("dve", lambda e: e.max(out=m16[:, 0:8], in_=sc_t[:]), reads=[sc_t], writes=[m16])
                c.op("dve", lambda e: e.match_replace(out=sc2_t[:], in_to_replace=m16[:, 0:8], in_values=sc_t[:], imm_value=-3.0e38),
                     reads=[sc_t, m16], writes=[sc2_t])
                c.op("dve", lambda e: e.max(out=m16[:, 8:16], in_=sc2_t[:]), reads=[sc2_t], writes=[m16])
                c.op("dve", lambda e: e.tensor_scalar(out=m16[:, 0:1], in0=m16[:, 15:16], scalar1=-1.0e29, scalar2=None, op0=ALU.max),
                     reads=[m16], writes=[m16])
                c.op("dve", lambda e: e.tensor_scalar(out=sel_b[:], in0=sc_t[:], scalar1=m16[:, 0:1], scalar2=None, op0=ALU.is_ge),
                     reads=[sc_t, m16], writes=[sel_b])
                for sl in range(4):
                    c.op("pe", lambda e: e.transpose(out=pT[0][0:32, sl, :], in_=sel_b[:, sl * 32:(sl + 1) * 32], identity=idb[:]),
                         reads=[sel_b, idb], writes=[pT[0]])
                c.op("act", lambda e: e.copy(out=selT[:], in_=pT[0][0:32, 0:4, :]), reads=[pT[0]], writes=[selT])
                for r_ in range(4):
                    c.op("dve", lambda e: e.tensor_scalar(out=negT[:, :, r_, :], in0=selT[:, :, :], scalar1=-1.0, scalar2=30000.0,
                                                          op0=ALU.add, op1=ALU.mult), reads=[selT], writes=[negT])
                zero_bank(pT1f[:, :], pT1f)
                for G8 in range(nkc // 8):
                    kb, vb_ = kbuf[G8 % 2], vbuf[G8 % 2]
                    c.dma("sp", lambda e: e.dma_start(out=kb[p0:p0 + 64, 0:1024], in_=kTs_d[p0:p0 + 64, gp, G8 * 1024:(G8 + 1) * 1024]),
                          reads=[kv_res], writes=[kb])
                    c.dma("sp", lambda e: e.dma_start(out=vb_[:, 0:8, :], in_=vs_d[g, :, G8 * 8:(G8 + 1) * 8, :]), reads=[kv_res], writes=[vb_])
                    for k8 in range(8):
                        kc = G8 * 8 + k8
                        diag = kc >= 8 * i
                        attn_unit(P, kb[p0:p0 + 64, k8 * 128:(k8 + 1) * 128], kb, qrhs, vb_[:, k8, :], vb_,
                                  None, None, dmask_t[:, kc - 8 * i, :] if diag else None, dmask_t if diag else None, kc == nkc - 1,
                                  bias=(e32_t[:, kc % 16, :], negT[:, kc // 16, :, :].rearrange("p r q -> p (r q)"), negT))
                finish_branch(P, g, 1, False)
                zero_bank(pT1f[:, :], pT1f)
                kc0 = max(0, 8 * i - 4)
                nw = 8 * i + 8 - kc0
                kb, vb_ = kbuf[0], vbuf[0]
                c.dma("sp", lambda e: e.dma_start(out=kb[p0:p0 + 64, 0:nw * 128], in_=kTw_d[p0:p0 + 64, gp, kc0 * 128:(kc0 + nw) * 128]),
                      reads=[kv_res], writes=[kb])
                c.dma("sp", lambda e: e.dma_start(out=vb_[:, 0:nw, :], in_=vw_d[g, :, kc0:kc0 + nw, :]), reads=[kv_res], writes=[vb_])
                for w_ in range(nw):
                    kc = kc0 + w_
                    wi = kc - (8 * i - 4)
                    attn_unit(P, kb[p0:p0 + 64, w_ * 128:(w_ + 1) * 128], kb, qrhs, vb_[:, w_, :], vb_,
                              wmask_t[:, wi, :], wmask_t, None, None, w_ == nw - 1)
                finish_branch(P, g, 2, False)
            c.op("act", lambda e: e.copy(out=attn_b[:], in_=attn_f[:].rearrange("p h d -> p (h d)")), reads=[attn_f], writes=[attn_b])
            for k in range(8):
                c.op("pe", lambda e: e.transpose(out=pT[0][:, k, :], in_=attn_b[:, k * 128:(k + 1) * 128], identity=idb[:]),
                     reads=[attn_b, idb], writes=[pT[0]])
            c.op("dve", lambda e: e.tensor_copy(out=catT[:, 8:16, :], in_=pT[0][:]), reads=[pT[0]], writes=[catT])

        def attention_sample():
            P = 4
            for pb_ in range(8):
                c.op("pe", lambda e: e.transpose(out=pT[0][:, pb_, 0:16], in_=q_r[0:16, pb_ * 128:(pb_ + 1) * 128], identity=idb[0:16, 0:16]),
                     reads=[q_r, idb], writes=[pT[0]])
            c.op("dve", lambda e: e.tensor_copy(out=qTp[:, :, 0:16], in_=pT[0][:, :, 0:16]), reads=[pT[0]], writes=[qTp])
            c.op("dve", lambda e: e.tensor_copy(out=kn_b[:, 0:256], in_=rows_t[0:16, 512:768]), reads=[rows_t], writes=[kn_b])
            c.op("dve", lambda e: e.tensor_copy(out=kn_b[:, 256:512], in_=win_t[0:16, 0:256]), reads=[win_t], writes=[kn_b])
            c.op("pool", lambda e: e.memset(vn[:], 1.0), writes=[vn])
            c.op("dve", lambda e: e.tensor_copy(out=vn[:, 0, :, 0:64], in_=rows_t[0:16, 768:1024].rearrange("p (g d) -> p g d", d=64)),
                 reads=[rows_t], writes=[vn])
            c.op("dve", lambda e: e.tensor_copy(out=vn[:, 1, :, 0:64], in_=win_t[0:16, 256:512].rearrange("p (g d) -> p g d", d=64)),
                 reads=[win_t], writes=[vn])
            for blk in range(4):
                c.op("pe", lambda e: e.transpose(out=pT[0][:, blk, 0:16], in_=kn_b[:, blk * 128:(blk + 1) * 128], identity=idb[0:16, 0:16]),
                     reads=[kn_b, idb], writes=[pT[0]])
            c.op("dve", lambda e: e.tensor_copy(out=kTn[:], in_=pT[0][:, 0:4, 0:16]), reads=[pT[0]], writes=[kTn])
            for b in range(NS):
                c.dma("sp", lambda e: e.dma_start(out=gq[:], in_=gates_t[4 * b:4 * b + 4, :]), reads=[gates_t], writes=[gq])
                c.op("dve", lambda e: e.tensor_copy(out=qs[:], in_=qTp[:, :, 4 * b:4 * b + 4]), reads=[qTp], writes=[qs])
                c.dma("sp", lambda e: e.dma_start(out=kcT2[:], in_=kc_s[b]), reads=[kv_res], writes=[kcT2])
                c.dma("sp", lambda e: e.dma_start(out=vc2[:], in_=vc_s[b]), reads=[kv_res], writes=[vc2])
                qsf = qs[:].rearrange("p a b -> p (a b)")
                for g in range(4):
                    p0, gp = 64 * (g % 2), g // 2
                    qrhs = qsf[p0:p0 + 64, gp * 16:(gp + 1) * 16]
                    zero_bank(pT1f[:, :], pT1f)
                    zero_bank(pc_full[1][:, :], pc_full[1])
                    for jc in range(4):
                        attn_unit(P, kcT2[p0:p0 + 64, gp, jc * 128:(jc + 1) * 128], kcT2, qrhs, vc2[:, jc, g, :], vc2,
                                  cm_s[:, jc, :], cm_s, None, None, jc == 3, want_imp=ov_sb[:, jc, :])
                    finish_branch(P, g, 0, True, gsrc=gq)
                    for r in range(4):
                        if r == 0:
                            c.op("dve", lambda e: e.tensor_scalar(out=imp_t[0:P, :], in0=iacc[0:P, 0, :], scalar1=rs4[0:P, 0:1], scalar2=None, op0=ALU.mult),
                                 reads=[iacc, rs4], writes=[imp_t])
                        else:
                            c.op("dve", lambda e: e.scalar_tensor_tensor(out=imp_t[0:P, :], in0=iacc[0:P, r, :], scalar=rs4[0:P, r:r + 1], in1=imp_t[0:P, :],
                                                                          op0=ALU.mult, op1=ALU.add), reads=[iacc, rs4, imp_t], writes=[imp_t])
                    c.op("dve", lambda e: e.tensor_tensor(out=sc_t[0:P, :], in0=imp_t[0:P, :], in1=fm_s[0:P, :], op=ALU.add), reads=[imp_t, fm_s], writes=[sc_t])
                    c.op("dve", lambda e: e.max(out=m16[0:P, 0:8], in_=sc_t[0:P, :]), reads=[sc_t], writes=[m16])
                    c.op("dve", lambda e: e.match_replace(out=sc2_t[0:P, :], in_to_replace=m16[0:P, 0:8], in_values=sc_t[0:P, :], imm_value=-3.0e38),
                         reads=[sc_t, m16], writes=[sc2_t])
                    c.op("dve", lambda e: e.max(out=m16[0:P, 8:16], in_=sc2_t[0:P, :]), reads=[sc2_t], writes=[m16])
                    c.op("dve", lambda e: e.tensor_scalar(out=sel_b[0:P, :], in0=sc_t[0:P, :], scalar1=m16[0:P, 14:15], scalar2=None, op0=ALU.is_ge),
                         reads=[sc_t, m16], writes=[sel_b])
                    for sl in range(4):
                        c.op("pe", lambda e: e.transpose(out=pT[0][0:32, sl, 0:P], in_=sel_b[0:P, sl * 32:(sl + 1) * 32], identity=idb[0:P, 0:P]),
                             reads=[sel_b, idb], writes=[pT[0]])
                    c.op("dve", lambda e: e.tensor_copy(out=selT[:, :, 0:P], in_=pT[0][0:32, 0:4, 0:P]), reads=[pT[0]], writes=[selT])
                    negS = negT[:].rearrange("p a r q -> p (a r q)")[:, 0:64].rearrange("p (a r q) -> p a r q", a=4, r=4)
                    for r_ in range(4):
                        c.op("dve", lambda e: e.tensor_scalar(out=negS[:, :, r_, :], in0=selT[:, :, 0:P], scalar1=-1.0, scalar2=30000.0,
                                                              op0=ALU.add, op1=ALU.mult), reads=[selT], writes=[negT])
                    zero_bank(pT1f[:, :], pT1f)
                    for G8 in range(8):
                        kb, vb_ = kbuf[G8 % 2], vbuf[G8 % 2]
                        c.dma("sp", lambda e: e.dma_start(out=kb[p0:p0 + 64, 0:1024], in_=kTs_s[b, p0:p0 + 64, gp, G8 * 1024:(G8 + 1) * 1024]),
                              reads=[kv_res], writes=[kb])
                        c.dma("sp", lambda e: e.dma_start(out=vb_[:, 0:8, :], in_=vs_s[b, g, :, G8 * 8:(G8 + 1) * 8, :]), reads=[kv_res], writes=[vb_])
                        for k8 in range(8):
                            kc = G8 * 8 + k8
                            attn_unit(P, kb[p0:p0 + 64, k8 * 128:(k8 + 1) * 128], kb, qrhs, vb_[:, k8, :], vb_, None, None, None, None, False,
                                      bias=(e32_t[:, kc % 16, :], negS[:, kc // 16, :, :].rearrange("p r q -> p (r q)"), negT))
                    attn_unit(P, kTn[p0:p0 + 64, gp, :], kTn, qrhs, vn[:, 0, g, :], vn, nm_s[:, b, :], nm_s, None, None, True, NK=16)
                    finish_branch(P, g, 1, False, gsrc=gq)
                    zero_bank(pT1f[:, :], pT1f)
                    kb, vb_ = kbuf[0], vbuf[0]
                    c.dma("sp", lambda e: e.dma_start(out=kb[p0:p0 + 64, 0:512], in_=kTw_s[b, p0:p0 + 64, gp, :]), reads=[kv_res], writes=[kb])
                    c.dma("sp", lambda e: e.dma_start(out=vb_[:, 0:4, :], in_=vw_s[b, g]), reads=[kv_res], writes=[vb_])
                    for wc in range(4):
                        attn_unit(P, kb[p0:p0 + 64, wc * 128:(wc + 1) * 128], kb, qrhs, vb_[:, wc, :], vb_, wm_s[:, wc, :], wm_s, None, None, False)
                    attn_unit(P, kTn[p0:p0 + 64, 2 + gp, :], kTn, qrhs, vn[:, 1, g, :], vn, nm_s[:, b, :], nm_s, None, None, True, NK=16)
                    finish_branch(P, g, 2, False, gsrc=gq)
                c.op("act", lambda e: e.copy(out=attn_b[0:P, :], in_=attn_f[0:P, :, :].rearrange("p h d -> p (h d)")), reads=[attn_f], writes=[attn_b])
                for k in range(8):
                    c.op("pe", lambda e: e.transpose(out=pT[0][:, k, 0:P], in_=attn_b[0:P, k * 128:(k + 1) * 128], identity=idb[0:P, 0:P]),
                         reads=[attn_b, idb], writes=[pT[0]])
                c.op("dve", lambda e: e.tensor_copy(out=catT[:, 8:16, 4 * b:4 * b + 4], in_=pT[0][:, :, 0:P]), reads=[pT[0]], writes=[catT])

        gen_mod(modA, 1, 16, 128, "scale", gain=g1)
        gen_mod(modB, 0, 16, 128, "plain")
        gen_mod(modG, 2, 16, 128, "plain")
        xin = xt[0]
        c.dma("sp", lambda e: e.dma_start(out=xin[0:16, :], in_=xs), writes=[xin])
        c.dma("sp", lambda e: e.dma_start(out=cs_t[0:16, :], in_=css), writes=[cs_t])
        c.dma("sp", lambda e: e.dma_start(out=scv_t[:], in_=scv), writes=[scv_t])
        c.dma("sp", lambda e: e.dma_start(out=win_s[:, 0:508, :], in_=swin[:, 4:512, :]), is_output=True)
        pa = next_pq()
        for j in range(8):
            c.op("pe", lambda e: e.transpose(out=pa[:, j * 8:(j + 1) * 8], in_=scv_t[:, j * 128:(j + 1) * 128], identity=idf[0:8, 0:8]),
                 reads=[scv_t, idf], writes=[pa])
        c.op("dve", lambda e: e.tensor_copy(out=sctx[:, :, :, 0:2], in_=pa[:, 0:64].rearrange("p (c s t) -> p c s t", c=8, s=NS)),
             reads=[pa], writes=[sctx])
        norm_mod(16, xin, hb, st1, modA, modB)
        transpose_h(16, hb, 0)
        proc_tile(16, 16, xin, True, 0)

        for i in range(NT):
            gen_mod(modA, 1, 128, 0, "scale", gain=g1)
            gen_mod(modB, 0, 128, 0, "plain")
            gen_mod(modG, 2, 128, 0, "plain")
            xin = xt[i % 2]
            c.dma("sp", lambda e: e.dma_start(out=xin[:], in_=xo[i]), writes=[xin])
            c.dma("sp", lambda e: e.dma_start(out=xp2[0:2, :], in_=xpv[i]), writes=[xp2])
            c.dma("sp", lambda e: e.dma_start(out=cs_t[:], in_=cso[i]), writes=[cs_t])
            norm_mod(2, xp2, hb2, st2, modA, modB)
            transpose_h(2, hb2, 0)
            norm_mod(128, xin, hb, st1, modA, modB)
            transpose_h(128, hb, 2)
            proc_tile(128, 130, xin, False, i)

        c.finish()
        print("instructions (approx):", c.ninst)
    return nc


def _tiles_of(core):
    return [core, 15 - core, 16 + core, 31 - core, 32 + core, 47 - core, 48 + core, 63 - core]


def _rope_table(pos):
    half = 32
    inv = (10000.0 ** (-np.arange(half, dtype=np.float32) / half)).astype(np.float32)
    ang = pos.astype(np.float32)[:, None] * inv[None, :]
    cos = np.cos(ang).astype(np.float32)
    sin = np.sin(ang).astype(np.float32)
    return np.concatenate([cos, cos, -sin, sin], axis=1).astype(np.float32)


_NC_CACHE = {}


def kernel(x_prompt, x_sample, c_prompt, c_sample, cache_kv, state_win, state_conv, page_table,
           w_ada, b_ada, norm1_g, norm2_g, w_in, conv_w, conv_b, cmp_pe, cmp_w1, cmp_b1, cmp_w2, cmp_b2,
           w_out, peer_wq, peer_keys, peer_u, peer_v, final_g):
    f32 = np.float32
    x_prompt = np.asarray(x_prompt, f32)
    x_sample = np.asarray(x_sample, f32)
    xp_t = x_prompt[0].reshape(64, 128, D)
    idn = np.eye(128, dtype=f32)
    css = _rope_table(SEQ + (np.arange(16) % 4))
    csa = np.stack([_rope_table(128 * t + np.arange(128)) for t in range(64)]).astype(f32)
    cscmp = np.stack([_rope_table(16 * (128 * H + np.arange(128)) + 31) for H in range(4)]).astype(f32)
    jj = np.arange(512)[:, None]
    sb_ = np.arange(128)[None, :]
    ov = ((16 * jj < 64 * (sb_ + 1)) & (16 * jj + 32 > 64 * sb_) & (jj <= 510)).astype(f32)
    ovm = np.ascontiguousarray(ov.reshape(4, 128, 128).transpose(1, 0, 2))
    piota = np.arange(128, dtype=f32).reshape(128, 1)
    cmask_sm = np.ones((128, 4, 4), f32)
    cmask_sm[127, 3, :] = 0.0
    fmask_sm = np.zeros((4, 128), f32)
    fmask_sm[:, 0] = 1.0e4
    fmask_sm[:, 127] = 1.0e4
    wmask_sm = np.ones((128, 4, 4), f32)
    for tq in range(4):
        wmask_sm[0:tq + 1, 0, tq] = 0.0
    nmask = np.zeros((16, NS, 4), f32)
    for b_ in range(NS):
        for t_ in range(4):
            for tq in range(4):
                if t_ <= tq:
                    nmask[4 * b_ + t_, b_, tq] = 1.0
    e32 = np.zeros((32, 16, 128), f32)
    for c_ in range(16):
        for k_ in range(128):
            e32[2 * c_ + k_ // 64, c_, k_] = 1.0
    common = {
        "piota": piota, "cmask_sm": cmask_sm, "fmask_sm": fmask_sm, "wmask_sm": wmask_sm, "nmask": nmask,
        "cache": np.ascontiguousarray(np.asarray(cache_kv, f32)[0].reshape(2560 * 128, 1024)),
        "idn": idn, "css": css, "xp": np.ascontiguousarray(xp_t), "csa": csa, "cscmp": cscmp, "ovm": ovm, "e32": e32,
        "cmp_pe": np.asarray(cmp_pe[0], f32), "cmp_w1": np.asarray(cmp_w1[0], f32), "cmp_b1": np.asarray(cmp_b1[0], f32),
        "cmp_w2": np.asarray(cmp_w2[0], f32), "cmp_b2": np.asarray(cmp_b2[0], f32),
        "w_ada": np.asarray(w_ada[0], f32), "b_ada": np.asarray(b_ada, f32).reshape(1, -1),
        "g1": np.asarray(norm1_g, f32).reshape(1, D), "g2": np.asarray(norm2_g, f32).reshape(1, D),
        "fg": np.asarray(final_g, f32).reshape(1, D),
        "w_in": np.asarray(w_in[0], f32), "conv_w": np.asarray(conv_w[0], f32), "conv_b": np.asarray(conv_b, f32).reshape(1, DC),
        "w_out": np.asarray(w_out[0], f32),
        "peer_wq": np.asarray(peer_wq[0], f32), "peer_keys": np.ascontiguousarray(np.asarray(peer_keys[0], f32).reshape(16, 128, 128)),
        "peer_u": np.asarray(peer_u[0], f32), "peer_v": np.asarray(peer_v[0], f32),
    }
    in_maps = []
    for core in range(NCORES):
        tl = _tiles_of(core)
        xo = np.ascontiguousarray(xp_t[tl])
        xpv = np.zeros((NT, 2, D), f32)
        pfl = np.ones((128, NT), f32)
        cso = np.zeros((NT, 128, 128), f32)
        for i, t in enumerate(tl):
            if t > 0:
                xpv[i] = x_prompt[0, 128 * t - 2:128 * t]
            else:
                pfl[:, i] = 0.0
            cso[i] = _rope_table(128 * t + np.arange(128))
        fmask = np.zeros((NT, 128, 128), f32)
        cmask = np.zeros((NT, 128, 4, 128), f32)
        dmask = np.zeros((NT, 128, 8, 128), f32)
        wmask = np.zeros((NT, 128, 12, 128), f32)
        qa = np.arange(128)
        for i, t in enumerate(tl):
            qpos = 128 * t + qa
            cur = qpos // 64
            blk = np.arange(128)[None, :]
            forced = (blk == 0) | (blk == cur[:, None]) | (blk == cur[:, None] - 1)
            valid = blk <= cur[:, None]
            fmask[i] = np.where(valid, np.where(forced, 1.0e4, 0.0), -1.0e30)
            for jc in range(4):
                j = 128 * jc + np.arange(128)
                cmask[i, :, jc, :] = ((16 * j[:, None] + 31 <= qpos[None, :]) & (j[:, None] <= 510))
            for d_ in range(8):
                kc = 8 * i + d_
                kpos = 128 * kc + np.arange(128)
                dmask[i, :, d_, :] = (kpos[:, None] <= qpos[None, :])
            for w_ in range(12):
                kc = 8 * i - 4 + w_
                if kc < 0:
                    continue
                kpos = 128 * kc + np.arange(128)
                dist = qpos[None, :] - kpos[:, None]
                wmask[i, :, w_, :] = ((dist >= 0) & (dist < 512))
        sq = slice(NS * core, NS * core + NS)
        m = dict(common)
        m.update({
            "xo": xo, "xpv": xpv, "pfl": pfl, "cso": cso, "fmask": fmask, "cmask": cmask, "dmask": dmask, "wmask": wmask,
            "xs": np.ascontiguousarray(x_sample[sq].reshape(16, D)),
            "ptab": np.ascontiguousarray(np.asarray(page_table)[sq].astype(np.int32)),
            "cc": np.ascontiguousarray(np.concatenate([np.asarray(c_prompt, f32), np.asarray(c_sample, f32)[sq]], axis=0)),
            "scv": np.ascontiguousarray(np.asarray(state_conv, f32)[0, sq].reshape(8, DC)),
            "swin": np.ascontiguousarray(np.asarray(state_win, f32)[0, sq].reshape(NS, 512, 512)),
        })
        in_maps.append(m)

    if "nc" not in _NC_CACHE:
        _NC_CACHE["nc"] = build_program()
    nc = _NC_CACHE["nc"]
    shp = getattr(nc, "_din_shapes", None)
    if shp is not None:
        in_maps = [{k: (m_[k] if tuple(m_[k].shape) == shp[k] else np.zeros(shp[k], f32)) for k in shp} for m_ in in_maps]
    res = run_bass_kernel_spmd(nc, in_maps, core_ids=list(range(NCORES)))
    R = res.results

    y_prompt = np.zeros((1, SEQ, D), f32)
    kv_rows_prompt = np.zeros((1, 1, SEQ, 4, 4, 64), f32)
    win_prompt = np.zeros((1, 1, 512, 2, 4, 64), f32)
    conv_prompt = np.zeros((1, 1, 2, DC), f32)
    y_sample = np.zeros((32, 4, D), f32)
    kv_rows_sample = np.zeros((1, 32, 4, 4, 4, 64), f32)
    win_sample = np.zeros((1, 32, 512, 2, 4, 64), f32)
    conv_sample = np.zeros((1, 32, 2, DC), f32)
    for core in range(NCORES):
        r = R[core]
        tl = _tiles_of(core)
        for i, t in enumerate(tl):
            y_prompt[0, 128 * t:128 * (t + 1)] = r["yo"][i]
            kv_rows_prompt[0, 0, 128 * t:128 * (t + 1)] = r["rows_o"][i].reshape(128, 4, 4, 64)
            if t >= 60:
                win_prompt[0, 0, 128 * (t - 60):128 * (t - 59)] = r["win_o"][i].reshape(128, 2, 4, 64)
            if t == 63:
                conv_prompt[0, 0] = r["conv_o"][i]
        sq = slice(NS * core, NS * core + NS)
        y_sample[sq] = r["ys"].reshape(NS, 4, D)
        kv_rows_sample[0, sq] = r["rows_s"].reshape(NS, 4, 4, 4, 64)
        win_sample[0, sq] = r["win_s"].reshape(NS, 512, 2, 4, 64)
        conv_sample[0, sq] = r["conv_s"]
    return (y_prompt, y_sample, kv_rows_prompt, kv_rows_sample, win_prompt, win_sample, conv_prompt, conv_sample)
```

```python
import contextlib
import numpy as np
import concourse.bass as bass
import concourse.mybir as mybir
from concourse.alu_op_type import AluOpType as ALU
from concourse.bass_utils import run_bass_kernel_spmd

AF = mybir.ActivationFunctionType
AX = mybir.AxisListType
F32 = mybir.dt.float32
BF16 = mybir.dt.bfloat16
I32 = mybir.dt.int32
U32 = mybir.dt.uint32

NCORES = 8
D = 2048
DC = 1024
NT = 8
SEQ = 8192
NS = 4
ST = 4
EPS = 1e-6
IN_COLS = 5680


class Res:
    __slots__ = ("w", "rs", "name")

    def __init__(self, name=""):
        self.w = None
        self.rs = []
        self.name = name


class T:
    def __init__(self, t, name=""):
        self.t = t
        self.r = Res(name)

    def __getitem__(self, k):
        return self.t[k]


class View:
    def __init__(self, ap, r):
        self.v = ap
        self.r = r.r if hasattr(r, "r") else r

    def __getitem__(self, k):
        return self.v[k]


class Ctx:
    NDMA = 8

    def __init__(self, nc, es):
        self.nc = nc
        self.es = es
        self.eng = {"pe": nc.tensor, "dve": nc.vector, "act": nc.scalar, "pool": nc.gpsimd, "sp": nc.sync}
        self.sem = {}
        self.cnt = {}
        for k in self.eng:
            self.sem[k] = es.enter_context(nc.semaphore("s_" + k))
            self.cnt[k] = 0
        self.dsem = {}
        self.dcnt = {}
        for q in ("sp", "pool", "act"):
            self.dsem[q] = [es.enter_context(nc.semaphore("d_%s%d" % (q, i))) for i in range(self.NDMA)]
            self.dcnt[q] = 0
        self.seen = {k: {} for k in self.eng}
        self.out_tokens = []
        self.ninst = 0

    def sb(self, name, shape, dt=F32):
        return T(self.es.enter_context(self.nc.sbuf_tensor(name, list(shape), dt)), name)

    def ps(self, name, shape, dt=F32):
        return T(self.es.enter_context(self.nc.psum_tensor(name, list(shape), dt)), name)

    def _wait(self, e, tok):
        if tok is None:
            return
        sem, val = tok
        key = id(sem)
        if self.seen[e].get(key, 0) >= val:
            return
        self.seen[e][key] = val
        self.eng[e].wait_ge(sem, val)
        self.ninst += 1

    @staticmethod
    def _res(x):
        return x.r if hasattr(x, "r") else x

    def _deps(self, e, reads, writes):
        own = self.sem[e]
        toks = []
        for r in reads:
            r = self._res(r)
            if r.w is not None:
                toks.append(r.w)
        for w in writes:
            w = self._res(w)
            if w.w is not None:
                toks.append(w.w)
            for t in w.rs:
                if t[0] is own:
                    continue
                toks.append(t)
        if e == "pe":
            toks = [t for t in toks if t[0] is not own]
        for t in toks:
            self._wait(e, t)

    def _commit(self, tok, reads, writes):
        for r in reads:
            r = self._res(r)
            r.rs.append(tok)
            if len(r.rs) > 96:
                r.rs = r.rs[-96:]
        for w in writes:
            w = self._res(w)
            w.w = tok
            w.rs = []

    def op(self, e, fn, reads=(), writes=()):
        self._deps(e, reads, writes)
        inst = fn(self.eng[e])
        self.cnt[e] += 1
        inst.then_inc(self.sem[e], 1)
        tok = (self.sem[e], self.cnt[e])
        self._commit(tok, reads, writes)
        self.ninst += 1
        return tok

    def dma(self, q, fn, reads=(), writes=(), is_output=False):
        i = self.dcnt[q]
        self.dcnt[q] += 1
        sem = self.dsem[q][i % self.NDMA]
        rnd = i // self.NDMA
        if rnd > 0:
            self._wait(q, (sem, 16 * rnd))
        self._deps(q, reads, writes)
        inst = fn(self.eng[q])
        inst.then_inc(sem, 16)
        tok = (sem, 16 * (rnd + 1))
        self._commit(tok, reads, writes)
        if is_output:
            self.out_tokens.append(tok)
        self.ninst += 1
        return tok

    def barrier(self):
        toks = [(self.sem[e], self.cnt[e]) for e in self.eng if self.cnt[e] > 0]
        for q in self.dsem:
            n = self.dcnt[q]
            for k in range(min(n, self.NDMA)):
                cntk = (n - k + self.NDMA - 1) // self.NDMA
                toks.append((self.dsem[q][k], 16 * cntk))
        for e in self.eng:
            for t in toks:
                self._wait(e, t)

    def finish(self):
        for tok in self.out_tokens:
            self._wait("sp", tok)
        for q in self.dsem:
            n = self.dcnt[q]
            for k in range(min(n, self.NDMA)):
                cntk = (n - k + self.NDMA - 1) // self.NDMA
                self._wait("sp", (self.dsem[q][k], 16 * cntk))
        for e in self.eng:
            if e != "sp" and self.cnt[e] > 0:
                self._wait("sp", (self.sem[e], self.cnt[e]))


def build_program():
    nc = bass.Bass("TRN2", target_bir_lowering=False)

    import os as _osd
    _DBG = _osd.environ.get("KDBG", "")
    _SKIP = set(_osd.environ.get("KSKIP", "").split(","))
    din_shapes = {}

    def din(name, shape, dt=F32):
        if _DBG and name in ("peer_u", "peer_v", "w_out", "peer_wq", "w_ada", "xo"):
            shape = [2, 2] if len(shape) == 2 else [2, 2, 2]
        din_shapes[name] = tuple(shape)
        return nc.dram_tensor(name, list(shape), dt, kind="ExternalInput").ap()

    def dout(name, shape, dt=F32):
        return nc.dram_tensor(name, list(shape), dt, kind="ExternalOutput").ap()

    xo = din("xo", [NT, 128, D])
    xpv = din("xpv", [NT, 2, D])
    pfl = din("pfl", [128, NT])
    cso = din("cso", [NT, 128, 128])
    css = din("css", [16, 128])
    xs = din("xs", [16, D])
    cc = din("cc", [5, D])
    idn = din("idn", [128, 128])
    scv = din("scv", [8, DC])
    swin = din("swin", [NS, 512, 512])
    w_ada = din("w_ada", [D, 6 * D])
    b_ada = din("b_ada", [1, 6 * D])
    g1 = din("g1", [1, D])
    g2 = din("g2", [1, D])
    fg = din("fg", [1, D])
    w_in = din("w_in", [D, IN_COLS])
    conv_w = din("conv_w", [3, DC])
    conv_b = din("conv_b", [1, DC])
    w_out = din("w_out", [D, D])
    peer_wq = din("peer_wq", [D, D])
    peer_keys = din("peer_keys", [16, 128, 128])
    peer_u = din("peer_u", [16384, D])
    peer_v = din("peer_v", [16384, D])
    xp = din("xp", [64, 128, D])
    csa = din("csa", [64, 128, 128])
    cscmp = din("cscmp", [4, 128, 128])
    ovm = din("ovm", [128, 4, 128])
    e32 = din("e32", [32, 16, 128])
    fmask = din("fmask", [NT, 128, 128])
    cmask = din("cmask", [NT, 128, 4, 128])
    dmask = din("dmask", [NT, 128, 8, 128])
    wmask = din("wmask", [NT, 128, 12, 128])
    cache = din("cache", [2560 * 128, 1024])
    ptab = din("ptab", [NS, 64], I32)
    piota = din("piota", [128, 1])
    cmask_sm = din("cmask_sm", [128, 4, 4])
    fmask_sm = din("fmask_sm", [4, 128])
    wmask_sm = din("wmask_sm", [128, 4, 4])
    nmask = din("nmask", [16, NS, 4])
    cmp_pe = din("cmp_pe", [2, 32, 64])
    cmp_w1 = din("cmp_w1", [2, 32, 64, 128])
    cmp_b1 = din("cmp_b1", [2, 128])
    cmp_w2 = din("cmp_w2", [2, 128, 64])
    cmp_b2 = din("cmp_b2", [2, 64])

    yo = dout("yo", [NT, 128, D])
    ys = dout("ys", [16, D])
    rows_o = dout("rows_o", [NT, 128, 1024])
    rows_s = dout("rows_s", [16, 1024])
    win_o = dout("win_o", [NT, 128, 512])
    win_s = dout("win_s", [NS, 512, 512])
    conv_o = dout("conv_o", [NT, 2, DC])
    conv_s = dout("conv_s", [NS, 2, DC])

    m_dram = nc.dram_tensor("m_dram", [5, 6 * D], F32, kind="Internal").ap()
    m_res = Res("m_dram")
    kTs_d = nc.dram_tensor("kTs_d", [128, 2, SEQ], BF16, kind="Internal").ap()
    kTw_d = nc.dram_tensor("kTw_d", [128, 2, SEQ], BF16, kind="Internal").ap()
    vs_d = nc.dram_tensor("vs_d", [4, 128, 64, 65], BF16, kind="Internal").ap()
    vw_d = nc.dram_tensor("vw_d", [4, 128, 64, 65], BF16, kind="Internal").ap()
    kv_res = Res("kv_scratch")
    kTs_s = nc.dram_tensor("kTs_s", [NS, 128, 2, SEQ], BF16, kind="Internal").ap()
    vs_s = nc.dram_tensor("vs_s", [NS, 4, 128, 64, 65], BF16, kind="Internal").ap()
    kTw_s = nc.dram_tensor("kTw_s", [NS, 128, 2, 512], BF16, kind="Internal").ap()
    vw_s = nc.dram_tensor("vw_s", [NS, 4, 128, 4, 65], BF16, kind="Internal").ap()
    kc_s = nc.dram_tensor("kc_s", [NS, 128, 2, 512], BF16, kind="Internal").ap()
    vc_s = nc.dram_tensor("vc_s", [NS, 128, 4, 4, 65], BF16, kind="Internal").ap()
    w_in_v = w_in.rearrange("(c p) n -> p c n", p=128)
    w_out_v = None if _DBG else w_out.rearrange("(c p) n -> p c n", p=128)
    w_ada_v = None if _DBG else w_ada.rearrange("(c p) n -> p c n", p=128)
    wq_v = None if _DBG else peer_wq.rearrange("(c p) n -> p c n", p=128)
    nc._din_shapes = din_shapes

    with contextlib.ExitStack() as es:
        c = Ctx(nc, es)
        idf = c.sb("idf", [128, 128])
        idb = c.sb("idb", [128, 128], BF16)
        zer = c.sb("zer", [128, 512], BF16)
        pfl_t = c.sb("pfl_t", [128, NT])
        cwT = c.sb("cwT", [128, 8, 3])
        cbT = c.sb("cbT", [128, 8])
        h2f = c.sb("h2f", [128, D])
        facc = c.sb("facc", [128, D])
        t12 = [c.sb("t12_%d" % i, [128, 16]) for i in range(2)]
        i12 = [c.sb("i12_%d" % i, [128, 16], U32) for i in range(2)]
        if12 = [c.sb("if12_%d" % i, [128, 16]) for i in range(2)]
        tmp128 = c.sb("tmp128", [128, 128])
        cand = c.sb("cand", [128, 256])
        cidx = c.sb("cidx", [128, 256])
        tmp256 = c.sb("tmp256", [128, 256])
        c16 = c.sb("c16", [128, 16])
        pst = c.sb("pst", [128, 4])
        ef = c.sb("ef", [128, 128])
        eu = c.sb("eu", [128, 128], U32)
        gw = c.sb("gw", [128, 128])
        adot = c.sb("adot", [128, 128])
        coef = c.sb("coef", [128, 128])
        modA = c.sb("modA", [128, D])
        modB = c.sb("modB", [128, D])
        modG = c.sb("modG", [128, D])
        ccT = c.sb("ccT", [128, 16, 5], BF16)
        xt1 = c.sb("xt", [128, D])
        xt = [xt1, xt1]
        junk = c.sb("junk", [128, D])
        yt = c.sb("yt", [128, D])
        hb = c.sb("hb", [128, D], BF16)
        st1 = c.sb("st1", [128, 4])
        st2 = c.sb("st2", [2, 4])
        hT = c.sb("hT", [128, 16, 130], BF16)
        cs_t = c.sb("cs_t", [128, 128])
        kcT = c.sb("kcT", [128, 2, 512], BF16)
        vc1 = c.sb("vc1", [128, 4, 4, 65], BF16)
        ov_sb = c.sb("ov_sb", [128, 4, 128], BF16)
        m_bk = [View(h2f[0:5, i * 512:(i + 1) * 512], Res("m_bk")) for i in range(2)]
        b_bk = [View(h2f[0:5, 1024 + i * 512:1024 + (i + 1) * 512], Res("b_bk")) for i in range(2)]
        pt_i = c.sb("pt_i", [128, 64], I32)
        idx_f = c.sb("idx_f", [128, 64])
        idx_u = c.sb("idx_u", [128, 64], U32)
        pio_t = c.sb("pio_t", [128, 1])
        ropa = View(h2f[:, 0:1024], h2f)
        ropb = View(h2f[:, 1024:2048], h2f)
        rows_t = View(yt[:, 0:1024], yt)
        win_t = View(yt[:, 1024:1536], yt)
        x1 = xt1
        xp2 = yt
        hb2 = hb
        cc_t = xt1
        cc_s = junk
        scv_t = View(junk[0:8, 0:DC], junk)

        pT = [c.ps("pT%d" % i, [128, 8, 128], BF16) for i in range(2)]
        pc_full = [c.ps("pc%d" % i, [128, 512]) for i in range(2)]

        class _PCV:
            def __init__(self, t):
                self.r = t.r
                self.v = t[:, 0:260].rearrange("p (a b) -> p a b", b=130)

            def __getitem__(self, k):
                return self.v[k]

        pc = [_PCV(t) for t in pc_full]
        pq = [c.ps("pq%d" % i, [128, 512]) for i in range(4)]
        pT1f = View(pT[1][:].rearrange("p a b -> p (a b)").bitcast(F32), pT[1])

        state = {"wb": 0, "pq": 0, "pc": 0, "wbufs": None}

        def next_wb():
            wl = state["wbufs"]
            b = wl[state["wb"] % 2]
            state["wb"] += 1
            return b

        def next_pq():
            b = pq[state["pq"] % 4]
            state["pq"] += 1
            return b

        def next_pc():
            b = pc[state["pc"] % 2]
            state["pc"] += 1
            return b

        def load_w(view, c0, ncol):
            b = next_wb()
            c.dma("pool", lambda e: e.dma_start(out=b[:, :, 0:ncol], in_=view[:, :, c0:c0 + ncol]), writes=[b, b.r2])
            return b

        esA = contextlib.ExitStack()
        c.es = esA
        wkv = c.sb("wkv", [128, 16, 1024], BF16)
        wkv2 = c.sb("wkv2", [128, 16, 512], BF16)
        rawT = [c.sb("rawT%d" % i, [128, 4, 2064], BF16) for i in range(2)]
        kTs_st = c.sb("kTs_st", [128, 2, 1024], BF16)
        kTw_st = c.sb("kTw_st", [128, 2, 1024], BF16)
        vs_st = c.sb("vs_st", [128, 4, 8, 65], BF16)
        vw_st = c.sb("vw_st", [128, 4, 8, 65], BF16)
        ks_b = c.sb("ks_b", [128, 256], BF16)
        w1_sb = c.sb("w1_sb", [128, 2, 32, 128], BF16)
        peT = c.sb("peT", [128, 2, 34], BF16)
        w2_sb = c.sb("w2_sb", [128, 2, 64], BF16)
        b1T = c.sb("b1T", [128, 2])
        b2bc = c.sb("b2bc", [128, 2, 64])
        biasH = c.sb("biasH", [128, 2])
        hidT = c.sb("hidT", [128, 128], BF16)
        hidT2 = c.sb("hidT2", [128, 128], BF16)
        kc_tok = c.sb("kc_tok", [128, 4, 64])
        kcr = c.sb("kcr", [128, 256], BF16)
        c.es = es

        class _WV:
            def __init__(self, t):
                self.v = t[:].rearrange("p a b -> p (a b)")[:, 0:8192].rearrange("p (k n) -> p k n", n=512)
                self.r = t.r
                self.r2 = Res("dummy")

            def __getitem__(self, k):
                return self.v[k]

        state["wbufs"] = [_WV(rawT[0]), _WV(rawT[1])]

        c.dma("sp", lambda e: e.dma_start(out=idf[:], in_=idn), writes=[idf])
        c.dma("sp", lambda e: e.dma_start(out=pfl_t[:], in_=pfl), writes=[pfl_t])
        for k_ in range(3):
            c.dma("sp", lambda e: e.dma_start(out=cwT[:, :, k_], in_=conv_w[k_].rearrange("(c p) -> p c", p=128),
                                              allow_slow_non_contiguous=True), writes=[cwT])
        c.dma("sp", lambda e: e.dma_start(out=cbT[:], in_=conv_b.rearrange("o (c p) -> p (o c)", p=128),
                                          allow_slow_non_contiguous=True), writes=[cbT])
        c.op("pool", lambda e: e.memset(zer[:], 0.0), writes=[zer])
        c.dma("sp", lambda e: e.dma_start(out=cc_t[0:5, :], in_=cc), writes=[cc_t])
        c.op("dve", lambda e: e.tensor_copy(out=idb[:], in_=idf[:]), reads=[idf], writes=[idb])

        c.op("act", lambda e: e.activation(out=cc_s[0:5, :], in_=cc_t[0:5, :], func=AF.Silu), reads=[cc_t], writes=[cc_s])
        pa = next_pq()
        for k in range(16):
            c.op("pe", lambda e: e.transpose(out=pa[:, k * 5:(k + 1) * 5], in_=cc_s[0:5, k * 128:(k + 1) * 128],
                                             identity=idf[0:5, 0:5]), reads=[cc_s, idf], writes=[pa])
        c.op("dve", lambda e: e.tensor_copy(out=ccT[:].rearrange("p a b -> p (a b)"), in_=pa[:, 0:80]),
             reads=[pa], writes=[ccT])
        for n in range(0 if _DBG else 24):
            b = load_w(w_ada_v, n * 512, 512)
            p = next_pq()
            for k in range(16):
                c.op("pe", lambda e: e.matmul(p[0:5, :], lhsT=ccT[:, k, :], rhs=b[:, k, :], start=(k == 0), stop=(k == 15)),
                     reads=[ccT, b, b.r2], writes=[p])
            bb = b_bk[n % 2]
            mb = m_bk[n % 2]
            c.dma("sp", lambda e: e.dma_start(out=bb[:, :], in_=b_ada[:, n * 512:(n + 1) * 512].partition_broadcast(5)), writes=[bb])
            c.op("dve", lambda e: e.tensor_tensor(out=mb[:, :], in0=p[0:5, :], in1=bb[:, :], op=ALU.add), reads=[p, bb], writes=[mb])
            c.dma("sp", lambda e: e.dma_start(out=m_dram[:, n * 512:(n + 1) * 512], in_=mb[:, :]), reads=[mb], writes=[m_res])

        c.barrier()
        import os as _os0
        if _os0.environ.get("KDBG", "") == "0":
            c.finish()
            esA.close()
            return nc

        def gen_mod(dst, j, P, col0, kind, gain=None):
            tgt = dst if kind == "plain" else junk
            if P == 128:
                c.dma("sp", lambda e: e.dma_start(out=tgt[:, :], in_=m_dram[0:1, j * D:(j + 1) * D].partition_broadcast(128)),
                      reads=[m_res], writes=[tgt])
            else:
                for s_ in range(NS):
                    c.dma("sp", lambda e: e.dma_start(out=tgt[4 * s_:4 * s_ + 4, :],
                                                      in_=m_dram[1 + s_:2 + s_, j * D:(j + 1) * D].partition_broadcast(4)),
                          reads=[m_res], writes=[tgt])
            if kind != "plain":
                c.dma("sp", lambda e: e.dma_start(out=facc[0:P, :], in_=gain.partition_broadcast(P)), writes=[facc])
                c.op("dve", lambda e: e.scalar_tensor_tensor(out=dst[0:P, :], in0=junk[0:P, :], scalar=1.0, in1=facc[0:P, :],
                                                              op0=ALU.add, op1=ALU.mult), reads=[junk, facc], writes=[dst])

        def norm_mod(P, xin, hout, stt, A, B):
            c.op("act", lambda e: e.activation(out=junk[0:P, :], in_=xin[0:P, :], func=AF.Square, accum_out=stt[0:P, 0:1]),
                 reads=[xin], writes=[junk, stt])
            c.op("dve", lambda e: e.tensor_scalar(out=stt[0:P, 1:2], in0=stt[0:P, 0:1], scalar1=1.0 / D, scalar2=EPS,
                                                  op0=ALU.mult, op1=ALU.add), reads=[stt], writes=[stt])
            c.op("act", lambda e: e.activation(out=stt[0:P, 2:3], in_=stt[0:P, 1:2], func=AF.Sqrt), reads=[stt], writes=[stt])
            c.op("dve", lambda e: e.reciprocal(out=stt[0:P, 3:4], in_=stt[0:P, 2:3]), reads=[stt], writes=[stt])
            c.op("dve", lambda e: e.scalar_tensor_tensor(out=junk[0:P, :], in0=xin[0:P, :], scalar=stt[0:P, 3:4], in1=A[0:P, :],
                                                          op0=ALU.mult, op1=ALU.mult), reads=[xin, stt, A], writes=[junk])
            c.op("dve", lambda e: e.tensor_tensor(out=hout[0:P, :], in0=junk[0:P, :], in1=B[0:P, :], op=ALU.add),
                 reads=[junk, B], writes=[hout])

        def transpose_h(P, hsrc, col0):
            for half in range(2):
                for k in range(8):
                    kk = half * 8 + k
                    c.op("pe", lambda e: e.transpose(out=pT[half][:, k, 0:P], in_=hsrc[0:P, kk * 128:(kk + 1) * 128],
                                                     identity=idb[0:P, 0:P]), reads=[hsrc, idb], writes=[pT[half]])
                eng = "act" if half == 0 else "dve"
                if eng == "act":
                    c.op("act", lambda e: e.copy(out=hT[:, half * 8:(half + 1) * 8, col0:col0 + P], in_=pT[half][:, :, 0:P]),
                         reads=[pT[half]], writes=[hT])
                else:
                    c.op("dve", lambda e: e.tensor_copy(out=hT[:, half * 8:(half + 1) * 8, col0:col0 + P], in_=pT[half][:, :, 0:P]),
                         reads=[pT[half]], writes=[hT])

        def rope_into(P, src, nh, cst, dst):
            s3 = src[0:P, 0:nh * 64].rearrange("p (h d) -> p h d", d=64)
            a3 = ropa[0:P, 0:nh * 64].rearrange("p (h d) -> p h d", d=64)
            b3 = ropb[0:P, 0:nh * 64].rearrange("p (h d) -> p h d", d=64)
            d3 = dst.rearrange("p (h d) -> p h d", d=64)
            cos2 = cst[0:P, 0:64].unsqueeze(1).broadcast_to([P, nh, 64])
            nsin = cst[0:P, 64:96].unsqueeze(1).broadcast_to([P, nh, 32])
            psin = cst[0:P, 96:128].unsqueeze(1).broadcast_to([P, nh, 32])
            c.op("dve", lambda e: e.tensor_tensor(out=a3, in0=s3, in1=cos2, op=ALU.mult), reads=[src, cst], writes=[ropa])
            c.op("dve", lambda e: e.tensor_tensor(out=b3[:, :, 0:32], in0=s3[:, :, 32:64], in1=nsin, op=ALU.mult),
                 reads=[src, cst], writes=[ropb])
            c.op("dve", lambda e: e.tensor_tensor(out=b3[:, :, 32:64], in0=s3[:, :, 0:32], in1=psin, op=ALU.mult),
                 reads=[src, cst], writes=[ropb])
            return a3, b3, d3

        def proc_tile(P, NTOK, xin, is_sample, slot):
            off = NTOK - P
            for bank in range(6):
                b = load_w(w_in_v, bank * 512, 512)
                for half in range(2):
                    p = next_pc()
                    for jj in range(2):
                        j4 = half * 2 + jj
                        for k in range(16):
                            c.op("pe", lambda e: e.matmul(p[:, jj, 0:NTOK], lhsT=b[:, k, j4 * 128:(j4 + 1) * 128],
                                                          rhs=hT[:, k, 0:NTOK], start=(k == 0), stop=(k == 15)),
                                 reads=[b, b.r2, hT], writes=[p])
                    j0 = (bank % 2) * 4 + half * 2
                    if bank < 2:
                        c.op("act", lambda e: e.copy(out=bgT[:, j0:j0 + 2, 0:P], in_=p[:, :, off:NTOK]), reads=[p], writes=[bgT])
                    elif bank < 4:
                        c.op("act", lambda e: e.copy(out=cgT[:, j0:j0 + 2, 0:NTOK], in_=p[:, :, 0:NTOK]), reads=[p], writes=[cgT])
                    else:
                        c.op("dve", lambda e: e.tensor_tensor(out=zcT[:, j0:j0 + 2, 0:NTOK], in0=p[:, :, 0:NTOK],
                                                              in1=cgT[:, j0:j0 + 2, 0:NTOK], op=ALU.mult),
                             reads=[p, cgT], writes=[zcT])
            if not is_sample:
                c.op("dve", lambda e: e.tensor_scalar(out=zcT[:, :, 0:2], in0=zcT[:, :, 0:2], scalar1=pfl_t[:, slot:slot + 1],
                                                      scalar2=None, op0=ALU.mult), reads=[zcT, pfl_t], writes=[zcT])
                for j in range(8):
                    c.op("dve", lambda e: e.tensor_scalar(out=acc[:, :], in0=zcT[:, j, 2:130], scalar1=cwT[:, j, 2:3],
                                                          scalar2=cbT[:, j:j + 1], op0=ALU.mult, op1=ALU.add),
                         reads=[zcT, cwT, cbT], writes=[acc])
                    c.op("dve", lambda e: e.scalar_tensor_tensor(out=acc[:, :], in0=zcT[:, j, 1:129], scalar=cwT[:, j, 1:2],
                                                                  in1=acc[:, :], op0=ALU.mult, op1=ALU.add),
                         reads=[zcT, cwT, acc], writes=[acc])
                    c.op("dve", lambda e: e.scalar_tensor_tensor(out=acc[:, :], in0=zcT[:, j, 0:128], scalar=cwT[:, j, 0:1],
                                                                  in1=acc[:, :], op0=ALU.mult, op1=ALU.add),
                         reads=[zcT, cwT, acc], writes=[acc])
                    c.op("dve", lambda e: e.tensor_tensor(out=catT[:, j, :], in0=acc[:, :], in1=bgT[:, j, :], op=ALU.mult),
                         reads=[acc, bgT], writes=[catT])
                for t_ in range(2):
                    c.dma("sp", lambda e: e.dma_start(out=conv_o[slot, t_].rearrange("(c p) -> p c", p=128), in_=zcT[:, :, 128 + t_],
                                                      allow_slow_non_contiguous=True), reads=[zcT], is_output=True)
            else:
                c.op("dve", lambda e: e.tensor_copy(out=sctx[:, :, :, 2:6],
                                                    in_=zcT[:, :, 0:16].rearrange("p c (s t) -> p c s t", t=4)),
                     reads=[zcT], writes=[sctx])
                for j in range(8):
                    c.op("dve", lambda e: e.tensor_scalar(out=sacc[:, :, :], in0=sctx[:, j, :, 2:6], scalar1=cwT[:, j, 2:3],
                                                          scalar2=cbT[:, j:j + 1], op0=ALU.mult, op1=ALU.add),
                         reads=[sctx, cwT, cbT], writes=[sacc])
                    c.op("dve", lambda e: e.scalar_tensor_tensor(out=sacc[:, :, :], in0=sctx[:, j, :, 1:5], scalar=cwT[:, j, 1:2],
                                                                  in1=sacc[:, :, :], op0=ALU.mult, op1=ALU.add),
                         reads=[sctx, cwT, sacc], writes=[sacc])
                    c.op("dve", lambda e: e.scalar_tensor_tensor(out=sacc[:, :, :], in0=sctx[:, j, :, 0:4], scalar=cwT[:, j, 0:1],
                                                                  in1=sacc[:, :, :], op0=ALU.mult, op1=ALU.add),
                         reads=[sctx, cwT, sacc], writes=[sacc])
                    c.op("dve", lambda e: e.tensor_tensor(out=catT[:, j, 0:16].rearrange("p (s t) -> p s t", t=4), in0=sacc[:, :, :],
                                                          in1=bgT[:, j, 0:16].rearrange("p (s t) -> p s t", t=4), op=ALU.mult),
                         reads=[sacc, bgT], writes=[catT])
                for s_ in range(NS):
                    for t_ in range(2):
                        c.dma("sp", lambda e: e.dma_start(out=conv_s[s_, t_].rearrange("(c p) -> p c", p=128), in_=sctx[:, :, s_, 4 + t_],
                                                          allow_slow_non_contiguous=True), reads=[sctx], is_output=True)

            cst = cs_t
            for bank in range(2):
                b = load_w(w_in_v, 3072 + bank * 512, 512)
                p = next_pq()
                for k in range(16):
                    c.op("pe", lambda e: e.matmul(p[0:P, :], lhsT=hT[:, k, off:NTOK], rhs=b[:, k, :], start=(k == 0), stop=(k == 15)),
                         reads=[hT, b, b.r2], writes=[p])
                a3, b3, d3 = rope_into(P, p, 8, cst, q_r[0:P, bank * 512:(bank + 1) * 512])
                d4 = q_r[0:P, bank * 512:(bank + 1) * 512].rearrange("p (r f d) -> p f r d", r=4, f=2, d=64)
                c.op("dve", lambda e: e.tensor_tensor(out=d4, in0=a3.rearrange("p (f r) d -> p f r d", f=2),
                                                      in1=b3.rearrange("p (f r) d -> p f r d", f=2), op=ALU.add),
                     reads=[ropa, ropb], writes=[q_r])
            for bank in range(3):
                b = load_w(w_in_v, 4096 + bank * 512, 512)
                p = next_pq()
                for k in range(16):
                    c.op("pe", lambda e: e.matmul(p[0:P, :], lhsT=hT[:, k, off:NTOK], rhs=b[:, k, :], start=(k == 0), stop=(k == 15)),
                         reads=[hT, b, b.r2], writes=[p])
                if bank == 0:
                    c.op("act", lambda e: e.copy(out=rows_t[0:P, 0:512], in_=p[0:P, :]), reads=[p], writes=[rows_t])
                elif bank == 1:
                    a3, b3, d3 = rope_into(P, p, 4, cst, rows_t[0:P, 512:768])
                    c.op("dve", lambda e: e.tensor_tensor(out=d3, in0=a3, in1=b3, op=ALU.add), reads=[ropa, ropb], writes=[rows_t])
                    c.op("act", lambda e: e.copy(out=rows_t[0:P, 768:1024], in_=p[0:P, 256:512]), reads=[p], writes=[rows_t])
                else:
                    a3, b3, d3 = rope_into(P, p, 4, cst, win_t[0:P, 0:256])
                    c.op("dve", lambda e: e.tensor_tensor(out=d3, in0=a3, in1=b3, op=ALU.add), reads=[ropa, ropb], writes=[win_t])
                    c.op("act", lambda e: e.copy(out=win_t[0:P, 256:512], in_=p[0:P, 256:512]), reads=[p], writes=[win_t])
            if not is_sample:
                c.dma("sp", lambda e: e.dma_start(out=rows_o[slot], in_=rows_t[:]), reads=[rows_t], is_output=True)
                c.dma("sp", lambda e: e.dma_start(out=win_o[slot], in_=win_t[:]), reads=[win_t], is_output=True)
            else:
                c.dma("sp", lambda e: e.dma_start(out=rows_s, in_=rows_t[0:16, :]), reads=[rows_t], is_output=True)
                for s_ in range(NS):
                    c.dma("sp", lambda e: e.dma_start(out=win_s[s_, 508:512, :], in_=win_t[4 * s_:4 * s_ + 4, :]),
                          reads=[win_t], is_output=True)

            b = load_w(w_in_v, 5632, 48)
            p = next_pq()
            for k in range(16):
                c.op("pe", lambda e: e.matmul(p[0:P, 0:48], lhsT=hT[:, k, off:NTOK], rhs=b[:, k, 0:48], start=(k == 0), stop=(k == 15)),
                     reads=[hT, b, b.r2], writes=[p])
            c.op("act", lambda e: e.activation(out=gates_t[0:P, :], in_=p[0:P, 0:48], func=AF.Sigmoid), reads=[p], writes=[gates_t])
            if is_sample:
                if "phaseS" in _SKIP:
                    c.op("pool", lambda e: e.memset(catT[:, 8:16, :], 0.0), writes=[catT])
                else:
                    attention_sample()
            else:
                attention_prompt(slot)

            for n in range(4):
                b = load_w(w_out_v, n * 512, 512)
                p = next_pq()
                for k in range(16):
                    c.op("pe", lambda e: e.matmul(p[0:P, :], lhsT=catT[:, k, 0:P], rhs=b[:, k, :], start=(k == 0), stop=(k == 15)),
                         reads=[catT, b, b.r2], writes=[p])
                c.op("dve", lambda e: e.tensor_tensor(out=junk[0:P, n * 512:(n + 1) * 512], in0=p[0:P, :],
                                                      in1=modG[0:P, n * 512:(n + 1) * 512], op=ALU.mult), reads=[p, modG], writes=[junk])
                c.op("dve", lambda e: e.tensor_tensor(out=x1[0:P, n * 512:(n + 1) * 512], in0=junk[0:P, n * 512:(n + 1) * 512],
                                                       in1=xin[0:P, n * 512:(n + 1) * 512], op=ALU.add), reads=[junk, xin], writes=[x1])
            col0 = 128 if is_sample else 0
            gen_mod(modA, 4, P, col0, "scale", gain=g2)
            gen_mod(modB, 3, P, col0, "plain")
            gen_mod(modG, 5, P, col0, "plain")
            norm_mod(P, x1, h2f, st1, modA, modB)
            c.op("act", lambda e: e.copy(out=hb[0:P, :], in_=h2f[0:P, :]), reads=[h2f], writes=[hb])
            transpose_h(P, hb, 0)
            qT = catT
            for n in range(4):
                b = load_w(wq_v, n * 512, 512)
                for half in range(2):
                    p = next_pc()
                    for jj in range(2):
                        j4 = half * 2 + jj
                        for k in range(16):
                            c.op("pe", lambda e: e.matmul(p[:, jj, 0:P], lhsT=b[:, k, j4 * 128:(j4 + 1) * 128],
                                                          rhs=hT[:, k, 0:P], start=(k == 0), stop=(k == 15)),
                                 reads=[b, b.r2, hT], writes=[p])
                    j0 = n * 4 + half * 2
                    c.op("act", lambda e: e.copy(out=qT[:, j0:j0 + 2, 0:P], in_=p[:, :, 0:P]), reads=[p], writes=[qT])
            S = junk
            S3 = junk[:].rearrange("p (j k) -> p j k", k=128)
            for n in range(4):
                p = next_pq()
                for jj in range(4):
                    j = n * 4 + jj
                    c.op("pe", lambda e: e.matmul(p[0:P, jj * 128:(jj + 1) * 128], lhsT=qT[:, j, 0:P], rhs=keysT[:, j, :],
                                                  start=True, stop=True), reads=[qT, keysT], writes=[p])
                c.op("act", lambda e: e.copy(out=junk[0:P, n * 512:(n + 1) * 512], in_=p[0:P, :]), reads=[p], writes=[S])
            for hp in range(8):
                for ci in range(2):
                    tt, ii = t12[ci], i12[ci]
                    src = S3[0:P, 2 * hp + ci, :]
                    c.op("dve", lambda e: e.max(out=tt[0:P, 0:8], in_=src), reads=[S], writes=[tt])
                    c.op("dve", lambda e: e.max_index(out=ii[0:P, 0:8], in_max=tt[0:P, 0:8], in_values=src), reads=[S, tt], writes=[ii])
                    c.op("dve", lambda e: e.match_replace(out=tmp128[0:P, :], in_to_replace=tt[0:P, 0:8], in_values=src, imm_value=-1e30),
                         reads=[S, tt], writes=[tmp128])
                    c.op("dve", lambda e: e.max(out=tt[0:P, 8:16], in_=tmp128[0:P, :]), reads=[tmp128], writes=[tt])
                    c.op("dve", lambda e: e.max_index(out=ii[0:P, 8:16], in_max=tt[0:P, 8:16], in_values=tmp128[0:P, :]),
                         reads=[tmp128, tt], writes=[ii])
                c.op("dve", lambda e: e.tensor_scalar(out=if12[0][0:P, :], in0=i12[0][0:P, :], scalar1=128.0, scalar2=None, op0=ALU.mult),
                     reads=[i12[0]], writes=[if12[0]])
                c.op("dve", lambda e: e.tensor_copy(out=if12[1][0:P, :], in_=i12[1][0:P, :]), reads=[i12[1]], writes=[if12[1]])
                cand3 = cand[0:P, :].rearrange("p (a b) -> p a b", b=16)
                cidx3 = cidx[0:P, :].rearrange("p (a b) -> p a b", b=16)
                c.op("dve", lambda e: e.tensor_tensor(out=cand3, in0=t12[0][0:P, :].unsqueeze(2).broadcast_to([P, 16, 16]),
                                                      in1=t12[1][0:P, :].unsqueeze(1).broadcast_to([P, 16, 16]), op=ALU.add),
                     reads=[t12[0], t12[1]], writes=[cand])
                c.op("dve", lambda e: e.tensor_tensor(out=cidx3, in0=if12[0][0:P, :].unsqueeze(2).broadcast_to([P, 16, 16]),
                                                      in1=if12[1][0:P, :].unsqueeze(1).broadcast_to([P, 16, 16]), op=ALU.add),
                     reads=[if12[0], if12[1]], writes=[cidx])
                c.op("dve", lambda e: e.max(out=c16[0:P, 0:8], in_=cand[0:P, :]), reads=[cand], writes=[c16])
                c.op("dve", lambda e: e.match_replace(out=tmp256[0:P, :], in_to_replace=c16[0:P, 0:8], in_values=cand[0:P, :], imm_value=-1e30),
                     reads=[cand, c16], writes=[tmp256])
                c.op("dve", lambda e: e.max(out=c16[0:P, 8:16], in_=tmp256[0:P, :]), reads=[tmp256], writes=[c16])
                for k in range(16):
                    c.op("dve", lambda e: e.scalar_tensor_tensor(out=tmp256[0:P, :], in0=cand[0:P, :], scalar=c16[0:P, k:k + 1],
                                                                  in1=cidx[0:P, :], op0=ALU.is_equal, op1=ALU.mult),
                         reads=[cand, c16, cidx], writes=[tmp256])
                    c.op("dve", lambda e: e.tensor_reduce(out=ef[0:P, hp * 16 + k:hp * 16 + k + 1], in_=tmp256[0:P, :], axis=AX.X, op=ALU.add),
                         reads=[tmp256], writes=[ef])
                c.op("dve", lambda e: e.tensor_scalar(out=pst[0:P, 0:1], in0=c16[0:P, 0:1], scalar1=-1.0, scalar2=None, op0=ALU.mult),
                     reads=[c16], writes=[pst])
                c.op("act", lambda e: e.activation(out=gw[0:P, hp * 16:(hp + 1) * 16], in_=c16[0:P, :], func=AF.Exp,
                                                   bias=pst[0:P, 0:1], scale=1.0, accum_out=pst[0:P, 1:2]),
                     reads=[c16, pst], writes=[gw, pst])
                c.op("dve", lambda e: e.reciprocal(out=pst[0:P, 2:3], in_=pst[0:P, 1:2]), reads=[pst], writes=[pst])
                c.op("dve", lambda e: e.tensor_scalar(out=gw[0:P, hp * 16:(hp + 1) * 16], in0=gw[0:P, hp * 16:(hp + 1) * 16],
                                                      scalar1=pst[0:P, 2:3], scalar2=None, op0=ALU.mult), reads=[gw, pst], writes=[gw])
            c.op("dve", lambda e: e.tensor_scalar(out=ef[0:P, :], in0=ef[0:P, :], scalar1=0.0, scalar2=16383.0, op0=ALU.max, op1=ALU.min),
                 reads=[ef], writes=[ef])
            c.op("dve", lambda e: e.tensor_copy(out=eu[0:P, :], in_=ef[0:P, :]), reads=[ef], writes=[eu])
            for j in range(128):
                U = (Ub + Vb)[j % 4]
                c.dma("pool", lambda e: e.indirect_dma_start(out=U[0:P, :], out_offset=None, in_=peer_u[:, :],
                                                             in_offset=bass.IndirectOffsetOnAxis(ap=eu[0:P, j:j + 1], axis=0)),
                      reads=[eu], writes=[U])
                prod = yt if j % 2 == 0 else junk
                c.op("dve", lambda e: e.tensor_tensor(out=prod[0:P, :], in0=U[0:P, :], in1=h2f[0:P, :], op=ALU.mult),
                     reads=[U, h2f], writes=[prod])
                c.op("act", lambda e: e.activation(out=prod[0:P, :], in_=prod[0:P, :], func=AF.Identity, accum_out=adot[0:P, j:j + 1]),
                     reads=[prod], writes=[prod, adot])
            c.op("act", lambda e: e.activation(out=coef[0:P, :], in_=adot[0:P, :], func=AF.Gelu_apprx_tanh), reads=[adot], writes=[coef])
            c.op("dve", lambda e: e.tensor_tensor(out=coef[0:P, :], in0=coef[0:P, :], in1=gw[0:P, :], op=ALU.mult),
                 reads=[coef, gw], writes=[coef])
            for j in range(128):
                V = (Vb + Ub)[j % 4]
                c.dma("pool", lambda e: e.indirect_dma_start(out=V[0:P, :], out_offset=None, in_=peer_v[:, :],
                                                             in_offset=bass.IndirectOffsetOnAxis(ap=eu[0:P, j:j + 1], axis=0)),
                      reads=[eu], writes=[V])
                if j == 0:
                    c.op("dve", lambda e: e.tensor_scalar(out=facc[0:P, :], in0=V[0:P, :], scalar1=coef[0:P, 0:1], scalar2=None, op0=ALU.mult),
                         reads=[V, coef], writes=[facc])
                else:
                    c.op("dve", lambda e: e.scalar_tensor_tensor(out=facc[0:P, :], in0=V[0:P, :], scalar=coef[0:P, j:j + 1], in1=facc[0:P, :],
                                                                  op0=ALU.mult, op1=ALU.add), reads=[V, coef, facc], writes=[facc])
            c.op("dve", lambda e: e.tensor_tensor(out=facc[0:P, :], in0=facc[0:P, :], in1=modG[0:P, :], op=ALU.mult),
                 reads=[facc, modG], writes=[facc])
            c.op("dve", lambda e: e.tensor_tensor(out=x1[0:P, :], in0=x1[0:P, :], in1=facc[0:P, :], op=ALU.add),
                 reads=[x1, facc], writes=[x1])
            stt = st1
            c.op("act", lambda e: e.activation(out=junk[0:P, :], in_=x1[0:P, :], func=AF.Square, accum_out=stt[0:P, 0:1]),
                 reads=[x1], writes=[junk, stt])
            c.op("dve", lambda e: e.tensor_scalar(out=stt[0:P, 1:2], in0=stt[0:P, 0:1], scalar1=1.0 / D, scalar2=EPS,
                                                  op0=ALU.mult, op1=ALU.add), reads=[stt], writes=[stt])
            c.op("act", lambda e: e.activation(out=stt[0:P, 2:3], in_=stt[0:P, 1:2], func=AF.Sqrt), reads=[stt], writes=[stt])
            c.op("dve", lambda e: e.reciprocal(out=stt[0:P, 3:4], in_=stt[0:P, 2:3]), reads=[stt], writes=[stt])
            c.dma("sp", lambda e: e.dma_start(out=facc[0:P, :], in_=fg.partition_broadcast(P)), writes=[facc])
            c.op("dve", lambda e: e.scalar_tensor_tensor(out=yt[0:P, :], in0=x1[0:P, :], scalar=stt[0:P, 3:4], in1=facc[0:P, :],
                                                          op0=ALU.mult, op1=ALU.mult), reads=[x1, stt, facc], writes=[yt])
            if not is_sample:
                c.dma("sp", lambda e: e.dma_start(out=yo[slot], in_=yt[:]), reads=[yt], is_output=True)
            else:
                c.dma("sp", lambda e: e.dma_start(out=ys, in_=yt[0:16, :]), reads=[yt], is_output=True)

        gen_mod(modA, 1, 128, 0, "scale", gain=g1)
        gen_mod(modB, 0, 128, 0, "plain")
        c.dma("pool", lambda e: e.dma_start(out=wkv[:], in_=w_in_v[:, :, 4096:5120]), writes=[wkv, rawT[0], rawT[1]])
        c.dma("pool", lambda e: e.dma_start(out=wkv2[:], in_=w_in_v[:, :, 5120:5632]), writes=[wkv2])
        c.op("pool", lambda e: e.memset(peT[:], 0.0), writes=[peT])
        for hh in range(2):
            for a_ in range(2):
                c.dma("pool", lambda e: e.dma_start(out=w1_sb[64 * hh:64 * hh + 64, a_, :, :], in_=cmp_w1[a_].rearrange("s d h -> d s h")),
                      writes=[w1_sb])
        c.dma("sp", lambda e: e.dma_start(out=tmp256[0:32, 0:128].rearrange("p (a d) -> p a d", a=2), in_=cmp_pe.rearrange("a s d -> s a d")),
              writes=[tmp256])
        ppe = next_pq()
        for a_ in range(2):
            c.op("pe", lambda e: e.transpose(out=ppe[0:64, a_ * 32:(a_ + 1) * 32], in_=tmp256[0:32, a_ * 64:(a_ + 1) * 64], identity=idf[0:32, 0:32]),
                 reads=[tmp256, idf], writes=[ppe])
        c.op("dve", lambda e: e.tensor_copy(out=peT[0:64, :, 0:32], in_=ppe[0:64, 0:64].rearrange("p (a s) -> p a s", a=2)),
             reads=[ppe], writes=[peT])
        c.dma("pool", lambda e: e.dma_start(out=w2_sb[:], in_=cmp_w2.rearrange("a h d -> h a d")), writes=[w2_sb])
        c.dma("sp", lambda e: e.dma_start(out=b1T[:], in_=cmp_b1.rearrange("a h -> h a"), allow_slow_non_contiguous=True), writes=[b1T])
        for a_ in range(2):
            c.dma("sp", lambda e: e.dma_start(out=b2bc[:, a_, :], in_=cmp_b2[a_:a_ + 1, :].partition_broadcast(128)), writes=[b2bc])
        c.dma("pool", lambda e: e.dma_start(out=ov_sb[:], in_=ovm), writes=[ov_sb])
        c.op("pool", lambda e: e.memset(vs_st[:], 1.0), writes=[vs_st])
        c.op("pool", lambda e: e.memset(vw_st[:], 1.0), writes=[vw_st])
        c.op("pool", lambda e: e.memset(vc1[:], 1.0), writes=[vc1])
        for a_ in range(2):
            p = next_pq()
            for s_ in range(32):
                c.op("pe", lambda e: e.matmul(p[:, 0:2], lhsT=w1_sb[0:64, a_, s_, :], rhs=peT[0:64, a_, s_:s_ + 2],
                                              start=(s_ == 0), stop=(s_ == 31)), reads=[w1_sb, peT], writes=[p])
            c.op("dve", lambda e: e.tensor_tensor(out=biasH[:, a_:a_ + 1], in0=p[:, 0:1], in1=b1T[:, a_:a_ + 1], op=ALU.add),
                 reads=[p, b1T], writes=[biasH])

        def compress_group(H, kdst=None, vdst=None):
            if "compress" in _SKIP:
                return
            kdst = kcT if kdst is None else kdst
            vdst = vc1 if vdst is None else vdst
            rt = rawT[H % 2]
            c.dma("sp", lambda e: e.dma_start(out=cs_t[:], in_=cscmp[H]), writes=[cs_t])
            pend = None
            ci = 0
            for a_ in range(2):
                for g in range(4):
                    p0 = 64 * (g % 2)
                    blk = a_ * 2 + g // 2
                    ph = next_pq()
                    for s_ in range(32):
                        c.op("pe", lambda e: e.matmul(ph[:, 0:128], lhsT=w1_sb[p0:p0 + 64, a_, s_, :],
                                                      rhs=rt[p0:p0 + 64, blk, s_:s_ + 16 * 127 + 1:16],
                                                      start=(s_ == 0), stop=(s_ == 31)), reads=[w1_sb, rt], writes=[ph])
                    hid = hidT if ci % 2 == 0 else hidT2
                    ci += 1
                    c.op("act", lambda e: e.activation(out=hid[:], in_=ph[:, 0:128], func=AF.Gelu_apprx_tanh, bias=biasH[:, a_:a_ + 1]),
                         reads=[ph, biasH], writes=[hid])

                    def fin(a_=a_, g=g, hid=hid):
                        po = next_pq()
                        c.op("pe", lambda e: e.matmul(po[:, 0:64], lhsT=hid[:], rhs=w2_sb[:, a_, :], start=True, stop=True),
                             reads=[hid, w2_sb], writes=[po])
                        if a_ == 0:
                            c.op("dve", lambda e: e.tensor_tensor(out=kc_tok[:, g, :], in0=po[:, 0:64], in1=b2bc[:, 0, :], op=ALU.add),
                                 reads=[po, b2bc], writes=[kc_tok])
                        else:
                            c.op("dve", lambda e: e.tensor_tensor(out=vdst[:, H, g, 0:64], in0=po[:, 0:64], in1=b2bc[:, 1, :], op=ALU.add),
                                 reads=[po, b2bc], writes=[vdst])
                    if pend is not None:
                        pend()
                    pend = fin
            if pend is not None:
                pend()
            kct = View(kc_tok[:].rearrange("p g d -> p (g d)"), kc_tok)
            a3, b3, d3 = rope_into(128, kct, 4, cs_t, kcr[:, :])
            c.op("dve", lambda e: e.tensor_tensor(out=d3, in0=a3, in1=b3, op=ALU.add), reads=[ropa, ropb], writes=[kcr])
            for gp in range(2):
                c.op("pe", lambda e: e.transpose(out=pT[0][:, gp, :], in_=kcr[:, gp * 128:(gp + 1) * 128], identity=idb[:]),
                     reads=[kcr, idb], writes=[pT[0]])
            c.op("act", lambda e: e.copy(out=kdst[:, :, H * 128:(H + 1) * 128], in_=pT[0][:, 0:2, :]), reads=[pT[0]], writes=[kdst])

        def flush_stage(G8):
            if "flush" in _SKIP:
                return
            c.dma("sp", lambda e: e.dma_start(out=kTs_d[:, :, G8 * 1024:(G8 + 1) * 1024], in_=kTs_st[:]), reads=[kTs_st], writes=[kv_res])
            c.dma("sp", lambda e: e.dma_start(out=kTw_d[:, :, G8 * 1024:(G8 + 1) * 1024], in_=kTw_st[:]), reads=[kTw_st], writes=[kv_res])
            for g in range(4):
                c.dma("sp", lambda e: e.dma_start(out=vs_d[g, :, G8 * 8:(G8 + 1) * 8, :], in_=vs_st[:, g, :, :]), reads=[vs_st], writes=[kv_res])
                c.dma("sp", lambda e: e.dma_start(out=vw_d[g, :, G8 * 8:(G8 + 1) * 8, :], in_=vw_st[:, g, :, :]), reads=[vw_st], writes=[kv_res])

        import os as _os1
        _ktiles = int(_os1.environ.get("KTILES", "64"))
        for t in range(_ktiles):
            xin = xt1
            c.dma("sp", lambda e: e.dma_start(out=xin[:], in_=xp[t]), writes=[xin])
            c.dma("sp", lambda e: e.dma_start(out=cs_t[:], in_=csa[t]), writes=[cs_t])
            norm_mod(128, xin, hb, st1, modA, modB)
            transpose_h(128, hb, 0)
            G16, t16 = t // 16, t % 16
            t8 = t % 8
            p = next_pq()
            for blk in range(0 if "raw" in _SKIP else 4):
                for k in range(16):
                    c.op("pe", lambda e: e.matmul(p[:, blk * 128:(blk + 1) * 128], lhsT=wkv[:, k, blk * 128:(blk + 1) * 128],
                                                  rhs=hT[:, k, 0:128], start=(k == 0), stop=(k == 15)), reads=[wkv, hT], writes=[p])
            c.op("act", lambda e: e.copy(out=rawT[G16 % 2][:, :, t16 * 128:(t16 + 1) * 128],
                                         in_=p[:, :].rearrange("p (b t) -> p b t", t=128)), reads=[p], writes=[rawT[G16 % 2]])
            if t16 == 0 and G16 >= 1:
                prev = rawT[(G16 - 1) % 2]
                c.op("act", lambda e: e.copy(out=prev[:, :, 2048:2064], in_=p[:, :].rearrange("p (b t) -> p b t", t=128)[:, :, 0:16]),
                     reads=[p], writes=[prev])
                compress_group(G16 - 1)
                c.dma("sp", lambda e: e.dma_start(out=cs_t[:], in_=csa[t]), writes=[cs_t])
            for which in range(0 if "which" in _SKIP else 2):
                wsrc = wkv[:, :, 512:1024] if which == 0 else wkv2[:, :, :]
                wres = wkv if which == 0 else wkv2
                kst = kTs_st if which == 0 else kTw_st
                vst = vs_st if which == 0 else vw_st
                p = next_pq()
                for k in range(16):
                    c.op("pe", lambda e: e.matmul(p[:, :], lhsT=hT[:, k, 0:128], rhs=wsrc[:, k, :], start=(k == 0), stop=(k == 15)),
                         reads=[hT, wres], writes=[p])
                a3, b3, d3 = rope_into(128, p, 4, cs_t, ks_b[:, :])
                c.op("dve", lambda e: e.tensor_tensor(out=d3, in0=a3, in1=b3, op=ALU.add), reads=[ropa, ropb], writes=[ks_b])
                if "vcopy" not in _SKIP:
                    c.op("dve", lambda e: e.tensor_copy(out=vst[:, :, t8, 0:64], in_=p[:, 256:512].rearrange("p (g d) -> p g d", d=64)),
                         reads=[p], writes=[vst])
                if "ktr" in _SKIP:
                    continue
                for gp in range(2):
                    c.op("pe", lambda e: e.transpose(out=pT[0][:, gp, :], in_=ks_b[:, gp * 128:(gp + 1) * 128], identity=idb[:]),
                         reads=[ks_b, idb], writes=[pT[0]])
                c.op("dve", lambda e: e.tensor_copy(out=kst[:, :, t8 * 128:(t8 + 1) * 128], in_=pT[0][:, 0:2, :]),
                     reads=[pT[0]], writes=[kst])
            if t8 == 7:
                flush_stage(t // 8)
        last = rawT[3 % 2]
        c.op("pool", lambda e: e.memset(last[:, :, 2048:2064], 0.0), writes=[last])
        compress_group(3)
        c.barrier()

        if "phaseS" not in _SKIP:
            wkv_f = wkv[:].rearrange("p a b -> p (a b)").bitcast(F32)
            wkv2_b = wkv2[:].rearrange("p a b -> p (a b)")
            pgb = [View(wkv_f[:, i * 1024:(i + 1) * 1024], Res("pgb")) for i in range(2)]
            pgh2 = [View(wkv2_b[:, 0:1024], Res("pgh0")), View(wkv2_b[:, 4096:5120], Res("pgh1"))]
            kcT_s = View(wkv2_b[:, 1024:2048].rearrange("p (a b) -> p a b", a=2), Res("kcT_s"))
            vc1_s = View(wkv2_b[:, 2048:2048 + 1040].rearrange("p (a g d) -> p a g d", a=4, g=4), Res("vc1_s"))
            c.dma("sp", lambda e: e.dma_start(out=pio_t[:], in_=piota), writes=[pio_t])
            c.op("pool", lambda e: e.memset(vc1_s[:, :, :, :], 1.0), writes=[vc1_s])
            for b in range(NS):
                c.dma("sp", lambda e: e.dma_start(out=pt_i[:], in_=ptab[b:b + 1, :].partition_broadcast(128)), writes=[pt_i])
                c.op("dve", lambda e: e.tensor_scalar(out=idx_f[:], in0=pt_i[:], scalar1=128.0, scalar2=pio_t[:, 0:1], op0=ALU.mult, op1=ALU.add),
                     reads=[pt_i, pio_t], writes=[idx_f])
                c.op("dve", lambda e: e.tensor_scalar(out=idx_f[:], in0=idx_f[:], scalar1=0.0, scalar2=float(2560 * 128 - 1), op0=ALU.max, op1=ALU.min),
                     reads=[idx_f], writes=[idx_f])
                c.op("dve", lambda e: e.tensor_copy(out=idx_u[:], in_=idx_f[:]), reads=[idx_f], writes=[idx_u])
                for pg in range(64):
                    pb_ = pgb[pg % 2]
                    pgh = pgh2[pg % 2]
                    c.dma("pool", lambda e: e.indirect_dma_start(out=pb_[:, :], out_offset=None, in_=cache[:, :],
                                                                 in_offset=bass.IndirectOffsetOnAxis(ap=idx_u[:, pg:pg + 1], axis=0)),
                          reads=[idx_u], writes=[pb_])
                    c.op("act", lambda e: e.copy(out=pgh[:, :], in_=pb_[:, :]), reads=[pb_], writes=[pgh])
                    G16, t16, t8 = pg // 16, pg % 16, pg % 8
                    for blk in range(4):
                        c.op("pe", lambda e: e.transpose(out=pT[0][:, blk, :], in_=pgh[:, blk * 128:(blk + 1) * 128], identity=idb[:]),
                             reads=[pgh, idb], writes=[pT[0]])
                    c.op("dve", lambda e: e.tensor_copy(out=rawT[G16 % 2][:, :, t16 * 128:(t16 + 1) * 128], in_=pT[0][:, 0:4, :]),
                         reads=[pT[0]], writes=[rawT[G16 % 2]])
                    if t16 == 0 and G16 >= 1:
                        prev = rawT[(G16 - 1) % 2]
                        c.op("dve", lambda e: e.tensor_copy(out=prev[:, :, 2048:2064], in_=pT[0][:, 0:4, 0:16]), reads=[pT[0]], writes=[prev])
                        compress_group(G16 - 1, kcT_s, vc1_s)
                    for gp in range(2):
                        c.op("pe", lambda e: e.transpose(out=pT[1][:, gp, :], in_=pgh[:, 512 + gp * 128:512 + (gp + 1) * 128], identity=idb[:]),
                             reads=[pgh, idb], writes=[pT[1]])
                    c.op("dve", lambda e: e.tensor_copy(out=kTs_st[:, :, t8 * 128:(t8 + 1) * 128], in_=pT[1][:, 0:2, :]),
                         reads=[pT[1]], writes=[kTs_st])
                    c.op("dve", lambda e: e.tensor_copy(out=vs_st[:, :, t8, 0:64], in_=pgh[:, 768:1024].rearrange("p (g d) -> p g d", d=64)),
                         reads=[pgh], writes=[vs_st])
                    if t8 == 7:
                        G8 = pg // 8
                        c.dma("sp", lambda e: e.dma_start(out=kTs_s[b, :, :, G8 * 1024:(G8 + 1) * 1024], in_=kTs_st[:]), reads=[kTs_st], writes=[kv_res])
                        for g in range(4):
                            c.dma("sp", lambda e: e.dma_start(out=vs_s[b, g, :, G8 * 8:(G8 + 1) * 8, :], in_=vs_st[:, g, :, :]), reads=[vs_st], writes=[kv_res])
                lastS = rawT[3 % 2]
                c.op("pool", lambda e: e.memset(lastS[:, :, 2048:2064], 0.0), writes=[lastS])
                compress_group(3, kcT_s, vc1_s)
                c.dma("sp", lambda e: e.dma_start(out=kc_s[b], in_=kcT_s[:, :, :]), reads=[kcT_s], writes=[kv_res])
                c.dma("sp", lambda e: e.dma_start(out=vc_s[b], in_=vc1_s[:, :, :, :]), reads=[vc1_s], writes=[kv_res])
                for wc in range(4):
                    pb_ = pgb[wc % 2]
                    pgh = pgh2[wc % 2]
                    c.dma("sp", lambda e: e.dma_start(out=pb_[:, 0:512], in_=swin[b, wc * 128:(wc + 1) * 128, :]), writes=[pb_])
                    c.op("act", lambda e: e.copy(out=pgh[:, 0:512], in_=pb_[:, 0:512]), reads=[pb_], writes=[pgh])
                    for gp in range(2):
                        c.op("pe", lambda e: e.transpose(out=pT[1][:, gp, :], in_=pgh[:, gp * 128:(gp + 1) * 128], identity=idb[:]),
                             reads=[pgh, idb], writes=[pT[1]])
                    c.op("dve", lambda e: e.tensor_copy(out=kTw_st[:, :, wc * 128:(wc + 1) * 128], in_=pT[1][:, 0:2, :]),
                         reads=[pT[1]], writes=[kTw_st])
                    c.op("dve", lambda e: e.tensor_copy(out=vw_st[:, :, wc, 0:64], in_=pgh[:, 256:512].rearrange("p (g d) -> p g d", d=64)),
                         reads=[pgh], writes=[vw_st])
                c.dma("sp", lambda e: e.dma_start(out=kTw_s[b], in_=kTw_st[:, :, 0:512]), reads=[kTw_st], writes=[kv_res])
                for g in range(4):
                    c.dma("sp", lambda e: e.dma_start(out=vw_s[b, g], in_=vw_st[:, g, 0:4, :]), reads=[vw_st], writes=[kv_res])
            c.barrier()
        esA.close()
        import os as _os
        _dbg = _os.environ.get("KDBG", "")
        if _dbg == "A":
            c.finish()
            return nc

        wb = [c.sb("wb%d" % i, [128, 16, 512], BF16) for i in range(2)]
        for b_ in wb:
            b_.r2 = Res("wb_hi")
        state["wbufs"] = wb

        def _f32v(b_):
            return b_[:].rearrange("p a b -> p (a b)").bitcast(F32)

        Ub = [View(_f32v(wb[0])[:, 0:D], wb[0].r), View(_f32v(wb[0])[:, D:2 * D], wb[0].r2)]
        Vb = [View(_f32v(wb[1])[:, 0:D], wb[1].r), View(_f32v(wb[1])[:, D:2 * D], wb[1].r2)]
        keysT = c.sb("keysT", [128, 16, 128], BF16)
        bgT = c.sb("bgT", [128, 8, 128])
        cgT = c.sb("cgT", [128, 8, 130])
        zcT = c.sb("zcT", [128, 8, 130])
        acc = c.sb("acc", [128, 128])
        catT = c.sb("catT", [128, 16, 128], BF16)
        q_r = c.sb("q_r", [128, 1024], BF16)
        sctx = c.sb("sctx", [128, 8, NS, 6])
        sacc = c.sb("sacc", [128, NS, 4])
        qTp = c.sb("qTp", [128, 8, 128], BF16)
        gates_t = c.sb("gates_t", [128, 48])
        e32_t = c.sb("e32_t", [32, 16, 128], BF16)
        fmask_t = c.sb("fmask_t", [128, 128])
        cmask_t = c.sb("cmask_t", [128, 4, 128], BF16)
        dmask_t = c.sb("dmask_t", [128, 8, 128], BF16)
        wmask_t = c.sb("wmask_t", [128, 12, 128], BF16)
        kbuf = [c.sb("kbuf%d" % i, [128, 1536], BF16) for i in range(2)]
        vbuf = [c.sb("vbuf%d" % i, [128, 12, 65], BF16) for i in range(2)]
        ebuf = [c.sb("ebuf%d" % i, [128, 4, 128], BF16) for i in range(2)]
        pTb = [c.sb("pTb%d" % i, [128, 4, 128], BF16) for i in range(2)]
        attn_f = c.sb("attn_f", [128, 16, 64])
        attn_b = c.sb("attn_b", [128, 1024], BF16)
        imp_t = c.sb("imp_t", [128, 128])
        sc_t = c.sb("sc_t", [128, 128])
        sc2_t = c.sb("sc2_t", [128, 128])
        sel_b = c.sb("sel_b", [128, 128], BF16)
        selT = c.sb("selT", [32, 4, 128], BF16)
        negT = c.sb("negT", [32, 4, 4, 128], BF16)
        m16 = c.sb("m16", [128, 16])
        rs4 = c.sb("rs4", [128, 8])
        c.dma("pool", lambda e: e.dma_start(out=e32_t[:], in_=e32), writes=[e32_t])
        kcT2 = c.sb("kcT2", [128, 2, 512], BF16)
        vc2 = c.sb("vc2", [128, 4, 4, 65], BF16)
        kn_b = c.sb("kn_b", [16, 512], BF16)
        kTn = c.sb("kTn", [128, 4, 16], BF16)
        vn = c.sb("vn", [16, 2, 4, 65], BF16)
        qs = c.sb("qs", [128, 8, 4], BF16)
        gq = c.sb("gq", [4, 48])
        cm_s = c.sb("cm_s", [128, 4, 4], BF16)
        fm_s = c.sb("fm_s", [4, 128])
        wm_s = c.sb("wm_s", [128, 4, 4], BF16)
        nm_s = c.sb("nm_s", [16, NS, 4], BF16)
        c.dma("pool", lambda e: e.dma_start(out=cm_s[:], in_=cmask_sm), writes=[cm_s])
        c.dma("pool", lambda e: e.dma_start(out=wm_s[:], in_=wmask_sm), writes=[wm_s])
        c.dma("pool", lambda e: e.dma_start(out=nm_s[:], in_=nmask), writes=[nm_s])
        c.dma("sp", lambda e: e.dma_start(out=fm_s[:], in_=fmask_sm), writes=[fm_s])

        c.dma("sp", lambda e: e.dma_start(out=junk[:].rearrange("p (j d) -> p j d", d=128), in_=peer_keys.rearrange("j k d -> k j d")),
              writes=[junk])
        for n in range(4):
            p = next_pq()
            for jj in range(4):
                j = n * 4 + jj
                c.op("pe", lambda e: e.transpose(out=p[:, jj * 128:(jj + 1) * 128], in_=junk[:, j * 128:(j + 1) * 128], identity=idf[:]),
                     reads=[junk, idf], writes=[p])
            c.op("dve", lambda e: e.tensor_copy(out=keysT[:, n * 4:(n + 1) * 4, :].rearrange("p a b -> p (a b)"), in_=p[:, :]),
                 reads=[p], writes=[keysT])


        oacc = View(pT1f[:, 0:260].rearrange("p (r d) -> p r d", d=65), pT1f)
        iacc = View(pc_full[1][:, :].rearrange("p (r s) -> p r s", s=128), pc_full[1])
        astate = {"e": 0}

        def zero_bank(bank_ap, res):
            c.op("pe", lambda e: e.matmul(bank_ap, lhsT=zer[:, 0:128], rhs=zer[:, 0:512], start=True, stop=False),
                 reads=[zer], writes=[res])

        def attn_unit(P, kT_ap, kres, qrhs, v_ap, vres, mask_ap, mres, mask2_ap, m2res, last, want_imp=None, NK=128, bias=None):
            ps = next_pq()
            W = 4 * P
            c.op("pe", lambda e: e.matmul(ps[0:NK, 0:W], lhsT=kT_ap, rhs=qrhs, start=True, stop=(bias is None)), reads=[kres, qTp], writes=[ps])
            if bias is not None:
                bl, br_, bres = bias
                c.op("pe", lambda e: e.matmul(ps[0:NK, 0:W], lhsT=bl, rhs=br_, start=False, stop=True), reads=[e32_t, bres], writes=[ps])
            eb = ebuf[astate["e"] % 2]
            pb = pTb[astate["e"] % 2]
            astate["e"] += 1
            ebf = eb[:].rearrange("p r q -> p (r q)")
            pbf = pb[:].rearrange("p r q -> p (r q)")
            masks = [(m, r_) for m, r_ in ((mask_ap, mres), (mask2_ap, m2res)) if m is not None]
            if not masks:
                c.op("act", lambda e: e.activation(out=pbf[0:NK, 0:W], in_=ps[0:NK, 0:W], func=AF.Exp, scale=0.125), reads=[ps], writes=[pb])
            else:
                c.op("act", lambda e: e.activation(out=ebf[0:NK, 0:W], in_=ps[0:NK, 0:W], func=AF.Exp, scale=0.125), reads=[ps], writes=[eb])
                src, sres = ebf, eb
                for m, r_ in masks:
                    c.op("dve", lambda e: e.tensor_tensor(out=pbf[0:NK, 0:W].rearrange("p (r q) -> p r q", q=P),
                                                          in0=src[0:NK, 0:W].rearrange("p (r q) -> p r q", q=P),
                                                          in1=m.unsqueeze(1).broadcast_to([NK, 4, P]), op=ALU.mult),
                         reads=[sres, r_], writes=[pb])
                    src, sres = pbf, pb
            def stage2():
                for r in range(4):
                    c.op("pe", lambda e: e.matmul(oacc[0:P, r, :], lhsT=pbf[0:NK, r * P:(r + 1) * P], rhs=v_ap, start=False, stop=last),
                         reads=[pb, vres], writes=[oacc])
                    if want_imp is not None:
                        c.op("pe", lambda e: e.matmul(iacc[0:P, r, :], lhsT=pbf[0:NK, r * P:(r + 1) * P], rhs=want_imp, start=False, stop=last),
                             reads=[pb, ov_sb], writes=[iacc])
            prev = astate.get("pending")
            astate["pending"] = stage2
            if prev is not None:
                prev()

        def flush_units():
            prev = astate.get("pending")
            astate["pending"] = None
            if prev is not None:
                prev()

        def finish_branch(P, g, br, first_branch, gsrc=None):
            flush_units()
            c.op("dve", lambda e: e.tensor_scalar(out=rs4[0:P, 0:4], in0=oacc[0:P, :, 64], scalar1=1e-30, scalar2=None, op0=ALU.max),
                 reads=[oacc], writes=[rs4])
            c.op("dve", lambda e: e.reciprocal(out=rs4[0:P, 0:4], in_=rs4[0:P, 0:4]), reads=[rs4], writes=[rs4])
            gsrc = gates_t if gsrc is None else gsrc
            g3 = gsrc[0:P, :].rearrange("p (h b) -> p h b", b=3)
            c.op("dve", lambda e: e.tensor_tensor(out=rs4[0:P, 4:8], in0=rs4[0:P, 0:4], in1=g3[:, 4 * g:4 * g + 4, br], op=ALU.mult),
                 reads=[rs4, gsrc], writes=[rs4])
            for r in range(4):
                h = 4 * g + r
                if first_branch:
                    c.op("dve", lambda e: e.tensor_scalar(out=attn_f[0:P, h, :], in0=oacc[0:P, r, 0:64], scalar1=rs4[0:P, 4 + r:5 + r],
                                                          scalar2=None, op0=ALU.mult), reads=[oacc, rs4], writes=[attn_f])
                else:
                    c.op("dve", lambda e: e.scalar_tensor_tensor(out=attn_f[0:P, h, :], in0=oacc[0:P, r, 0:64], scalar=rs4[0:P, 4 + r:5 + r],
                                                                  in1=attn_f[0:P, h, :], op0=ALU.mult, op1=ALU.add),
                         reads=[oacc, rs4, attn_f], writes=[attn_f])

        def attention_prompt(i):
            P = 128
            c.dma("sp", lambda e: e.dma_start(out=fmask_t[:], in_=fmask[i]), writes=[fmask_t])
            c.dma("pool", lambda e: e.dma_start(out=cmask_t[:], in_=cmask[i]), writes=[cmask_t])
            c.dma("pool", lambda e: e.dma_start(out=dmask_t[:], in_=dmask[i]), writes=[dmask_t])
            c.dma("pool", lambda e: e.dma_start(out=wmask_t[:], in_=wmask[i]), writes=[wmask_t])
            for pb_ in range(8):
                c.op("pe", lambda e: e.transpose(out=pT[0][:, pb_, :], in_=q_r[:, pb_ * 128:(pb_ + 1) * 128], identity=idb[:]),
                     reads=[q_r, idb], writes=[pT[0]])
            c.op("act", lambda e: e.copy(out=qTp[:], in_=pT[0][:]), reads=[pT[0]], writes=[qTp])
            njc = min(4, (64 * i + 62) // 128 + 1)
            nkc = 8 * i + 8
            for g in range(4):
                p0, gp = 64 * (g % 2), g // 2
                qrhs = qTp[:].rearrange("p a b -> p (a b)")[p0:p0 + 64, gp * 512:(gp + 1) * 512]
                zero_bank(pT1f[:, :], pT1f)
                zero_bank(pc_full[1][:, :], pc_full[1])
                for jc in range(njc):
                    attn_unit(P, kcT[p0:p0 + 64, gp, jc * 128:(jc + 1) * 128], kcT, qrhs, vc1[:, jc, g, :], vc1,
                              cmask_t[:, jc, :], cmask_t, None, None, jc == njc - 1, want_imp=ov_sb[:, jc, :])
                finish_branch(P, g, 0, True)
                for r in range(4):
                    if r == 0:
                        c.op("dve", lambda e: e.tensor_scalar(out=imp_t[:], in0=iacc[:, 0, :], scalar1=rs4[:, 0:1], scalar2=None, op0=ALU.mult),
                             reads=[iacc, rs4], writes=[imp_t])
                    else:
                        c.op("dve", lambda e: e.scalar_tensor_tensor(out=imp_t[:], in0=iacc[:, r, :], scalar=rs4[:, r:r + 1], in1=imp_t[:],
                                                                      op0=ALU.mult, op1=ALU.add), reads=[iacc, rs4, imp_t], writes=[imp_t])
                c.op("dve", lambda e: e.tensor_tensor(out=sc_t[:], in0=imp_t[:], in1=fmask_t[:], op=ALU.add), reads=[imp_t, fmask_t], writes=[sc_t])
                c.op("dve", lambda e: e.max(out=m16[:, 0:8], in_=sc_t[:]), reads=[sc_t], writes=[m16])
                c.op("dve", lambda e: e.match_replace(out=sc2_t[:], in_to_replace=m16[:, 0:8], in_values=sc_t[:], imm_value=-3.0e38),
                     reads=[sc_t, m16], writes=[sc2_t])
                c.op("dve", lambda e: e.max(out=m16[:, 8:16], in_=sc2_t[:]), reads=[sc2_t], writes=[m16])
                c.op("dve", lambda e: e.tensor_scalar(out=m16[:, 0:1], in0=m16[:, 15:16], scalar1=-1.0e29, scalar2=None, op0=ALU.max),
                     reads=[m16], writes=[m16])
                c.op("dve", lambda e: e.tensor_scalar(out=sel_b[:], in0=sc_t[:], scalar1=m16[:, 0:1], scalar2=None, op0=ALU.is_ge),
                     reads=[sc_t, m16], writes=[sel_b])
                for sl in range(4):
                    c.op("pe", lambda e: e.transpose(out=pT[0][0:32, sl, :], in_=sel_b[:, sl * 32:(sl + 1) * 32], identity=idb[:]),
                         reads=[sel_b, idb], writes=[pT[0]])
                c.op("act", lambda e: e.copy(out=selT[:], in_=pT[0][0:32, 0:4, :]), reads=[pT[0]], writes=[selT])
                for r_ in range(4):
                    c.op("dve", lambda e: e.tensor_scalar(out=negT[:, :, r_, :], in0=selT[:, :, :], scalar1=-1.0, scalar2=30000.0,
                                                          op0=ALU.add, op1=ALU.mult), reads=[selT], writes=[negT])
                zero_bank(pT1f[:, :], pT1f)
                for G8 in range(nkc // 8):
                    kb, vb_ = kbuf[G8 % 2], vbuf[G8 % 2]
                    c.dma("sp", lambda e: e.dma_start(out=kb[p0:p0 + 64, 0:1024], in_=kTs_d[p0:p0 + 64, gp, G8 * 1024:(G8 + 1) * 1024]),
                          reads=[kv_res], writes=[kb])
                    c.dma("sp", lambda e: e.dma_start(out=vb_[:, 0:8, :], in_=vs_d[g, :, G8 * 8:(G8 + 1) * 8, :]), reads=[kv_res], writes=[vb_])
                    for k8 in range(8):
                        kc = G8 * 8 + k8
                        diag = kc >= 8 * i
                        attn_unit(P, kb[p0:p0 + 64, k8 * 128:(k8 + 1) * 128], kb, qrhs, vb_[:, k8, :], vb_,
                                  None, None, dmask_t[:, kc - 8 * i, :] if diag else None, dmask_t if diag else None, kc == nkc - 1,
                                  bias=(e32_t[:, kc % 16, :], negT[:, kc // 16, :, :].rearrange("p r q -> p (r q)"), negT))
                finish_branch(P, g, 1, False)
                zero_bank(pT1f[:, :], pT1f)
                kc0 = max(0, 8 * i - 4)
                nw = 8 * i + 8 - kc0
                kb, vb_ = kbuf[0], vbuf[0]
                c.dma("sp", lambda e: e.dma_start(out=kb[p0:p0 + 64, 0:nw * 128], in_=kTw_d[p0:p0 + 64, gp, kc0 * 128:(kc0 + nw) * 128]),
                      reads=[kv_res], writes=[kb])
                c.dma("sp", lambda e: e.dma_start(out=vb_[:, 0:nw, :], in_=vw_d[g, :, kc0:kc0 + nw, :]), reads=[kv_res], writes=[vb_])
                for w_ in range(nw):
                    kc = kc0 + w_
                    wi = kc - (8 * i - 4)
                    attn_unit(P, kb[p0:p0 + 64, w_ * 128:(w_ + 1) * 128], kb, qrhs, vb_[:, w_, :], vb_,
                              wmask_t[:, wi, :], wmask_t, None, None, w_ == nw - 1)
                finish_branch(P, g, 2, False)
            c.op("act", lambda e: e.copy(out=attn_b[:], in_=attn_f[:].rearrange("p h d -> p (h d)")), reads=[attn_f], writes=[attn_b])
            for k in range(8):
                c.op("pe", lambda e: e.transpose(out=pT[0][:, k, :], in_=attn_b[:, k * 128:(k + 1) * 128], identity=idb[:]),
                     reads=[attn_b, idb], writes=[pT[0]])
            c.op("dve", lambda e: e.tensor_copy(out=catT[:, 8:16, :], in_=pT[0][:]), reads=[pT[0]], writes=[catT])

        def attention_sample():
            P = 4
            for pb_ in range(8):
                c.op("pe", lambda e: e.transpose(out=pT[0][:, pb_, 0:16], in_=q_r[0:16, pb_ * 128:(pb_ + 1) * 128], identity=idb[0:16, 0:16]),
                     reads=[q_r, idb], writes=[pT[0]])
            c.op("dve", lambda e: e.tensor_copy(out=qTp[:, :, 0:16], in_=pT[0][:, :, 0:16]), reads=[pT[0]], writes=[qTp])
            c.op("dve", lambda e: e.tensor_copy(out=kn_b[:, 0:256], in_=rows_t[0:16, 512:768]), reads=[rows_t], writes=[kn_b])
            c.op("dve", lambda e: e.tensor_copy(out=kn_b[:, 256:512], in_=win_t[0:16, 0:256]), reads=[win_t], writes=[kn_b])
            c.op("pool", lambda e: e.memset(vn[:], 1.0), writes=[vn])
            c.op("dve", lambda e: e.tensor_copy(out=vn[:, 0, :, 0:64], in_=rows_t[0:16, 768:1024].rearrange("p (g d) -> p g d", d=64)),
                 reads=[rows_t], writes=[vn])
            c.op("dve", lambda e: e.tensor_copy(out=vn[:, 1, :, 0:64], in_=win_t[0:16, 256:512].rearrange("p (g d) -> p g d", d=64)),
                 reads=[win_t], writes=[vn])
            for blk in range(4):
                c.op("pe", lambda e: e.transpose(out=pT[0][:, blk, 0:16], in_=kn_b[:, blk * 128:(blk + 1) * 128], identity=idb[0:16, 0:16]),
                     reads=[kn_b, idb], writes=[pT[0]])
            c.op("dve", lambda e: e.tensor_copy(out=kTn[:], in_=pT[0][:, 0:4, 0:16]), reads=[pT[0]], writes=[kTn])
            for b in range(NS):
                c.dma("sp", lambda e: e.dma_start(out=gq[:], in_=gates_t[4 * b:4 * b + 4, :]), reads=[gates_t], writes=[gq])
                c.op("dve", lambda e: e.tensor_copy(out=qs[:], in_=qTp[:, :, 4 * b:4 * b + 4]), reads=[qTp], writes=[qs])
                c.dma("sp", lambda e: e.dma_start(out=kcT2[:], in_=kc_s[b]), reads=[kv_res], writes=[kcT2])
                c.dma("sp", lambda e: e.dma_start(out=vc2[:], in_=vc_s[b]), reads=[kv_res], writes=[vc2])
                qsf = qs[:].rearrange("p a b -> p (a b)")
                for g in range(4):
                    p0, gp = 64 * (g % 2), g // 2
                    qrhs = qsf[p0:p0 + 64, gp * 16:(gp + 1) * 16]
                    zero_bank(pT1f[:, :], pT1f)
                    zero_bank(pc_full[1][:, :], pc_full[1])
                    for jc in range(4):
                        attn_unit(P, kcT2[p0:p0 + 64, gp, jc * 128:(jc + 1) * 128], kcT2, qrhs, vc2[:, jc, g, :], vc2,
                                  cm_s[:, jc, :], cm_s, None, None, jc == 3, want_imp=ov_sb[:, jc, :])
                    finish_branch(P, g, 0, True, gsrc=gq)
                    for r in range(4):
                        if r == 0:
                            c.op("dve", lambda e: e.tensor_scalar(out=imp_t[0:P, :], in0=iacc[0:P, 0, :], scalar1=rs4[0:P, 0:1], scalar2=None, op0=ALU.mult),
                                 reads=[iacc, rs4], writes=[imp_t])
                        else:
                            c.op("dve", lambda e: e.scalar_tensor_tensor(out=imp_t[0:P, :], in0=iacc[0:P, r, :], scalar=rs4[0:P, r:r + 1], in1=imp_t[0:P, :],
                                                                          op0=ALU.mult, op1=ALU.add), reads=[iacc, rs4, imp_t], writes=[imp_t])
                    c.op("dve", lambda e: e.tensor_tensor(out=sc_t[0:P, :], in0=imp_t[0:P, :], in1=fm_s[0:P, :], op=ALU.add), reads=[imp_t, fm_s], writes=[sc_t])
                    c.op("dve", lambda e: e.max(out=m16[0:P, 0:8], in_=sc_t[0:P, :]), reads=[sc_t], writes=[m16])
                    c.op("dve", lambda e: e.match_replace(out=sc2_t[0:P, :], in_to_replace=m16[0:P, 0:8], in_values=sc_t[0:P, :], imm_value=-3.0e38),
                         reads=[sc_t, m16], writes=[sc2_t])
                    c.op("dve", lambda e: e.max(out=m16[0:P, 8:16], in_=sc2_t[0:P, :]), reads=[sc2_t], writes=[m16])
                    c.op("dve", lambda e: e.tensor_scalar(out=sel_b[0:P, :], in0=sc_t[0:P, :], scalar1=m16[0:P, 14:15], scalar2=None, op0=ALU.is_ge),
                         reads=[sc_t, m16], writes=[sel_b])
                    for sl in range(4):
                        c.op("pe", lambda e: e.transpose(out=pT[0][0:32, sl, 0:P], in_=sel_b[0:P, sl * 32:(sl + 1) * 32], identity=idb[0:P, 0:P]),
                             reads=[sel_b, idb], writes=[pT[0]])
                    c.op("dve", lambda e: e.tensor_copy(out=selT[:, :, 0:P], in_=pT[0][0:32, 0:4, 0:P]), reads=[pT[0]], writes=[selT])
                    negS = negT[:].rearrange("p a r q -> p (a r q)")[:, 0:64].rearrange("p (a r q) -> p a r q", a=4, r=4)
                    for r_ in range(4):
                        c.op("dve", lambda e: e.tensor_scalar(out=negS[:, :, r_, :], in0=selT[:, :, 0:P], scalar1=-1.0, scalar2=30000.0,
                                                              op0=ALU.add, op1=ALU.mult), reads=[selT], writes=[negT])
                    zero_bank(pT1f[:, :], pT1f)
                    for G8 in range(8):
                        kb, vb_ = kbuf[G8 % 2], vbuf[G8 % 2]
                        c.dma("sp", lambda e: e.dma_start(out=kb[p0:p0 + 64, 0:1024], in_=kTs_s[b, p0:p0 + 64, gp, G8 * 1024:(G8 + 1) * 1024]),
                              reads=[kv_res], writes=[kb])
                        c.dma("sp", lambda e: e.dma_start(out=vb_[:, 0:8, :], in_=vs_s[b, g, :, G8 * 8:(G8 + 1) * 8, :]), reads=[kv_res], writes=[vb_])
                        for k8 in range(8):
                            kc = G8 * 8 + k8
                            attn_unit(P, kb[p0:p0 + 64, k8 * 128:(k8 + 1) * 128], kb, qrhs, vb_[:, k8, :], vb_, None, None, None, None, False,
                                      bias=(e32_t[:, kc % 16, :], negS[:, kc // 16, :, :].rearrange("p r q -> p (r q)"), negT))
                    attn_unit(P, kTn[p0:p0 + 64, gp, :], kTn, qrhs, vn[:, 0, g, :], vn, nm_s[:, b, :], nm_s, None, None, True, NK=16)
                    finish_branch(P, g, 1, False, gsrc=gq)
                    zero_bank(pT1f[:, :], pT1f)
                    kb, vb_ = kbuf[0], vbuf[0]
                    c.dma("sp", lambda e: e.dma_start(out=kb[p0:p0 + 64, 0:512], in_=kTw_s[b, p0:p0 + 64, gp, :]), reads=[kv_res], writes=[kb])
                    c.dma("sp", lambda e: e.dma_start(out=vb_[:, 0:4, :], in_=vw_s[b, g]), reads=[kv_res], writes=[vb_])
                    for wc in range(4):
                        attn_unit(P, kb[p0:p0 + 64, wc * 128:(wc + 1) * 128], kb, qrhs, vb_[:, wc, :], vb_, wm_s[:, wc, :], wm_s, None, None, False)
                    attn_unit(P, kTn[p0:p0 + 64, 2 + gp, :], kTn, qrhs, vn[:, 1, g, :], vn, nm_s[:, b, :], nm_s, None, None, True, NK=16)
                    finish_branch(P, g, 2, False, gsrc=gq)
                c.op("act", lambda e: e.copy(out=attn_b[0:P, :], in_=attn_f[0:P, :, :].rearrange("p h d -> p (h d)")), reads=[attn_f], writes=[attn_b])
                for k in range(8):
                    c.op("pe", lambda e: e.transpose(out=pT[0][:, k, 0:P], in_=attn_b[0:P, k * 128:(k + 1) * 128], identity=idb[0:P, 0:P]),
                         reads=[attn_b, idb], writes=[pT[0]])
                c.op("dve", lambda e: e.tensor_copy(out=catT[:, 8:16, 4 * b:4 * b + 4], in_=pT[0][:, :, 0:P]), reads=[pT[0]], writes=[catT])

        gen_mod(modA, 1, 16, 128, "scale", gain=g1)
        gen_mod(modB, 0, 16, 128, "plain")
        gen_mod(modG, 2, 16, 128, "plain")
        xin = xt[0]
        c.dma("sp", lambda e: e.dma_start(out=xin[0:16, :], in_=xs), writes=[xin])
        c.dma("sp", lambda e: e.dma_start(out=cs_t[0:16, :], in_=css), writes=[cs_t])
        c.dma("sp", lambda e: e.dma_start(out=scv_t[:], in_=scv), writes=[scv_t])
        c.dma("sp", lambda e: e.dma_start(out=win_s[:, 0:508, :], in_=swin[:, 4:512, :]), is_output=True)
        pa = next_pq()
        for j in range(8):
            c.op("pe", lambda e: e.transpose(out=pa[:, j * 8:(j + 1) * 8], in_=scv_t[:, j * 128:(j + 1) * 128], identity=idf[0:8, 0:8]),
                 reads=[scv_t, idf], writes=[pa])
        c.op("dve", lambda e: e.tensor_copy(out=sctx[:, :, :, 0:2], in_=pa[:, 0:64].rearrange("p (c s t) -> p c s t", c=8, s=NS)),
             reads=[pa], writes=[sctx])
        norm_mod(16, xin, hb, st1, modA, modB)
        transpose_h(16, hb, 0)
        proc_tile(16, 16, xin, True, 0)

        for i in range(NT):
            gen_mod(modA, 1, 128, 0, "scale", gain=g1)
            gen_mod(modB, 0, 128, 0, "plain")
            gen_mod(modG, 2, 128, 0, "plain")
            xin = xt[i % 2]
            c.dma("sp", lambda e: e.dma_start(out=xin[:], in_=xo[i]), writes=[xin])
            c.dma("sp", lambda e: e.dma_start(out=xp2[0:2, :], in_=xpv[i]), writes=[xp2])
            c.dma("sp", lambda e: e.dma_start(out=cs_t[:], in_=cso[i]), writes=[cs_t])
            norm_mod(2, xp2, hb2, st2, modA, modB)
            transpose_h(2, hb2, 0)
            norm_mod(128, xin, hb, st1, modA, modB)
            transpose_h(128, hb, 2)
            proc_tile(128, 130, xin, False, i)

        c.finish()
        print("instructions (approx):", c.ninst)
    return nc


def _tiles_of(core):
    return [core, 15 - core, 16 + core, 31 - core, 32 + core, 47 - core, 48 + core, 63 - core]


def _rope_table(pos):
    half = 32
    inv = (10000.0 ** (-np.arange(half, dtype=np.float32) / half)).astype(np.float32)
    ang = pos.astype(np.float32)[:, None] * inv[None, :]
    cos = np.cos(ang).astype(np.float32)
    sin = np.sin(ang).astype(np.float32)
    return np.concatenate([cos, cos, -sin, sin], axis=1).astype(np.float32)


_NC_CACHE = {}


def kernel(x_prompt, x_sample, c_prompt, c_sample, cache_kv, state_win, state_conv, page_table,
           w_ada, b_ada, norm1_g, norm2_g, w_in, conv_w, conv_b, cmp_pe, cmp_w1, cmp_b1, cmp_w2, cmp_b2,
           w_out, peer_wq, peer_keys, peer_u, peer_v, final_g):
    f32 = np.float32
    x_prompt = np.asarray(x_prompt, f32)
    x_sample = np.asarray(x_sample, f32)
    xp_t = x_prompt[0].reshape(64, 128, D)
    idn = np.eye(128, dtype=f32)
    css = _rope_table(SEQ + (np.arange(16) % 4))
    csa = np.stack([_rope_table(128 * t + np.arange(128)) for t in range(64)]).astype(f32)
    cscmp = np.stack([_rope_table(16 * (128 * H + np.arange(128)) + 31) for H in range(4)]).astype(f32)
    jj = np.arange(512)[:, None]
    sb_ = np.arange(128)[None, :]
    ov = ((16 * jj < 64 * (sb_ + 1)) & (16 * jj + 32 > 64 * sb_) & (jj <= 510)).astype(f32)
    ovm = np.ascontiguousarray(ov.reshape(4, 128, 128).transpose(1, 0, 2))
    piota = np.arange(128, dtype=f32).reshape(128, 1)
    cmask_sm = np.ones((128, 4, 4), f32)
    cmask_sm[127, 3, :] = 0.0
    fmask_sm = np.zeros((4, 128), f32)
    fmask_sm[:, 0] = 1.0e4
    fmask_sm[:, 127] = 1.0e4
    wmask_sm = np.ones((128, 4, 4), f32)
    for tq in range(4):
        wmask_sm[0:tq + 1, 0, tq] = 0.0
    nmask = np.zeros((16, NS, 4), f32)
    for b_ in range(NS):
        for t_ in range(4):
            for tq in range(4):
                if t_ <= tq:
                    nmask[4 * b_ + t_, b_, tq] = 1.0
    e32 = np.zeros((32, 16, 128), f32)
    for c_ in range(16):
        for k_ in range(128):
            e32[2 * c_ + k_ // 64, c_, k_] = 1.0
    common = {
        "piota": piota, "cmask_sm": cmask_sm, "fmask_sm": fmask_sm, "wmask_sm": wmask_sm, "nmask": nmask,
        "cache": np.ascontiguousarray(np.asarray(cache_kv, f32)[0].reshape(2560 * 128, 1024)),
        "idn": idn, "css": css, "xp": np.ascontiguousarray(xp_t), "csa": csa, "cscmp": cscmp, "ovm": ovm, "e32": e32,
        "cmp_pe": np.asarray(cmp_pe[0], f32), "cmp_w1": np.asarray(cmp_w1[0], f32), "cmp_b1": np.asarray(cmp_b1[0], f32),
        "cmp_w2": np.asarray(cmp_w2[0], f32), "cmp_b2": np.asarray(cmp_b2[0], f32),
        "w_ada": np.asarray(w_ada[0], f32), "b_ada": np.asarray(b_ada, f32).reshape(1, -1),
        "g1": np.asarray(norm1_g, f32).reshape(1, D), "g2": np.asarray(norm2_g, f32).reshape(1, D),
        "fg": np.asarray(final_g, f32).reshape(1, D),
        "w_in": np.asarray(w_in[0], f32), "conv_w": np.asarray(conv_w[0], f32), "conv_b": np.asarray(conv_b, f32).reshape(1, DC),
        "w_out": np.asarray(w_out[0], f32),
        "peer_wq": np.asarray(peer_wq[0], f32), "peer_keys": np.ascontiguousarray(np.asarray(peer_keys[0], f32).reshape(16, 128, 128)),
        "peer_u": np.asarray(peer_u[0], f32), "peer_v": np.asarray(peer_v[0], f32),
    }
    in_maps = []
    for core in range(NCORES):
        tl = _tiles_of(core)
        xo = np.ascontiguousarray(xp_t[tl])
        xpv = np.zeros((NT, 2, D), f32)
        pfl = np.ones((128, NT), f32)
        cso = np.zeros((NT, 128, 128), f32)
        for i, t in enumerate(tl):
            if t > 0:
                xpv[i] = x_prompt[0, 128 * t - 2:128 * t]
            else:
                pfl[:, i] = 0.0
            cso[i] = _rope_table(128 * t + np.arange(128))
        fmask = np.zeros((NT, 128, 128), f32)
        cmask = np.zeros((NT, 128, 4, 128), f32)
        dmask = np.zeros((NT, 128, 8, 128), f32)
        wmask = np.zeros((NT, 128, 12, 128), f32)
        qa = np.arange(128)
        for i, t in enumerate(tl):
            qpos = 128 * t + qa
            cur = qpos // 64
            blk = np.arange(128)[None, :]
            forced = (blk == 0) | (blk == cur[:, None]) | (blk == cur[:, None] - 1)
            valid = blk <= cur[:, None]
            fmask[i] = np.where(valid, np.where(forced, 1.0e4, 0.0), -1.0e30)
            for jc in range(4):
                j = 128 * jc + np.arange(128)
                cmask[i, :, jc, :] = ((16 * j[:, None] + 31 <= qpos[None, :]) & (j[:, None] <= 510))
            for d_ in range(8):
                kc = 8 * i + d_
                kpos = 128 * kc + np.arange(128)
                dmask[i, :, d_, :] = (kpos[:, None] <= qpos[None, :])
            for w_ in range(12):
                kc = 8 * i - 4 + w_
                if kc < 0:
                    continue
                kpos = 128 * kc + np.arange(128)
                dist = qpos[None, :] - kpos[:, None]
                wmask[i, :, w_, :] = ((dist >= 0) & (dist < 512))
        sq = slice(NS * core, NS * core + NS)
        m = dict(common)
        m.update({
            "xo": xo, "xpv": xpv, "pfl": pfl, "cso": cso, "fmask": fmask, "cmask": cmask, "dmask": dmask, "wmask": wmask,
            "xs": np.ascontiguousarray(x_sample[sq].reshape(16, D)),
            "ptab": np.ascontiguousarray(np.asarray(page_table)[sq].astype(np.int32)),
            "cc": np.ascontiguousarray(np.concatenate([np.asarray(c_prompt, f32), np.asarray(c_sample, f32)[sq]], axis=0)),
            "scv": np.ascontiguousarray(np.asarray(state_conv, f32)[0, sq].reshape(8, DC)),
            "swin": np.ascontiguousarray(np.asarray(state_win, f32)[0, sq].reshape(NS, 512, 512)),
        })
        in_maps.append(m)

    if "nc" not in _NC_CACHE:
        _NC_CACHE["nc"] = build_program()
    nc = _NC_CACHE["nc"]
    shp = getattr(nc, "_din_shapes", None)
    if shp is not None:
        in_maps = [{k: (m_[k] if tuple(m_[k].shape) == shp[k] else np.zeros(shp[k], f32)) for k in shp} for m_ in in_maps]
    res = run_bass_kernel_spmd(nc, in_maps, core_ids=list(range(NCORES)))
    R = res.results

    y_prompt = np.zeros((1, SEQ, D), f32)
    kv_rows_prompt = np.zeros((1, 1, SEQ, 4, 4, 64), f32)
    win_prompt = np.zeros((1, 1, 512, 2, 4, 64), f32)
    conv_prompt = np.zeros((1, 1, 2, DC), f32)
    y_sample = np.zeros((32, 4, D), f32)
    kv_rows_sample = np.zeros((1, 32, 4, 4, 4, 64), f32)
    win_sample = np.zeros((1, 32, 512, 2, 4, 64), f32)
    conv_sample = np.zeros((1, 32, 2, DC), f32)
    for core in range(NCORES):
        r = R[core]
        tl = _tiles_of(core)
        for i, t in enumerate(tl):
            y_prompt[0, 128 * t:128 * (t + 1)] = r["yo"][i]
            kv_rows_prompt[0, 0, 128 * t:128 * (t + 1)] = r["rows_o"][i].reshape(128, 4, 4, 64)
            if t >= 60:
                win_prompt[0, 0, 128 * (t - 60):128 * (t - 59)] = r["win_o"][i].reshape(128, 2, 4, 64)
            if t == 63:
                conv_prompt[0, 0] = r["conv_o"][i]
        sq = slice(NS * core, NS * core + NS)
        y_sample[sq] = r["ys"].reshape(NS, 4, D)
        kv_rows_sample[0, sq] = r["rows_s"].reshape(NS, 4, 4, 4, 64)
        win_sample[0, sq] = r["win_s"].reshape(NS, 512, 2, 4, 64)
        conv_sample[0, sq] = r["conv_s"]
    return (y_prompt, y_sample, kv_rows_prompt, kv_rows_sample, win_prompt, win_sample, conv_prompt, conv_sample)
```

```python
import contextlib
import numpy as np
import concourse.bass as bass
import concourse.mybir as mybir
from concourse.alu_op_type import AluOpType as ALU
from concourse.bass_utils import run_bass_kernel_spmd

AF = mybir.ActivationFunctionType
AX = mybir.AxisListType
F32 = mybir.dt.float32
BF16 = mybir.dt.bfloat16
I32 = mybir.dt.int32
U32 = mybir.dt.uint32

NCORES = 8
D = 2048
DC = 1024
NT = 8
SEQ = 8192
NS = 4
ST = 4
EPS = 1e-6
IN_COLS = 5680


class Res:
    __slots__ = ("w", "rs", "name")

    def __init__(self, name=""):
        self.w = None
        self.rs = []
        self.name = name


class T:
    def __init__(self, t, name=""):
        self.t = t
        self.r = Res(name)

    def __getitem__(self, k):
        return self.t[k]


class View:
    def __init__(self, ap, r):
        self.v = ap
        self.r = r.r if hasattr(r, "r") else r

    def __getitem__(self, k):
        return self.v[k]


class Ctx:
    NDMA = 8

    def __init__(self, nc, es):
        self.nc = nc
        self.es = es
        self.eng = {"pe": nc.tensor, "dve": nc.vector, "act": nc.scalar, "pool": nc.gpsimd, "sp": nc.sync}
        self.sem = {}
        self.cnt = {}
        for k in self.eng:
            self.sem[k] = es.enter_context(nc.semaphore("s_" + k))
            self.cnt[k] = 0
        self.dsem = {}
        self.dcnt = {}
        for q in ("sp", "pool", "act"):
            self.dsem[q] = [es.enter_context(nc.semaphore("d_%s%d" % (q, i))) for i in range(self.NDMA)]
            self.dcnt[q] = 0
        self.seen = {k: {} for k in self.eng}
        self.out_tokens = []
        self.ninst = 0

    def sb(self, name, shape, dt=F32):
        return T(self.es.enter_context(self.nc.sbuf_tensor(name, list(shape), dt)), name)

    def ps(self, name, shape, dt=F32):
        return T(self.es.enter_context(self.nc.psum_tensor(name, list(shape), dt)), name)

    def _wait(self, e, tok):
        if tok is None:
            return
        sem, val = tok
        key = id(sem)
        if self.seen[e].get(key, 0) >= val:
            return
        self.seen[e][key] = val
        self.eng[e].wait_ge(sem, val)
        self.ninst += 1

    @staticmethod
    def _res(x):
        return x.r if hasattr(x, "r") else x

    def _deps(self, e, reads, writes):
        own = self.sem[e]
        toks = []
        for r in reads:
            r = self._res(r)
            if r.w is not None:
                toks.append(r.w)
        for w in writes:
            w = self._res(w)
            if w.w is not None:
                toks.append(w.w)
            for t in w.rs:
                if t[0] is own:
                    continue
                toks.append(t)
        if e == "pe":
            toks = [t for t in toks if t[0] is not own]
        for t in toks:
            self._wait(e, t)

    def _commit(self, tok, reads, writes):
        for r in reads:
            r = self._res(r)
            r.rs.append(tok)
            if len(r.rs) > 96:
                r.rs = r.rs[-96:]
        for w in writes:
            w = self._res(w)
            w.w = tok
            w.rs = []

    def op(self, e, fn, reads=(), writes=()):
        self._deps(e, reads, writes)
        inst = fn(self.eng[e])
        self.cnt[e] += 1
        inst.then_inc(self.sem[e], 1)
        tok = (self.sem[e], self.cnt[e])
        self._commit(tok, reads, writes)
        self.ninst += 1
        return tok

    def dma(self, q, fn, reads=(), writes=(), is_output=False):
        i = self.dcnt[q]
        self.dcnt[q] += 1
        sem = self.dsem[q][i % self.NDMA]
        rnd = i // self.NDMA
        if rnd > 0:
            self._wait(q, (sem, 16 * rnd))
        self._deps(q, reads, writes)
        inst = fn(self.eng[q])
        inst.then_inc(sem, 16)
        tok = (sem, 16 * (rnd + 1))
        self._commit(tok, reads, writes)
        if is_output:
            self.out_tokens.append(tok)
        self.ninst += 1
        return tok

    def barrier(self):
        toks = [(self.sem[e], self.cnt[e]) for e in self.eng if self.cnt[e] > 0]
        for q in self.dsem:
            n = self.dcnt[q]
            for k in range(min(n, self.NDMA)):
                cntk = (n - k + self.NDMA - 1) // self.NDMA
                toks.append((self.dsem[q][k], 16 * cntk))
        for e in self.eng:
            for t in toks:
                self._wait(e, t)

    def finish(self):
        for tok in self.out_tokens:
            self._wait("sp", tok)
        for q in self.dsem:
            n = self.dcnt[q]
            for k in range(min(n, self.NDMA)):
                cntk = (n - k + self.NDMA - 1) // self.NDMA
                self._wait("sp", (self.dsem[q][k], 16 * cntk))
        for e in self.eng:
            if e != "sp" and self.cnt[e] > 0:
                self._wait("sp", (self.sem[e], self.cnt[e]))


def build_program():
    nc = bass.Bass("TRN2", target_bir_lowering=False)

    import os as _osd
    _DBG = _osd.environ.get("KDBG", "")
    _SKIP = set(_osd.environ.get("KSKIP", "").split(","))
    din_shapes = {}

    def din(name, shape, dt=F32):
        if _DBG and name in ("peer_u", "peer_v", "w_out", "peer_wq", "w_ada", "xo"):
            shape = [2, 2] if len(shape) == 2 else [2, 2, 2]
        din_shapes[name] = tuple(shape)
        return nc.dram_tensor(name, list(shape), dt, kind="ExternalInput").ap()

    def dout(name, shape, dt=F32):
        return nc.dram_tensor(name, list(shape), dt, kind="ExternalOutput").ap()

    xo = din("xo", [NT, 128, D])
    xpv = din("xpv", [NT, 2, D])
    pfl = din("pfl", [128, NT])
    cso = din("cso", [NT, 128, 128])
    css = din("css", [16, 128])
    xs = din("xs", [16, D])
    cc = din("cc", [5, D])
    idn = din("idn", [128, 128])
    scv = din("scv", [8, DC])
    swin = din("swin", [NS, 512, 512])
    w_ada = din("w_ada", [D, 6 * D])
    b_ada = din("b_ada", [1, 6 * D])
    g1 = din("g1", [1, D])
    g2 = din("g2", [1, D])
    fg = din("fg", [1, D])
    w_in = din("w_in", [D, IN_COLS])
    conv_w = din("conv_w", [3, DC])
    conv_b = din("conv_b", [1, DC])
    w_out = din("w_out", [D, D])
    peer_wq = din("peer_wq", [D, D])
    peer_keys = din("peer_keys", [16, 128, 128])
    peer_u = din("peer_u", [16384, D])
    peer_v = din("peer_v", [16384, D])
    xp = din("xp", [64, 128, D])
    csa = din("csa", [64, 128, 128])
    cscmp = din("cscmp", [4, 128, 128])
    ovm = din("ovm", [128, 4, 128])
    e32 = din("e32", [32, 16, 128])
    fmask = din("fmask", [NT, 128, 128])
    cmask = din("cmask", [NT, 128, 4, 128])
    dmask = din("dmask", [NT, 128, 8, 128])
    wmask = din("wmask", [NT, 128, 12, 128])
    cache = din("cache", [2560 * 128, 1024])
    ptab = din("ptab", [NS, 64], I32)
    piota = din("piota", [128, 1])
    cmask_sm = din("cmask_sm", [128, 4, 4])
    fmask_sm = din("fmask_sm", [4, 128])
    wmask_sm = din("wmask_sm", [128, 4, 4])
    nmask = din("nmask", [16, NS, 4])
    cmp_pe = din("cmp_pe", [2, 32, 64])
    cmp_w1 = din("cmp_w1", [2, 32, 64, 128])
    cmp_b1 = din("cmp_b1", [2, 128])
    cmp_w2 = din("cmp_w2", [2, 128, 64])
    cmp_b2 = din("cmp_b2", [2, 64])

    yo = dout("yo", [NT, 128, D])
    ys = dout("ys", [16, D])
    rows_o = dout("rows_o", [NT, 128, 1024])
    rows_s = dout("rows_s", [16, 1024])
    win_o = dout("win_o", [NT, 128, 512])
    win_s = dout("win_s", [NS, 512, 512])
    conv_o = dout("conv_o", [NT, 2, DC])
    conv_s = dout("conv_s", [NS, 2, DC])

    m_dram = nc.dram_tensor("m_dram", [5, 6 * D], F32, kind="Internal").ap()
    m_res = Res("m_dram")
    kTs_d = nc.dram_tensor("kTs_d", [128, 2, SEQ], BF16, kind="Internal").ap()
    kTw_d = nc.dram_tensor("kTw_d", [128, 2, SEQ], BF16, kind="Internal").ap()
    vs_d = nc.dram_tensor("vs_d", [4, 128, 64, 65], BF16, kind="Internal").ap()
    vw_d = nc.dram_tensor("vw_d", [4, 128, 64, 65], BF16, kind="Internal").ap()
    kv_res = Res("kv_scratch")
    kTs_s = nc.dram_tensor("kTs_s", [NS, 128, 2, SEQ], BF16, kind="Internal").ap()
    vs_s = nc.dram_tensor("vs_s", [NS, 4, 128, 64, 65], BF16, kind="Internal").ap()
    kTw_s = nc.dram_tensor("kTw_s", [NS, 128, 2, 512], BF16, kind="Internal").ap()
    vw_s = nc.dram_tensor("vw_s", [NS, 4, 128, 4, 65], BF16, kind="Internal").ap()
    kc_s = nc.dram_tensor("kc_s", [NS, 128, 2, 512], BF16, kind="Internal").ap()
    vc_s = nc.dram_tensor("vc_s", [NS, 128, 4, 4, 65], BF16, kind="Internal").ap()
    w_in_v = w_in.rearrange("(c p) n -> p c n", p=128)
    w_out_v = None if _DBG else w_out.rearrange("(c p) n -> p c n", p=128)
    w_ada_v = None if _DBG else w_ada.rearrange("(c p) n -> p c n", p=128)
    wq_v = None if _DBG else peer_wq.rearrange("(c p) n -> p c n", p=128)
    nc._din_shapes = din_shapes

    with contextlib.ExitStack() as es:
        c = Ctx(nc, es)
        idf = c.sb("idf", [128, 128])
        idb = c.sb("idb", [128, 128], BF16)
        zer = c.sb("zer", [128, 512], BF16)
        pfl_t = c.sb("pfl_t", [128, NT])
        cwT = c.sb("cwT", [128, 8, 3])
        cbT = c.sb("cbT", [128, 8])
        h2f = c.sb("h2f", [128, D])
        facc = c.sb("facc", [128, D])
        t12 = [c.sb("t12_%d" % i, [128, 16]) for i in range(2)]
        i12 = [c.sb("i12_%d" % i, [128, 16], U32) for i in range(2)]
        if12 = [c.sb("if12_%d" % i, [128, 16]) for i in range(2)]
        tmp128 = c.sb("tmp128", [128, 128])
        cand = c.sb("cand", [128, 256])
        cidx = c.sb("cidx", [128, 256])
        tmp256 = c.sb("tmp256", [128, 256])
        c16 = c.sb("c16", [128, 16])
        pst = c.sb("pst", [128, 4])
        ef = c.sb("ef", [128, 128])
        eu = c.sb("eu", [128, 128], U32)
        gw = c.sb("gw", [128, 128])
        adot = c.sb("adot", [128, 128])
        coef = c.sb("coef", [128, 128])
        modA = c.sb("modA", [128, D])
        modB = c.sb("modB", [128, D])
        modG = c.sb("modG", [128, D])
        ccT = c.sb("ccT", [128, 16, 5], BF16)
        xt1 = c.sb("xt", [128, D])
        xt = [xt1, xt1]
        junk = c.sb("junk", [128, D])
        yt = c.sb("yt", [128, D])
        hb = c.sb("hb", [128, D], BF16)
        st1 = c.sb("st1", [128, 4])
        st2 = c.sb("st2", [2, 4])
        hT = c.sb("hT", [128, 16, 130], BF16)
        cs_t = c.sb("cs_t", [128, 128])
        kcT = c.sb("kcT", [128, 2, 512], BF16)
        vc1 = c.sb("vc1", [128, 4, 4, 65], BF16)
        ov_sb = c.sb("ov_sb", [128, 4, 128], BF16)
        m_bk = [View(h2f[0:5, i * 512:(i + 1) * 512], Res("m_bk")) for i in range(2)]
        b_bk = [View(h2f[0:5, 1024 + i * 512:1024 + (i + 1) * 512], Res("b_bk")) for i in range(2)]
        pt_i = c.sb("pt_i", [128, 64], I32)
        idx_f = c.sb("idx_f", [128, 64])
        idx_u = c.sb("idx_u", [128, 64], U32)
        pio_t = c.sb("pio_t", [128, 1])
        ropa = View(h2f[:, 0:1024], h2f)
        ropb = View(h2f[:, 1024:2048], h2f)
        rows_t = View(yt[:, 0:1024], yt)
        win_t = View(yt[:, 1024:1536], yt)
        x1 = xt1
        xp2 = yt
        hb2 = hb
        cc_t = xt1
        cc_s = junk
        scv_t = View(junk[0:8, 0:DC], junk)

        pT = [c.ps("pT%d" % i, [128, 8, 128], BF16) for i in range(2)]
        pc_full = [c.ps("pc%d" % i, [128, 512]) for i in range(2)]

        class _PCV:
            def __init__(self, t):
                self.r = t.r
                self.v = t[:, 0:260].rearrange("p (a b) -> p a b", b=130)

            def __getitem__(self, k):
                return self.v[k]

        pc = [_PCV(t) for t in pc_full]
        pq = [c.ps("pq%d" % i, [128, 512]) for i in range(4)]
        pT1f = View(pT[1][:].rearrange("p a b -> p (a b)").bitcast(F32), pT[1])

        state = {"wb": 0, "pq": 0, "pc": 0, "wbufs": None}

        def next_wb():
            wl = state["wbufs"]
            b = wl[state["wb"] % 2]
            state["wb"] += 1
            return b

        def next_pq():
            b = pq[state["pq"] % 4]
            state["pq"] += 1
            return b

        def next_pc():
            b = pc[state["pc"] % 2]
            state["pc"] += 1
            return b

        def load_w(view, c0, ncol):
            b = next_wb()
            c.dma("pool", lambda e: e.dma_start(out=b[:, :, 0:ncol], in_=view[:, :, c0:c0 + ncol]), writes=[b, b.r2])
            return b

        esA = contextlib.ExitStack()
        c.es = esA
        wkv = c.sb("wkv", [128, 16, 1024], BF16)
        wkv2 = c.sb("wkv2", [128, 16, 512], BF16)
        rawT = [c.sb("rawT%d" % i, [128, 4, 2064], BF16) for i in range(2)]
        kTs_st = c.sb("kTs_st", [128, 2, 1024], BF16)
        kTw_st = c.sb("kTw_st", [128, 2, 1024], BF16)
        vs_st = c.sb("vs_st", [128, 4, 8, 65], BF16)
        vw_st = c.sb("vw_st", [128, 4, 8, 65], BF16)
        ks_b = c.sb("ks_b", [128, 256], BF16)
        ks_b2 = c.sb("ks_b2", [128, 256], BF16)
        w1_sb = c.sb("w1_sb", [128, 2, 32, 128], BF16)
        peT = c.sb("peT", [128, 2, 34], BF16)
        w2_sb = c.sb("w2_sb", [128, 2, 64], BF16)
        b1T = c.sb("b1T", [128, 2])
        b2bc = c.sb("b2bc", [128, 2, 64])
        biasH = c.sb("biasH", [128, 2])
        hidT = c.sb("hidT", [128, 128], BF16)
        hidT2 = c.sb("hidT2", [128, 128], BF16)
        kc_tok = c.sb("kc_tok", [128, 4, 64])
        kcr = c.sb("kcr", [128, 256], BF16)
        c.es = es

        class _WV:
            def __init__(self, t):
                self.v = t[:].rearrange("p a b -> p (a b)")[:, 0:8192].rearrange("p (k n) -> p k n", n=512)
                self.r = t.r
                self.r2 = Res("dummy")

            def __getitem__(self, k):
                return self.v[k]

        state["wbufs"] = [_WV(rawT[0]), _WV(rawT[1])]

        c.dma("sp", lambda e: e.dma_start(out=idf[:], in_=idn), writes=[idf])
        c.dma("sp", lambda e: e.dma_start(out=pfl_t[:], in_=pfl), writes=[pfl_t])
        for k_ in range(3):
            c.dma("sp", lambda e: e.dma_start(out=cwT[:, :, k_], in_=conv_w[k_].rearrange("(c p) -> p c", p=128),
                                              allow_slow_non_contiguous=True), writes=[cwT])
        c.dma("sp", lambda e: e.dma_start(out=cbT[:], in_=conv_b.rearrange("o (c p) -> p (o c)", p=128),
                                          allow_slow_non_contiguous=True), writes=[cbT])
        c.op("pool", lambda e: e.memset(zer[:], 0.0), writes=[zer])
        c.dma("sp", lambda e: e.dma_start(out=cc_t[0:5, :], in_=cc), writes=[cc_t])
        c.op("dve", lambda e: e.tensor_copy(out=idb[:], in_=idf[:]), reads=[idf], writes=[idb])

        c.op("act", lambda e: e.activation(out=cc_s[0:5, :], in_=cc_t[0:5, :], func=AF.Silu), reads=[cc_t], writes=[cc_s])
        pa = next_pq()
        for k in range(16):
            c.op("pe", lambda e: e.transpose(out=pa[:, k * 5:(k + 1) * 5], in_=cc_s[0:5, k * 128:(k + 1) * 128],
                                             identity=idf[0:5, 0:5]), reads=[cc_s, idf], writes=[pa])
        c.op("dve", lambda e: e.tensor_copy(out=ccT[:].rearrange("p a b -> p (a b)"), in_=pa[:, 0:80]),
             reads=[pa], writes=[ccT])
        for n in range(0 if _DBG else 24):
            b = load_w(w_ada_v, n * 512, 512)
            p = next_pq()
            for k in range(16):
                c.op("pe", lambda e: e.matmul(p[0:5, :], lhsT=ccT[:, k, :], rhs=b[:, k, :], start=(k == 0), stop=(k == 15)),
                     reads=[ccT, b, b.r2], writes=[p])
            bb = b_bk[n % 2]
            mb = m_bk[n % 2]
            c.dma("sp", lambda e: e.dma_start(out=bb[:, :], in_=b_ada[:, n * 512:(n + 1) * 512].partition_broadcast(5)), writes=[bb])
            c.op("dve", lambda e: e.tensor_tensor(out=mb[:, :], in0=p[0:5, :], in1=bb[:, :], op=ALU.add), reads=[p, bb], writes=[mb])
            c.dma("sp", lambda e: e.dma_start(out=m_dram[:, n * 512:(n + 1) * 512], in_=mb[:, :]), reads=[mb], writes=[m_res])

        c.barrier()
        import os as _os0
        if _os0.environ.get("KDBG", "") == "0":
            c.finish()
            esA.close()
            return nc

        def gen_mod(dst, j, P, col0, kind, gain=None):
            tgt = dst if kind == "plain" else junk
            if P == 128:
                c.dma("sp", lambda e: e.dma_start(out=tgt[:, :], in_=m_dram[0:1, j * D:(j + 1) * D].partition_broadcast(128)),
                      reads=[m_res], writes=[tgt])
            else:
                for s_ in range(NS):
                    c.dma("sp", lambda e: e.dma_start(out=tgt[4 * s_:4 * s_ + 4, :],
                                                      in_=m_dram[1 + s_:2 + s_, j * D:(j + 1) * D].partition_broadcast(4)),
                          reads=[m_res], writes=[tgt])
            if kind != "plain":
                c.dma("sp", lambda e: e.dma_start(out=facc[0:P, :], in_=gain.partition_broadcast(P)), writes=[facc])
                c.op("dve", lambda e: e.scalar_tensor_tensor(out=dst[0:P, :], in0=junk[0:P, :], scalar=1.0, in1=facc[0:P, :],
                                                              op0=ALU.add, op1=ALU.mult), reads=[junk, facc], writes=[dst])

        def norm_mod(P, xin, hout, stt, A, B):
            c.op("act", lambda e: e.activation(out=junk[0:P, :], in_=xin[0:P, :], func=AF.Square, accum_out=stt[0:P, 0:1]),
                 reads=[xin], writes=[junk, stt])
            c.op("dve", lambda e: e.tensor_scalar(out=stt[0:P, 1:2], in0=stt[0:P, 0:1], scalar1=1.0 / D, scalar2=EPS,
                                                  op0=ALU.mult, op1=ALU.add), reads=[stt], writes=[stt])
            c.op("act", lambda e: e.activation(out=stt[0:P, 2:3], in_=stt[0:P, 1:2], func=AF.Sqrt), reads=[stt], writes=[stt])
            c.op("dve", lambda e: e.reciprocal(out=stt[0:P, 3:4], in_=stt[0:P, 2:3]), reads=[stt], writes=[stt])
            c.op("dve", lambda e: e.scalar_tensor_tensor(out=junk[0:P, :], in0=xin[0:P, :], scalar=stt[0:P, 3:4], in1=A[0:P, :],
                                                          op0=ALU.mult, op1=ALU.mult), reads=[xin, stt, A], writes=[junk])
            c.op("dve", lambda e: e.tensor_tensor(out=hout[0:P, :], in0=junk[0:P, :], in1=B[0:P, :], op=ALU.add),
                 reads=[junk, B], writes=[hout])

        def transpose_h(P, hsrc, col0):
            for half in range(2):
                for k in range(8):
                    kk = half * 8 + k
                    c.op("pe", lambda e: e.transpose(out=pT[half][:, k, 0:P], in_=hsrc[0:P, kk * 128:(kk + 1) * 128],
                                                     identity=idb[0:P, 0:P]), reads=[hsrc, idb], writes=[pT[half]])
                eng = "act" if half == 0 else "dve"
                if eng == "act":
                    c.op("act", lambda e: e.copy(out=hT[:, half * 8:(half + 1) * 8, col0:col0 + P], in_=pT[half][:, :, 0:P]),
                         reads=[pT[half]], writes=[hT])
                else:
                    c.op("dve", lambda e: e.tensor_copy(out=hT[:, half * 8:(half + 1) * 8, col0:col0 + P], in_=pT[half][:, :, 0:P]),
                         reads=[pT[half]], writes=[hT])

        def rope_into(P, src, nh, cst, dst):
            s3 = src[0:P, 0:nh * 64].rearrange("p (h d) -> p h d", d=64)
            a3 = ropa[0:P, 0:nh * 64].rearrange("p (h d) -> p h d", d=64)
            b3 = ropb[0:P, 0:nh * 64].rearrange("p (h d) -> p h d", d=64)
            d3 = dst.rearrange("p (h d) -> p h d", d=64)
            cos2 = cst[0:P, 0:64].unsqueeze(1).broadcast_to([P, nh, 64])
            nsin = cst[0:P, 64:96].unsqueeze(1).broadcast_to([P, nh, 32])
            psin = cst[0:P, 96:128].unsqueeze(1).broadcast_to([P, nh, 32])
            c.op("dve", lambda e: e.tensor_tensor(out=a3, in0=s3, in1=cos2, op=ALU.mult), reads=[src, cst], writes=[ropa])
            c.op("dve", lambda e: e.tensor_tensor(out=b3[:, :, 0:32], in0=s3[:, :, 32:64], in1=nsin, op=ALU.mult),
                 reads=[src, cst], writes=[ropb])
            c.op("dve", lambda e: e.tensor_tensor(out=b3[:, :, 32:64], in0=s3[:, :, 0:32], in1=psin, op=ALU.mult),
                 reads=[src, cst], writes=[ropb])
            return a3, b3, d3

        def proc_tile(P, NTOK, xin, is_sample, slot):
            off = NTOK - P
            for bank in range(6):
                b = load_w(w_in_v, bank * 512, 512)
                for half in range(2):
                    p = next_pc()
                    for jj in range(2):
                        j4 = half * 2 + jj
                        for k in range(16):
                            c.op("pe", lambda e: e.matmul(p[:, jj, 0:NTOK], lhsT=b[:, k, j4 * 128:(j4 + 1) * 128],
                                                          rhs=hT[:, k, 0:NTOK], start=(k == 0), stop=(k == 15)),
                                 reads=[b, b.r2, hT], writes=[p])
                    j0 = (bank % 2) * 4 + half * 2
                    if bank < 2:
                        c.op("act", lambda e: e.copy(out=bgT[:, j0:j0 + 2, 0:P], in_=p[:, :, off:NTOK]), reads=[p], writes=[bgT])
                    elif bank < 4:
                        c.op("act", lambda e: e.copy(out=cgT[:, j0:j0 + 2, 0:NTOK], in_=p[:, :, 0:NTOK]), reads=[p], writes=[cgT])
                    else:
                        c.op("dve", lambda e: e.tensor_tensor(out=zcT[:, j0:j0 + 2, 0:NTOK], in0=p[:, :, 0:NTOK],
                                                              in1=cgT[:, j0:j0 + 2, 0:NTOK], op=ALU.mult),
                             reads=[p, cgT], writes=[zcT])
            if not is_sample:
                c.op("dve", lambda e: e.tensor_scalar(out=zcT[:, :, 0:2], in0=zcT[:, :, 0:2], scalar1=pfl_t[:, slot:slot + 1],
                                                      scalar2=None, op0=ALU.mult), reads=[zcT, pfl_t], writes=[zcT])
                for j in range(8):
                    c.op("dve", lambda e: e.tensor_scalar(out=acc[:, :], in0=zcT[:, j, 2:130], scalar1=cwT[:, j, 2:3],
                                                          scalar2=cbT[:, j:j + 1], op0=ALU.mult, op1=ALU.add),
                         reads=[zcT, cwT, cbT], writes=[acc])
                    c.op("dve", lambda e: e.scalar_tensor_tensor(out=acc[:, :], in0=zcT[:, j, 1:129], scalar=cwT[:, j, 1:2],
                                                                  in1=acc[:, :], op0=ALU.mult, op1=ALU.add),
                         reads=[zcT, cwT, acc], writes=[acc])
                    c.op("dve", lambda e: e.scalar_tensor_tensor(out=acc[:, :], in0=zcT[:, j, 0:128], scalar=cwT[:, j, 0:1],
                                                                  in1=acc[:, :], op0=ALU.mult, op1=ALU.add),
                         reads=[zcT, cwT, acc], writes=[acc])
                    c.op("dve", lambda e: e.tensor_tensor(out=catT[:, j, :], in0=acc[:, :], in1=bgT[:, j, :], op=ALU.mult),
                         reads=[acc, bgT], writes=[catT])
                for t_ in range(2):
                    c.dma("sp", lambda e: e.dma_start(out=conv_o[slot, t_].rearrange("(c p) -> p c", p=128), in_=zcT[:, :, 128 + t_],
                                                      allow_slow_non_contiguous=True), reads=[zcT], is_output=True)
            else:
                c.op("dve", lambda e: e.tensor_copy(out=sctx[:, :, :, 2:6],
                                                    in_=zcT[:, :, 0:16].rearrange("p c (s t) -> p c s t", t=4)),
                     reads=[zcT], writes=[sctx])
                for j in range(8):
                    c.op("dve", lambda e: e.tensor_scalar(out=sacc[:, :, :], in0=sctx[:, j, :, 2:6], scalar1=cwT[:, j, 2:3],
                                                          scalar2=cbT[:, j:j + 1], op0=ALU.mult, op1=ALU.add),
                         reads=[sctx, cwT, cbT], writes=[sacc])
                    c.op("dve", lambda e: e.scalar_tensor_tensor(out=sacc[:, :, :], in0=sctx[:, j, :, 1:5], scalar=cwT[:, j, 1:2],
                                                                  in1=sacc[:, :, :], op0=ALU.mult, op1=ALU.add),
                         reads=[sctx, cwT, sacc], writes=[sacc])
                    c.op("dve", lambda e: e.scalar_tensor_tensor(out=sacc[:, :, :], in0=sctx[:, j, :, 0:4], scalar=cwT[:, j, 0:1],
                                                                  in1=sacc[:, :, :], op0=ALU.mult, op1=ALU.add),
                         reads=[sctx, cwT, sacc], writes=[sacc])
                    c.op("dve", lambda e: e.tensor_tensor(out=catT[:, j, 0:16].rearrange("p (s t) -> p s t", t=4), in0=sacc[:, :, :],
                                                          in1=bgT[:, j, 0:16].rearrange("p (s t) -> p s t", t=4), op=ALU.mult),
                         reads=[sacc, bgT], writes=[catT])
                for s_ in range(NS):
                    for t_ in range(2):
                        c.dma("sp", lambda e: e.dma_start(out=conv_s[s_, t_].rearrange("(c p) -> p c", p=128), in_=sctx[:, :, s_, 4 + t_],
                                                          allow_slow_non_contiguous=True), reads=[sctx], is_output=True)

            cst = cs_t
            for bank in range(2):
                b = load_w(w_in_v, 3072 + bank * 512, 512)
                p = next_pq()
                for k in range(16):
                    c.op("pe", lambda e: e.matmul(p[0:P, :], lhsT=hT[:, k, off:NTOK], rhs=b[:, k, :], start=(k == 0), stop=(k == 15)),
                         reads=[hT, b, b.r2], writes=[p])
                a3, b3, d3 = rope_into(P, p, 8, cst, q_r[0:P, bank * 512:(bank + 1) * 512])
                d4 = q_r[0:P, bank * 512:(bank + 1) * 512].rearrange("p (r f d) -> p f r d", r=4, f=2, d=64)
                c.op("dve", lambda e: e.tensor_tensor(out=d4, in0=a3.rearrange("p (f r) d -> p f r d", f=2),
                                                      in1=b3.rearrange("p (f r) d -> p f r d", f=2), op=ALU.add),
                     reads=[ropa, ropb], writes=[q_r])
            for bank in range(3):
                b = load_w(w_in_v, 4096 + bank * 512, 512)
                p = next_pq()
                for k in range(16):
                    c.op("pe", lambda e: e.matmul(p[0:P, :], lhsT=hT[:, k, off:NTOK], rhs=b[:, k, :], start=(k == 0), stop=(k == 15)),
                         reads=[hT, b, b.r2], writes=[p])
                if bank == 0:
                    c.op("act", lambda e: e.copy(out=rows_t[0:P, 0:512], in_=p[0:P, :]), reads=[p], writes=[rows_t])
                elif bank == 1:
                    a3, b3, d3 = rope_into(P, p, 4, cst, rows_t[0:P, 512:768])
                    c.op("dve", lambda e: e.tensor_tensor(out=d3, in0=a3, in1=b3, op=ALU.add), reads=[ropa, ropb], writes=[rows_t])
                    c.op("act", lambda e: e.copy(out=rows_t[0:P, 768:1024], in_=p[0:P, 256:512]), reads=[p], writes=[rows_t])
                else:
                    a3, b3, d3 = rope_into(P, p, 4, cst, win_t[0:P, 0:256])
                    c.op("dve", lambda e: e.tensor_tensor(out=d3, in0=a3, in1=b3, op=ALU.add), reads=[ropa, ropb], writes=[win_t])
                    c.op("act", lambda e: e.copy(out=win_t[0:P, 256:512], in_=p[0:P, 256:512]), reads=[p], writes=[win_t])
            if not is_sample:
                c.dma("sp", lambda e: e.dma_start(out=rows_o[slot], in_=rows_t[:]), reads=[rows_t], is_output=True)
                c.dma("sp", lambda e: e.dma_start(out=win_o[slot], in_=win_t[:]), reads=[win_t], is_output=True)
            else:
                c.dma("sp", lambda e: e.dma_start(out=rows_s, in_=rows_t[0:16, :]), reads=[rows_t], is_output=True)
                for s_ in range(NS):
                    c.dma("sp", lambda e: e.dma_start(out=win_s[s_, 508:512, :], in_=win_t[4 * s_:4 * s_ + 4, :]),
                          reads=[win_t], is_output=True)

            b = load_w(w_in_v, 5632, 48)
            p = next_pq()
            for k in range(16):
                c.op("pe", lambda e: e.matmul(p[0:P, 0:48], lhsT=hT[:, k, off:NTOK], rhs=b[:, k, 0:48], start=(k == 0), stop=(k == 15)),
                     reads=[hT, b, b.r2], writes=[p])
            c.op("act", lambda e: e.activation(out=gates_t[0:P, :], in_=p[0:P, 0:48], func=AF.Sigmoid), reads=[p], writes=[gates_t])
            if is_sample:
                if "phaseS" in _SKIP:
                    c.op("pool", lambda e: e.memset(catT[:, 8:16, :], 0.0), writes=[catT])
                else:
                    attention_sample()
            else:
                attention_prompt(slot)

            for n in range(4):
                b = load_w(w_out_v, n * 512, 512)
                p = next_pq()
                for k in range(16):
                    c.op("pe", lambda e: e.matmul(p[0:P, :], lhsT=catT[:, k, 0:P], rhs=b[:, k, :], start=(k == 0), stop=(k == 15)),
                         reads=[catT, b, b.r2], writes=[p])
                c.op("dve", lambda e: e.tensor_tensor(out=junk[0:P, n * 512:(n + 1) * 512], in0=p[0:P, :],
                                                      in1=modG[0:P, n * 512:(n + 1) * 512], op=ALU.mult), reads=[p, modG], writes=[junk])
                c.op("dve", lambda e: e.tensor_tensor(out=x1[0:P, n * 512:(n + 1) * 512], in0=junk[0:P, n * 512:(n + 1) * 512],
                                                       in1=xin[0:P, n * 512:(n + 1) * 512], op=ALU.add), reads=[junk, xin], writes=[x1])
            col0 = 128 if is_sample else 0
            gen_mod(modA, 4, P, col0, "scale", gain=g2)
            gen_mod(modB, 3, P, col0, "plain")
            gen_mod(modG, 5, P, col0, "plain")
            norm_mod(P, x1, h2f, st1, modA, modB)
            c.op("act", lambda e: e.copy(out=hb[0:P, :], in_=h2f[0:P, :]), reads=[h2f], writes=[hb])
            transpose_h(P, hb, 0)
            qT = catT
            for n in range(4):
                b = load_w(wq_v, n * 512, 512)
                for half in range(2):
                    p = next_pc()
                    for jj in range(2):
                        j4 = half * 2 + jj
                        for k in range(16):
                            c.op("pe", lambda e: e.matmul(p[:, jj, 0:P], lhsT=b[:, k, j4 * 128:(j4 + 1) * 128],
                                                          rhs=hT[:, k, 0:P], start=(k == 0), stop=(k == 15)),
                                 reads=[b, b.r2, hT], writes=[p])
                    j0 = n * 4 + half * 2
                    c.op("act", lambda e: e.copy(out=qT[:, j0:j0 + 2, 0:P], in_=p[:, :, 0:P]), reads=[p], writes=[qT])
            S = junk
            S3 = junk[:].rearrange("p (j k) -> p j k", k=128)
            for n in range(4):
                p = next_pq()
                for jj in range(4):
                    j = n * 4 + jj
                    c.op("pe", lambda e: e.matmul(p[0:P, jj * 128:(jj + 1) * 128], lhsT=qT[:, j, 0:P], rhs=keysT[:, j, :],
                                                  start=True, stop=True), reads=[qT, keysT], writes=[p])
                c.op("act", lambda e: e.copy(out=junk[0:P, n * 512:(n + 1) * 512], in_=p[0:P, :]), reads=[p], writes=[S])
            for hp in range(8):
                for ci in range(2):
                    tt, ii = t12[ci], i12[ci]
                    src = S3[0:P, 2 * hp + ci, :]
                    c.op("dve", lambda e: e.max(out=tt[0:P, 0:8], in_=src), reads=[S], writes=[tt])
                    c.op("dve", lambda e: e.max_index(out=ii[0:P, 0:8], in_max=tt[0:P, 0:8], in_values=src), reads=[S, tt], writes=[ii])
                    c.op("dve", lambda e: e.match_replace(out=tmp128[0:P, :], in_to_replace=tt[0:P, 0:8], in_values=src, imm_value=-1e30),
                         reads=[S, tt], writes=[tmp128])
                    c.op("dve", lambda e: e.max(out=tt[0:P, 8:16], in_=tmp128[0:P, :]), reads=[tmp128], writes=[tt])
                    c.op("dve", lambda e: e.max_index(out=ii[0:P, 8:16], in_max=tt[0:P, 8:16], in_values=tmp128[0:P, :]),
                         reads=[tmp128, tt], writes=[ii])
                c.op("dve", lambda e: e.tensor_scalar(out=if12[0][0:P, :], in0=i12[0][0:P, :], scalar1=128.0, scalar2=None, op0=ALU.mult),
                     reads=[i12[0]], writes=[if12[0]])
                c.op("dve", lambda e: e.tensor_copy(out=if12[1][0:P, :], in_=i12[1][0:P, :]), reads=[i12[1]], writes=[if12[1]])
                cand3 = cand[0:P, :].rearrange("p (a b) -> p a b", b=16)
                cidx3 = cidx[0:P, :].rearrange("p (a b) -> p a b", b=16)
                c.op("dve", lambda e: e.tensor_tensor(out=cand3, in0=t12[0][0:P, :].unsqueeze(2).broadcast_to([P, 16, 16]),
                                                      in1=t12[1][0:P, :].unsqueeze(1).broadcast_to([P, 16, 16]), op=ALU.add),
                     reads=[t12[0], t12[1]], writes=[cand])
                c.op("dve", lambda e: e.tensor_tensor(out=cidx3, in0=if12[0][0:P, :].unsqueeze(2).broadcast_to([P, 16, 16]),
                                                      in1=if12[1][0:P, :].unsqueeze(1).broadcast_to([P, 16, 16]), op=ALU.add),
                     reads=[if12[0], if12[1]], writes=[cidx])
                c.op("dve", lambda e: e.max(out=c16[0:P, 0:8], in_=cand[0:P, :]), reads=[cand], writes=[c16])
                c.op("dve", lambda e: e.match_replace(out=tmp256[0:P, :], in_to_replace=c16[0:P, 0:8], in_values=cand[0:P, :], imm_value=-1e30),
                     reads=[cand, c16], writes=[tmp256])
                c.op("dve", lambda e: e.max(out=c16[0:P, 8:16], in_=tmp256[0:P, :]), reads=[tmp256], writes=[c16])
                for k in range(16):
                    c.op("dve", lambda e: e.scalar_tensor_tensor(out=tmp256[0:P, :], in0=cand[0:P, :], scalar=c16[0:P, k:k + 1],
                                                                  in1=cidx[0:P, :], op0=ALU.is_equal, op1=ALU.mult),
                         reads=[cand, c16, cidx], writes=[tmp256])
                    c.op("dve", lambda e: e.tensor_reduce(out=ef[0:P, hp * 16 + k:hp * 16 + k + 1], in_=tmp256[0:P, :], axis=AX.X, op=ALU.add),
                         reads=[tmp256], writes=[ef])
                c.op("dve", lambda e: e.tensor_scalar(out=pst[0:P, 0:1], in0=c16[0:P, 0:1], scalar1=-1.0, scalar2=None, op0=ALU.mult),
                     reads=[c16], writes=[pst])
                c.op("act", lambda e: e.activation(out=gw[0:P, hp * 16:(hp + 1) * 16], in_=c16[0:P, :], func=AF.Exp,
                                                   bias=pst[0:P, 0:1], scale=1.0, accum_out=pst[0:P, 1:2]),
                     reads=[c16, pst], writes=[gw, pst])
                c.op("dve", lambda e: e.reciprocal(out=pst[0:P, 2:3], in_=pst[0:P, 1:2]), reads=[pst], writes=[pst])
                c.op("dve", lambda e: e.tensor_scalar(out=gw[0:P, hp * 16:(hp + 1) * 16], in0=gw[0:P, hp * 16:(hp + 1) * 16],
                                                      scalar1=pst[0:P, 2:3], scalar2=None, op0=ALU.mult), reads=[gw, pst], writes=[gw])
            c.op("dve", lambda e: e.tensor_scalar(out=ef[0:P, :], in0=ef[0:P, :], scalar1=0.0, scalar2=16383.0, op0=ALU.max, op1=ALU.min),
                 reads=[ef], writes=[ef])
            c.op("dve", lambda e: e.tensor_copy(out=eu[0:P, :], in_=ef[0:P, :]), reads=[ef], writes=[eu])
            for j in range(128):
                U = (Ub + Vb)[j % 4]
                c.dma("pool", lambda e: e.indirect_dma_start(out=U[0:P, :], out_offset=None, in_=peer_u[:, :],
                                                             in_offset=bass.IndirectOffsetOnAxis(ap=eu[0:P, j:j + 1], axis=0)),
                      reads=[eu], writes=[U])
                prod = yt if j % 2 == 0 else junk
                c.op("dve", lambda e: e.tensor_tensor(out=prod[0:P, :], in0=U[0:P, :], in1=h2f[0:P, :], op=ALU.mult),
                     reads=[U, h2f], writes=[prod])
                c.op("act", lambda e: e.activation(out=prod[0:P, :], in_=prod[0:P, :], func=AF.Identity, accum_out=adot[0:P, j:j + 1]),
                     reads=[prod], writes=[prod, adot])
            c.op("act", lambda e: e.activation(out=coef[0:P, :], in_=adot[0:P, :], func=AF.Gelu_apprx_tanh), reads=[adot], writes=[coef])
            c.op("dve", lambda e: e.tensor_tensor(out=coef[0:P, :], in0=coef[0:P, :], in1=gw[0:P, :], op=ALU.mult),
                 reads=[coef, gw], writes=[coef])
            for j in range(128):
                V = (Vb + Ub)[j % 4]
                c.dma("pool", lambda e: e.indirect_dma_start(out=V[0:P, :], out_offset=None, in_=peer_v[:, :],
                                                             in_offset=bass.IndirectOffsetOnAxis(ap=eu[0:P, j:j + 1], axis=0)),
                      reads=[eu], writes=[V])
                if j == 0:
                    c.op("dve", lambda e: e.tensor_scalar(out=facc[0:P, :], in0=V[0:P, :], scalar1=coef[0:P, 0:1], scalar2=None, op0=ALU.mult),
                         reads=[V, coef], writes=[facc])
                else:
                    c.op("dve", lambda e: e.scalar_tensor_tensor(out=facc[0:P, :], in0=V[0:P, :], scalar=coef[0:P, j:j + 1], in1=facc[0:P, :],
                                                                  op0=ALU.mult, op1=ALU.add), reads=[V, coef, facc], writes=[facc])
            c.op("dve", lambda e: e.tensor_tensor(out=facc[0:P, :], in0=facc[0:P, :], in1=modG[0:P, :], op=ALU.mult),
                 reads=[facc, modG], writes=[facc])
            c.op("dve", lambda e: e.tensor_tensor(out=x1[0:P, :], in0=x1[0:P, :], in1=facc[0:P, :], op=ALU.add),
                 reads=[x1, facc], writes=[x1])
            stt = st1
            c.op("act", lambda e: e.activation(out=junk[0:P, :], in_=x1[0:P, :], func=AF.Square, accum_out=stt[0:P, 0:1]),
                 reads=[x1], writes=[junk, stt])
            c.op("dve", lambda e: e.tensor_scalar(out=stt[0:P, 1:2], in0=stt[0:P, 0:1], scalar1=1.0 / D, scalar2=EPS,
                                                  op0=ALU.mult, op1=ALU.add), reads=[stt], writes=[stt])
            c.op("act", lambda e: e.activation(out=stt[0:P, 2:3], in_=stt[0:P, 1:2], func=AF.Sqrt), reads=[stt], writes=[stt])
            c.op("dve", lambda e: e.reciprocal(out=stt[0:P, 3:4], in_=stt[0:P, 2:3]), reads=[stt], writes=[stt])
            c.dma("sp", lambda e: e.dma_start(out=facc[0:P, :], in_=fg.partition_broadcast(P)), writes=[facc])
            c.op("dve", lambda e: e.scalar_tensor_tensor(out=yt[0:P, :], in0=x1[0:P, :], scalar=stt[0:P, 3:4], in1=facc[0:P, :],
                                                          op0=ALU.mult, op1=ALU.mult), reads=[x1, stt, facc], writes=[yt])
            if not is_sample:
                c.dma("sp", lambda e: e.dma_start(out=yo[slot], in_=yt[:]), reads=[yt], is_output=True)
            else:
                c.dma("sp", lambda e: e.dma_start(out=ys, in_=yt[0:16, :]), reads=[yt], is_output=True)

        gen_mod(modA, 1, 128, 0, "scale", gain=g1)
        gen_mod(modB, 0, 128, 0, "plain")
        c.dma("pool", lambda e: e.dma_start(out=wkv[:], in_=w_in_v[:, :, 4096:5120]), writes=[wkv, rawT[0], rawT[1]])
        c.dma("pool", lambda e: e.dma_start(out=wkv2[:], in_=w_in_v[:, :, 5120:5632]), writes=[wkv2])
        c.op("pool", lambda e: e.memset(peT[:], 0.0), writes=[peT])
        for hh in range(2):
            for a_ in range(2):
                c.dma("pool", lambda e: e.dma_start(out=w1_sb[64 * hh:64 * hh + 64, a_, :, :], in_=cmp_w1[a_].rearrange("s d h -> d s h")),
                      writes=[w1_sb])
        c.dma("sp", lambda e: e.dma_start(out=tmp256[0:32, 0:128].rearrange("p (a d) -> p a d", a=2), in_=cmp_pe.rearrange("a s d -> s a d")),
              writes=[tmp256])
        ppe = next_pq()
        for a_ in range(2):
            c.op("pe", lambda e: e.transpose(out=ppe[0:64, a_ * 32:(a_ + 1) * 32], in_=tmp256[0:32, a_ * 64:(a_ + 1) * 64], identity=idf[0:32, 0:32]),
                 reads=[tmp256, idf], writes=[ppe])
        c.op("dve", lambda e: e.tensor_copy(out=peT[0:64, :, 0:32], in_=ppe[0:64, 0:64].rearrange("p (a s) -> p a s", a=2)),
             reads=[ppe], writes=[peT])
        c.dma("pool", lambda e: e.dma_start(out=w2_sb[:], in_=cmp_w2.rearrange("a h d -> h a d")), writes=[w2_sb])
        c.dma("sp", lambda e: e.dma_start(out=b1T[:], in_=cmp_b1.rearrange("a h -> h a"), allow_slow_non_contiguous=True), writes=[b1T])
        for a_ in range(2):
            c.dma("sp", lambda e: e.dma_start(out=b2bc[:, a_, :], in_=cmp_b2[a_:a_ + 1, :].partition_broadcast(128)), writes=[b2bc])
        c.dma("pool", lambda e: e.dma_start(out=ov_sb[:], in_=ovm), writes=[ov_sb])
        c.op("pool", lambda e: e.memset(vs_st[:], 1.0), writes=[vs_st])
        c.op("pool", lambda e: e.memset(vw_st[:], 1.0), writes=[vw_st])
        c.op("pool", lambda e: e.memset(vc1[:], 1.0), writes=[vc1])
        for a_ in range(2):
            p = next_pq()
            for s_ in range(32):
                c.op("pe", lambda e: e.matmul(p[:, 0:2], lhsT=w1_sb[0:64, a_, s_, :], rhs=peT[0:64, a_, s_:s_ + 2],
                                              start=(s_ == 0), stop=(s_ == 31)), reads=[w1_sb, peT], writes=[p])
            c.op("dve", lambda e: e.tensor_tensor(out=biasH[:, a_:a_ + 1], in0=p[:, 0:1], in1=b1T[:, a_:a_ + 1], op=ALU.add),
                 reads=[p, b1T], writes=[biasH])

        def compress_group(H, kdst=None, vdst=None):
            if "compress" in _SKIP:
                return
            kdst = kcT if kdst is None else kdst
            vdst = vc1 if vdst is None else vdst
            rt = rawT[H % 2]
            c.dma("sp", lambda e: e.dma_start(out=cs_t[:], in_=cscmp[H]), writes=[cs_t])
            pend = None
            ci = 0
            for a_ in range(2):
                for g in range(4):
                    p0 = 64 * (g % 2)
                    blk = a_ * 2 + g // 2
                    ph = next_pq()
                    for s_ in range(32):
                        c.op("pe", lambda e: e.matmul(ph[:, 0:128], lhsT=w1_sb[p0:p0 + 64, a_, s_, :],
                                                      rhs=rt[p0:p0 + 64, blk, s_:s_ + 16 * 127 + 1:16],
                                                      start=(s_ == 0), stop=(s_ == 31)), reads=[w1_sb, rt], writes=[ph])
                    hid = hidT if ci % 2 == 0 else hidT2
                    ci += 1
                    c.op("act", lambda e: e.activation(out=hid[:], in_=ph[:, 0:128], func=AF.Gelu_apprx_tanh, bias=biasH[:, a_:a_ + 1]),
                         reads=[ph, biasH], writes=[hid])

                    def fin(a_=a_, g=g, hid=hid):
                        po = next_pq()
                        c.op("pe", lambda e: e.matmul(po[:, 0:64], lhsT=hid[:], rhs=w2_sb[:, a_, :], start=True, stop=True),
                             reads=[hid, w2_sb], writes=[po])
                        if a_ == 0:
                            c.op("dve", lambda e: e.tensor_tensor(out=kc_tok[:, g, :], in0=po[:, 0:64], in1=b2bc[:, 0, :], op=ALU.add),
                                 reads=[po, b2bc], writes=[kc_tok])
                        else:
                            c.op("dve", lambda e: e.tensor_tensor(out=vdst[:, H, g, 0:64], in0=po[:, 0:64], in1=b2bc[:, 1, :], op=ALU.add),
                                 reads=[po, b2bc], writes=[vdst])
                    if pend is not None:
                        pend()
                    pend = fin
            if pend is not None:
                pend()
            kct = View(kc_tok[:].rearrange("p g d -> p (g d)"), kc_tok)
            a3, b3, d3 = rope_into(128, kct, 4, cs_t, kcr[:, :])
            c.op("dve", lambda e: e.tensor_tensor(out=d3, in0=a3, in1=b3, op=ALU.add), reads=[ropa, ropb], writes=[kcr])
            for gp in range(2):
                c.op("pe", lambda e: e.transpose(out=pT[0][:, gp, :], in_=kcr[:, gp * 128:(gp + 1) * 128], identity=idb[:]),
                     reads=[kcr, idb], writes=[pT[0]])
            c.op("act", lambda e: e.copy(out=kdst[:, :, H * 128:(H + 1) * 128], in_=pT[0][:, 0:2, :]), reads=[pT[0]], writes=[kdst])

        def flush_stage(G8):
            if "flush" in _SKIP:
                return
            c.dma("sp", lambda e: e.dma_start(out=kTs_d[:, :, G8 * 1024:(G8 + 1) * 1024], in_=kTs_st[:]), reads=[kTs_st], writes=[kv_res])
            c.dma("sp", lambda e: e.dma_start(out=kTw_d[:, :, G8 * 1024:(G8 + 1) * 1024], in_=kTw_st[:]), reads=[kTw_st], writes=[kv_res])
            for g in range(4):
                c.dma("sp", lambda e: e.dma_start(out=vs_d[g, :, G8 * 8:(G8 + 1) * 8, :], in_=vs_st[:, g, :, :]), reads=[vs_st], writes=[kv_res])
                c.dma("sp", lambda e: e.dma_start(out=vw_d[g, :, G8 * 8:(G8 + 1) * 8, :], in_=vw_st[:, g, :, :]), reads=[vw_st], writes=[kv_res])

        import os as _os1
        _ktiles = int(_os1.environ.get("KTILES", "64"))
        for t in range(_ktiles):
            xin = xt1
            c.dma("sp", lambda e: e.dma_start(out=xin[:], in_=xp[t]), writes=[xin])
            c.dma("sp", lambda e: e.dma_start(out=cs_t[:], in_=csa[t]), writes=[cs_t])
            norm_mod(128, xin, hb, st1, modA, modB)
            transpose_h(128, hb, 0)
            G16, t16 = t // 16, t % 16
            t8 = t % 8
            p = next_pq()
            for blk in range(0 if "raw" in _SKIP else 4):
                for k in range(16):
                    c.op("pe", lambda e: e.matmul(p[:, blk * 128:(blk + 1) * 128], lhsT=wkv[:, k, blk * 128:(blk + 1) * 128],
                                                  rhs=hT[:, k, 0:128], start=(k == 0), stop=(k == 15)), reads=[wkv, hT], writes=[p])
            c.op("act", lambda e: e.copy(out=rawT[G16 % 2][:, :, t16 * 128:(t16 + 1) * 128],
                                         in_=p[:, :].rearrange("p (b t) -> p b t", t=128)), reads=[p], writes=[rawT[G16 % 2]])
            if t16 == 0 and G16 >= 1:
                prev = rawT[(G16 - 1) % 2]
                c.op("act", lambda e: e.copy(out=prev[:, :, 2048:2064], in_=p[:, :].rearrange("p (b t) -> p b t", t=128)[:, :, 0:16]),
                     reads=[p], writes=[prev])
                compress_group(G16 - 1)
                c.dma("sp", lambda e: e.dma_start(out=cs_t[:], in_=csa[t]), writes=[cs_t])
            pj = []
            for which in range(2):
                wsrc = wkv[:, :, 512:1024] if which == 0 else wkv2[:, :, :]
                wres = wkv if which == 0 else wkv2
                p = next_pq()
                for k in range(16):
                    c.op("pe", lambda e: e.matmul(p[:, :], lhsT=hT[:, k, 0:128], rhs=wsrc[:, k, :], start=(k == 0), stop=(k == 15)),
                         reads=[hT, wres], writes=[p])
                pj.append(p)
            for which in range(2):
                p = pj[which]
                kst = kTs_st if which == 0 else kTw_st
                vst = vs_st if which == 0 else vw_st
                ksb = ks_b if which == 0 else ks_b2
                pt_ = pT[0] if which == 0 else pT[1]
                a3, b3, d3 = rope_into(128, p, 4, cs_t, ksb[:, :])
                c.op("dve", lambda e: e.tensor_tensor(out=d3, in0=a3, in1=b3, op=ALU.add), reads=[ropa, ropb], writes=[ksb])
                c.op("dve", lambda e: e.tensor_copy(out=vst[:, :, t8, 0:64], in_=p[:, 256:512].rearrange("p (g d) -> p g d", d=64)),
                     reads=[p], writes=[vst])
                for gp in range(2):
                    c.op("pe", lambda e: e.transpose(out=pt_[:, gp, :], in_=ksb[:, gp * 128:(gp + 1) * 128], identity=idb[:]),
                         reads=[ksb, idb], writes=[pt_])
                c.op("act", lambda e: e.copy(out=kst[:, :, t8 * 128:(t8 + 1) * 128], in_=pt_[:, 0:2, :]),
                     reads=[pt_], writes=[kst])
            if t8 == 7:
                flush_stage(t // 8)
        last = rawT[3 % 2]
        c.op("pool", lambda e: e.memset(last[:, :, 2048:2064], 0.0), writes=[last])
        compress_group(3)
        c.barrier()

        if "phaseS" not in _SKIP:
            wkv_f = wkv[:].rearrange("p a b -> p (a b)").bitcast(F32)
            wkv2_b = wkv2[:].rearrange("p a b -> p (a b)")
            pgb = [View(wkv_f[:, i * 1024:(i + 1) * 1024], Res("pgb")) for i in range(2)]
            pgh2 = [View(wkv2_b[:, 0:1024], Res("pgh0")), View(wkv2_b[:, 4096:5120], Res("pgh1"))]
            kcT_s = View(wkv2_b[:, 1024:2048].rearrange("p (a b) -> p a b", a=2), Res("kcT_s"))
            vc1_s = View(wkv2_b[:, 2048:2048 + 1040].rearrange("p (a g d) -> p a g d", a=4, g=4), Res("vc1_s"))
            c.dma("sp", lambda e: e.dma_start(out=pio_t[:], in_=piota), writes=[pio_t])
            c.op("pool", lambda e: e.memset(vc1_s[:, :, :, :], 1.0), writes=[vc1_s])
            for b in range(NS):
                c.dma("sp", lambda e: e.dma_start(out=pt_i[:], in_=ptab[b:b + 1, :].partition_broadcast(128)), writes=[pt_i])
                c.op("dve", lambda e: e.tensor_scalar(out=idx_f[:], in0=pt_i[:], scalar1=128.0, scalar2=pio_t[:, 0:1], op0=ALU.mult, op1=ALU.add),
                     reads=[pt_i, pio_t], writes=[idx_f])
                c.op("dve", lambda e: e.tensor_scalar(out=idx_f[:], in0=idx_f[:], scalar1=0.0, scalar2=float(2560 * 128 - 1), op0=ALU.max, op1=ALU.min),
                     reads=[idx_f], writes=[idx_f])
                c.op("dve", lambda e: e.tensor_copy(out=idx_u[:], in_=idx_f[:]), reads=[idx_f], writes=[idx_u])
                for pg in range(64):
                    pb_ = pgb[pg % 2]
                    pgh = pgh2[pg % 2]
                    c.dma("pool", lambda e: e.indirect_dma_start(out=pb_[:, :], out_offset=None, in_=cache[:, :],
                                                                 in_offset=bass.IndirectOffsetOnAxis(ap=idx_u[:, pg:pg + 1], axis=0)),
                          reads=[idx_u], writes=[pb_])
                    c.op("act", lambda e: e.copy(out=pgh[:, :], in_=pb_[:, :]), reads=[pb_], writes=[pgh])
                    G16, t16, t8 = pg // 16, pg % 16, pg % 8
                    for blk in range(4):
                        c.op("pe", lambda e: e.transpose(out=pT[0][:, blk, :], in_=pgh[:, blk * 128:(blk + 1) * 128], identity=idb[:]),
                             reads=[pgh, idb], writes=[pT[0]])
                    c.op("dve", lambda e: e.tensor_copy(out=rawT[G16 % 2][:, :, t16 * 128:(t16 + 1) * 128], in_=pT[0][:, 0:4, :]),
                         reads=[pT[0]], writes=[rawT[G16 % 2]])
                    if t16 == 0 and G16 >= 1:
                        prev = rawT[(G16 - 1) % 2]
                        c.op("dve", lambda e: e.tensor_copy(out=prev[:, :, 2048:2064], in_=pT[0][:, 0:4, 0:16]), reads=[pT[0]], writes=[prev])
                        compress_group(G16 - 1, kcT_s, vc1_s)
                    for gp in range(2):
                        c.op("pe", lambda e: e.transpose(out=pT[1][:, gp, :], in_=pgh[:, 512 + gp * 128:512 + (gp + 1) * 128], identity=idb[:]),
                             reads=[pgh, idb], writes=[pT[1]])
                    c.op("dve", lambda e: e.tensor_copy(out=kTs_st[:, :, t8 * 128:(t8 + 1) * 128], in_=pT[1][:, 0:2, :]),
                         reads=[pT[1]], writes=[kTs_st])
                    c.op("dve", lambda e: e.tensor_copy(out=vs_st[:, :, t8, 0:64], in_=pgh[:, 768:1024].rearrange("p (g d) -> p g d", d=64)),
                         reads=[pgh], writes=[vs_st])
                    if t8 == 7:
                        G8 = pg // 8
                        c.dma("sp", lambda e: e.dma_start(out=kTs_s[b, :, :, G8 * 1024:(G8 + 1) * 1024], in_=kTs_st[:]), reads=[kTs_st], writes=[kv_res])
                        for g in range(4):
                            c.dma("sp", lambda e: e.dma_start(out=vs_s[b, g, :, G8 * 8:(G8 + 1) * 8, :], in_=vs_st[:, g, :, :]), reads=[vs_st], writes=[kv_res])
                lastS = rawT[3 % 2]
                c.op("pool", lambda e: e.memset(lastS[:, :, 2048:2064], 0.0), writes=[lastS])
                compress_group(3, kcT_s, vc1_s)
                c.dma("sp", lambda e: e.dma_start(out=kc_s[b], in_=kcT_s[:, :, :]), reads=[kcT_s], writes=[kv_res])
                c.dma("sp", lambda e: e.dma_start(out=vc_s[b], in_=vc1_s[:, :, :, :]), reads=[vc1_s], writes=[kv_res])
                for wc in range(4):
                    pb_ = pgb[wc % 2]
                    pgh = pgh2[wc % 2]
                    c.dma("sp", lambda e: e.dma_start(out=pb_[:, 0:512], in_=swin[b, wc * 128:(wc + 1) * 128, :]), writes=[pb_])
                    c.op("act", lambda e: e.copy(out=pgh[:, 0:512], in_=pb_[:, 0:512]), reads=[pb_], writes=[pgh])
                    for gp in range(2):
                        c.op("pe", lambda e: e.transpose(out=pT[1][:, gp, :], in_=pgh[:, gp * 128:(gp + 1) * 128], identity=idb[:]),
                             reads=[pgh, idb], writes=[pT[1]])
                    c.op("dve", lambda e: e.tensor_copy(out=kTw_st[:, :, wc * 128:(wc + 1) * 128], in_=pT[1][:, 0:2, :]),
                         reads=[pT[1]], writes=[kTw_st])
                    c.op("dve", lambda e: e.tensor_copy(out=vw_st[:, :, wc, 0:64], in_=pgh[:, 256:512].rearrange("p (g d) -> p g d", d=64)),
                         reads=[pgh], writes=[vw_st])
                c.dma("sp", lambda e: e.dma_start(out=kTw_s[b], in_=kTw_st[:, :, 0:512]), reads=[kTw_st], writes=[kv_res])
                for g in range(4):
                    c.dma("sp", lambda e: e.dma_start(out=vw_s[b, g], in_=vw_st[:, g, 0:4, :]), reads=[vw_st], writes=[kv_res])
            c.barrier()
        esA.close()
        import os as _os
        _dbg = _os.environ.get("KDBG", "")
        if _dbg == "A":
            c.finish()
            return nc

        wb = [c.sb("wb%d" % i, [128, 16, 512], BF16) for i in range(2)]
        for b_ in wb:
            b_.r2 = Res("wb_hi")
        state["wbufs"] = wb

        def _f32v(b_):
            return b_[:].rearrange("p a b -> p (a b)").bitcast(F32)

        Ub = [View(_f32v(wb[0])[:, 0:D], wb[0].r), View(_f32v(wb[0])[:, D:2 * D], wb[0].r2)]
        Vb = [View(_f32v(wb[1])[:, 0:D], wb[1].r), View(_f32v(wb[1])[:, D:2 * D], wb[1].r2)]
        keysT = c.sb("keysT", [128, 16, 128], BF16)
        bgT = c.sb("bgT", [128, 8, 128])
        cgT = c.sb("cgT", [128, 8, 130])
        zcT = c.sb("zcT", [128, 8, 130])
        acc = c.sb("acc", [128, 128])
        catT = c.sb("catT", [128, 16, 128], BF16)
        q_r = c.sb("q_r", [128, 1024], BF16)
        sctx = c.sb("sctx", [128, 8, NS, 6])
        sacc = c.sb("sacc", [128, NS, 4])
        qTp = c.sb("qTp", [128, 8, 128], BF16)
        gates_t = c.sb("gates_t", [128, 48])
        e32_t = c.sb("e32_t", [32, 16, 128], BF16)
        fmask_t = c.sb("fmask_t", [128, 128])
        cmask_t = c.sb("cmask_t", [128, 4, 128], BF16)
        dmask_t = c.sb("dmask_t", [128, 8, 128], BF16)
        wmask_t = c.sb("wmask_t", [128, 12, 128], BF16)
        kbuf = [c.sb("kbuf%d" % i, [128, 1536], BF16) for i in range(2)]
        vbuf = [c.sb("vbuf%d" % i, [128, 12, 65], BF16) for i in range(2)]
        ebuf = [c.sb("ebuf%d" % i, [128, 4, 128], BF16) for i in range(2)]
        pTb = [c.sb("pTb%d" % i, [128, 4, 128], BF16) for i in range(2)]
        attn_f = c.sb("attn_f", [128, 16, 64])
        attn_b = c.sb("attn_b", [128, 1024], BF16)
        imp_t = c.sb("imp_t", [128, 128])
        sc_t = c.sb("sc_t", [128, 128])
        sc2_t = c.sb("sc2_t", [128, 128])
        sel_b = c.sb("sel_b", [128, 128], BF16)
        selT = c.sb("selT", [32, 4, 128], BF16)
        negT = c.sb("negT", [32, 4, 4, 128], BF16)
        m16 = c.sb("m16", [128, 16])
        rs4 = c.sb("rs4", [128, 8])
        c.dma("pool", lambda e: e.dma_start(out=e32_t[:], in_=e32), writes=[e32_t])
        kcT2 = c.sb("kcT2", [128, 2, 512], BF16)
        vc2 = c.sb("vc2", [128, 4, 4, 65], BF16)
        kn_b = c.sb("kn_b", [16, 512], BF16)
        kTn = c.sb("kTn", [128, 4, 16], BF16)
        vn = c.sb("vn", [16, 2, 4, 65], BF16)
        qs = c.sb("qs", [128, 8, 4], BF16)
        gq = c.sb("gq", [4, 48])
        cm_s = c.sb("cm_s", [128, 4, 4], BF16)
        fm_s = c.sb("fm_s", [4, 128])
        wm_s = c.sb("wm_s", [128, 4, 4], BF16)
        nm_s = c.sb("nm_s", [16, NS, 4], BF16)
        c.dma("pool", lambda e: e.dma_start(out=cm_s[:], in_=cmask_sm), writes=[cm_s])
        c.dma("pool", lambda e: e.dma_start(out=wm_s[:], in_=wmask_sm), writes=[wm_s])
        c.dma("pool", lambda e: e.dma_start(out=nm_s[:], in_=nmask), writes=[nm_s])
        c.dma("sp", lambda e: e.dma_start(out=fm_s[:], in_=fmask_sm), writes=[fm_s])

        c.dma("sp", lambda e: e.dma_start(out=junk[:].rearrange("p (j d) -> p j d", d=128), in_=peer_keys.rearrange("j k d -> k j d")),
              writes=[junk])
        for n in range(4):
            p = next_pq()
            for jj in range(4):
                j = n * 4 + jj
                c.op("pe", lambda e: e.transpose(out=p[:, jj * 128:(jj + 1) * 128], in_=junk[:, j * 128:(j + 1) * 128], identity=idf[:]),
                     reads=[junk, idf], writes=[p])
            c.op("dve", lambda e: e.tensor_copy(out=keysT[:, n * 4:(n + 1) * 4, :].rearrange("p a b -> p (a b)"), in_=p[:, :]),
                 reads=[p], writes=[keysT])


        oacc = View(pT1f[:, 0:260].rearrange("p (r d) -> p r d", d=65), pT1f)
        iacc = View(pc_full[1][:, :].rearrange("p (r s) -> p r s", s=128), pc_full[1])
        astate = {"e": 0}

        def zero_bank(bank_ap, res):
            c.op("pe", lambda e: e.matmul(bank_ap, lhsT=zer[:, 0:128], rhs=zer[:, 0:512], start=True, stop=False),
                 reads=[zer], writes=[res])

        def attn_unit(P, kT_ap, kres, qrhs, v_ap, vres, mask_ap, mres, mask2_ap, m2res, last, want_imp=None, NK=128, bias=None):
            ps = next_pq()
            W = 4 * P
            c.op("pe", lambda e: e.matmul(ps[0:NK, 0:W], lhsT=kT_ap, rhs=qrhs, start=True, stop=(bias is None)), reads=[kres, qTp], writes=[ps])
            if bias is not None:
                bl, br_, bres = bias
                c.op("pe", lambda e: e.matmul(ps[0:NK, 0:W], lhsT=bl, rhs=br_, start=False, stop=True), reads=[e32_t, bres], writes=[ps])
            eb = ebuf[astate["e"] % 2]
            pb = pTb[astate["e"] % 2]
            astate["e"] += 1
            ebf = eb[:].rearrange("p r q -> p (r q)")
            pbf = pb[:].rearrange("p r q -> p (r q)")
            masks = [(m, r_) for m, r_ in ((mask_ap, mres), (mask2_ap, m2res)) if m is not None]
            if not masks:
                c.op("act", lambda e: e.activation(out=pbf[0:NK, 0:W], in_=ps[0:NK, 0:W], func=AF.Exp, scale=0.125), reads=[ps], writes=[pb])
            else:
                c.op("act", lambda e: e.activation(out=ebf[0:NK, 0:W], in_=ps[0:NK, 0:W], func=AF.Exp, scale=0.125), reads=[ps], writes=[eb])
                src, sres = ebf, eb
                for m, r_ in masks:
                    c.op("dve", lambda e: e.tensor_tensor(out=pbf[0:NK, 0:W].rearrange("p (r q) -> p r q", q=P),
                                                          in0=src[0:NK, 0:W].rearrange("p (r q) -> p r q", q=P),
                                                          in1=m.unsqueeze(1).broadcast_to([NK, 4, P]), op=ALU.mult),
                         reads=[sres, r_], writes=[pb])
                    src, sres = pbf, pb
            def stage2():
                for r in range(4):
                    c.op("pe", lambda e: e.matmul(oacc[0:P, r, :], lhsT=pbf[0:NK, r * P:(r + 1) * P], rhs=v_ap, start=False, stop=last),
                         reads=[pb, vres], writes=[oacc])
                    if want_imp is not None:
                        c.op("pe", lambda e: e.matmul(iacc[0:P, r, :], lhsT=pbf[0:NK, r * P:(r + 1) * P], rhs=want_imp, start=False, stop=last),
                             reads=[pb, ov_sb], writes=[iacc])
            prev = astate.get("pending")
            astate["pending"] = stage2
            if prev is not None:
                prev()

        def flush_units():
            prev = astate.get("pending")
            astate["pending"] = None
            if prev is not None:
                prev()

        def finish_branch(P, g, br, first_branch, gsrc=None):
            flush_units()
            c.op("dve", lambda e: e.tensor_scalar(out=rs4[0:P, 0:4], in0=oacc[0:P, :, 64], scalar1=1e-30, scalar2=None, op0=ALU.max),
                 reads=[oacc], writes=[rs4])
            c.op("dve", lambda e: e.reciprocal(out=rs4[0:P, 0:4], in_=rs4[0:P, 0:4]), reads=[rs4], writes=[rs4])
            gsrc = gates_t if gsrc is None else gsrc
            g3 = gsrc[0:P, :].rearrange("p (h b) -> p h b", b=3)
            c.op("dve", lambda e: e.tensor_tensor(out=rs4[0:P, 4:8], in0=rs4[0:P, 0:4], in1=g3[:, 4 * g:4 * g + 4, br], op=ALU.mult),
                 reads=[rs4, gsrc], writes=[rs4])
            for r in range(4):
                h = 4 * g + r
                if first_branch:
                    c.op("dve", lambda e: e.tensor_scalar(out=attn_f[0:P, h, :], in0=oacc[0:P, r, 0:64], scalar1=rs4[0:P, 4 + r:5 + r],
                                                          scalar2=None, op0=ALU.mult), reads=[oacc, rs4], writes=[attn_f])
                else:
                    c.op("dve", lambda e: e.scalar_tensor_tensor(out=attn_f[0:P, h, :], in0=oacc[0:P, r, 0:64], scalar=rs4[0:P, 4 + r:5 + r],
                                                                  in1=attn_f[0:P, h, :], op0=ALU.mult, op1=ALU.add),
                         reads=[oacc, rs4, attn_f], writes=[attn_f])

        def attention_prompt(i):
            P = 128
            c.dma("sp", lambda e: e.dma_start(out=fmask_t[:], in_=fmask[i]), writes=[fmask_t])
            c.dma("pool", lambda e: e.dma_start(out=cmask_t[:], in_=cmask[i]), writes=[cmask_t])
            c.dma("pool", lambda e: e.dma_start(out=dmask_t[:], in_=dmask[i]), writes=[dmask_t])
            c.dma("pool", lambda e: e.dma_start(out=wmask_t[:], in_=wmask[i]), writes=[wmask_t])
            for pb_ in range(8):
                c.op("pe", lambda e: e.transpose(out=pT[0][:, pb_, :], in_=q_r[:, pb_ * 128:(pb_ + 1) * 128], identity=idb[:]),
                     reads=[q_r, idb], writes=[pT[0]])
            c.op("act", lambda e: e.copy(out=qTp[:], in_=pT[0][:]), reads=[pT[0]], writes=[qTp])
            njc = min(4, (64 * i + 62) // 128 + 1)
            nkc = 8 * i + 8
            for g in range(4):
                p0, gp = 64 * (g % 2), g // 2
                qrhs = qTp[:].rearrange("p a b -> p (a b)")[p0:p0 + 64, gp * 512:(gp + 1) * 512]
                zero_bank(pT1f[:, :], pT1f)
                zero_bank(pc_full[1][:, :], pc_full[1])
                for jc in range(njc):
                    attn_unit(P, kcT[p0:p0 + 64, gp, jc * 128:(jc + 1) * 128], kcT, qrhs, vc1[:, jc, g, :], vc1,
                              cmask_t[:, jc, :], cmask_t, None, None, jc == njc - 1, want_imp=ov_sb[:, jc, :])
                finish_branch(P, g, 0, True)
                for r in range(4):
                    if r == 0:
                        c.op("dve", lambda e: e.tensor_scalar(out=imp_t[:], in0=iacc[:, 0, :], scalar1=rs4[:, 0:1], scalar2=None, op0=ALU.mult),
                             reads=[iacc, rs4], writes=[imp_t])
                    else:
                        c.op("dve", lambda e: e.scalar_tensor_tensor(out=imp_t[:], in0=iacc[:, r, :], scalar=rs4[:, r:r + 1], in1=imp_t[:],
                                                                      op0=ALU.mult, op1=ALU.add), reads=[iacc, rs4, imp_t], writes=[imp_t])
                c.op("dve", lambda e: e.tensor_tensor(out=sc_t[:], in0=imp_t[:], in1=fmask_t[:], op=ALU.add), reads=[imp_t, fmask_t], writes=[sc_t])
                c.op("dve", lambda e: e.max(out=m16[:, 0:8], in_=sc_t[:]), reads=[sc_t], writes=[m16])
                c.op("dve", lambda e: e.match_replace(out=sc2_t[:], in_to_replace=m16[:, 0:8], in_values=sc_t[:], imm_value=-3.0e38),
                     reads=[sc_t, m16], writes=[sc2_t])
                c.op("dve", lambda e: e.max(out=m16[:, 8:16], in_=sc2_t[:]), reads=[sc2_t], writes=[m16])
                c.op("dve", lambda e: e.tensor_scalar(out=m16[:, 0:1], in0=m16[:, 15:16], scalar1=-1.0e29, scalar2=None, op0=ALU.max),
                     reads=[m16], writes=[m16])
                c.op("dve", lambda e: e.tensor_scalar(out=sel_b[:], in0=sc_t[:], scalar1=m16[:, 0:1], scalar2=None, op0=ALU.is_ge),
                     reads=[sc_t, m16], writes=[sel_b])
                for sl in range(4):
                    c.op("pe", lambda e: e.transpose(out=pT[0][0:32, sl, :], in_=sel_b[:, sl * 32:(sl + 1) * 32], identity=idb[:]),
                         reads=[sel_b, idb], writes=[pT[0]])
                c.op("act", lambda e: e.copy(out=selT[:], in_=pT[0][0:32, 0:4, :]), reads=[pT[0]], writes=[selT])
                for r_ in range(4):
                    c.op("dve", lambda e: e.tensor_scalar(out=negT[:, :, r_, :], in0=selT[:, :, :], scalar1=-1.0, scalar2=30000.0,
                                                          op0=ALU.add, op1=ALU.mult), reads=[selT], writes=[negT])
                zero_bank(pT1f[:, :], pT1f)
                for G8 in range(nkc // 8):
                    kb, vb_ = kbuf[G8 % 2], vbuf[G8 % 2]
                    c.dma("sp", lambda e: e.dma_start(out=kb[p0:p0 + 64, 0:1024], in_=kTs_d[p0:p0 + 64, gp, G8 * 1024:(G8 + 1) * 1024]),
                          reads=[kv_res], writes=[kb])
                    c.dma("sp", lambda e: e.dma_start(out=vb_[:, 0:8, :], in_=vs_d[g, :, G8 * 8:(G8 + 1) * 8, :]), reads=[kv_res], writes=[vb_])
                    for k8 in range(8):
                        kc = G8 * 8 + k8
                        diag = kc >= 8 * i
                        attn_unit(P, kb[p0:p0 + 64, k8 * 128:(k8 + 1) * 128], kb, qrhs, vb_[:, k8, :], vb_,
                                  None, None, dmask_t[:, kc - 8 * i, :] if diag else None, dmask_t if diag else None, kc == nkc - 1,
                                  bias=(e32_t[:, kc % 16, :], negT[:, kc // 16, :, :].rearrange("p r q -> p (r q)"), negT))
                finish_branch(P, g, 1, False)
                zero_bank(pT1f[:, :], pT1f)
                kc0 = max(0, 8 * i - 4)
                nw = 8 * i + 8 - kc0
                kb, vb_ = kbuf[0], vbuf[0]
                c.dma("sp", lambda e: e.dma_start(out=kb[p0:p0 + 64, 0:nw * 128], in_=kTw_d[p0:p0 + 64, gp, kc0 * 128:(kc0 + nw) * 128]),
                      reads=[kv_res], writes=[kb])
                c.dma("sp", lambda e: e.dma_start(out=vb_[:, 0:nw, :], in_=vw_d[g, :, kc0:kc0 + nw, :]), reads=[kv_res], writes=[vb_])
                for w_ in range(nw):
                    kc = kc0 + w_
                    wi = kc - (8 * i - 4)
                    attn_unit(P, kb[p0:p0 + 64, w_ * 128:(w_ + 1) * 128], kb, qrhs, vb_[:, w_, :], vb_,
                              wmask_t[:, wi, :], wmask_t, None, None, w_ == nw - 1)
                finish_branch(P, g, 2, False)
            c.op("act", lambda e: e.copy(out=attn_b[:], in_=attn_f[:].rearrange("p h d -> p (h d)")), reads=[attn_f], writes=[attn_b])
            for k in range(8):
                c.op("pe", lambda e: e.transpose(out=pT[0][:, k, :], in_=attn_b[:, k * 128:(k + 1) * 128], identity=idb[:]),
                     reads=[attn_b, idb], writes=[pT[0]])
            c.op("dve", lambda e: e.tensor_copy(out=catT[:, 8:16, :], in_=pT[0][:]), reads=[pT[0]], writes=[catT])

        def attention_sample():
            P = 4
            for pb_ in range(8):
                c.op("pe", lambda e: e.transpose(out=pT[0][:, pb_, 0:16], in_=q_r[0:16, pb_ * 128:(pb_ + 1) * 128], identity=idb[0:16, 0:16]),
                     reads=[q_r, idb], writes=[pT[0]])
            c.op("dve", lambda e: e.tensor_copy(out=qTp[:, :, 0:16], in_=pT[0][:, :, 0:16]), reads=[pT[0]], writes=[qTp])
            c.op("dve", lambda e: e.tensor_copy(out=kn_b[:, 0:256], in_=rows_t[0:16, 512:768]), reads=[rows_t], writes=[kn_b])
            c.op("dve", lambda e: e.tensor_copy(out=kn_b[:, 256:512], in_=win_t[0:16, 0:256]), reads=[win_t], writes=[kn_b])
            c.op("pool", lambda e: e.memset(vn[:], 1.0), writes=[vn])
            c.op("dve", lambda e: e.tensor_copy(out=vn[:, 0, :, 0:64], in_=rows_t[0:16, 768:1024].rearrange("p (g d) -> p g d", d=64)),
                 reads=[rows_t], writes=[vn])
            c.op("dve", lambda e: e.tensor_copy(out=vn[:, 1, :, 0:64], in_=win_t[0:16, 256:512].rearrange("p (g d) -> p g d", d=64)),
                 reads=[win_t], writes=[vn])
            for blk in range(4):
                c.op("pe", lambda e: e.transpose(out=pT[0][:, blk, 0:16], in_=kn_b[:, blk * 128:(blk + 1) * 128], identity=idb[0:16, 0:16]),
                     reads=[kn_b, idb], writes=[pT[0]])
            c.op("dve", lambda e: e.tensor_copy(out=kTn[:], in_=pT[0][:, 0:4, 0:16]), reads=[pT[0]], writes=[kTn])
            for b in range(NS):
                c.dma("sp", lambda e: e.dma_start(out=gq[:], in_=gates_t[4 * b:4 * b + 4, :]), reads=[gates_t], writes=[gq])
                c.op("dve", lambda e: e.tensor_copy(out=qs[:], in_=qTp[:, :, 4 * b:4 * b + 4]), reads=[qTp], writes=[qs])
                c.dma("sp", lambda e: e.dma_start(out=kcT2[:], in_=kc_s[b]), reads=[kv_res], writes=[kcT2])
                c.dma("sp", lambda e: e.dma_start(out=vc2[:], in_=vc_s[b]), reads=[kv_res], writes=[vc2])
                qsf = qs[:].rearrange("p a b -> p (a b)")
                for g in range(4):
                    p0, gp = 64 * (g % 2), g // 2
                    qrhs = qsf[p0:p0 + 64, gp * 16:(gp + 1) * 16]
                    zero_bank(pT1f[:, :], pT1f)
                    zero_bank(pc_full[1][:, :], pc_full[1])
                    for jc in range(4):
                        attn_unit(P, kcT2[p0:p0 + 64, gp, jc * 128:(jc + 1) * 128], kcT2, qrhs, vc2[:, jc, g, :], vc2,
                                  cm_s[:, jc, :], cm_s, None, None, jc == 3, want_imp=ov_sb[:, jc, :])
                    finish_branch(P, g, 0, True, gsrc=gq)
                    for r in range(4):
                        if r == 0:
                            c.op("dve", lambda e: e.tensor_scalar(out=imp_t[0:P, :], in0=iacc[0:P, 0, :], scalar1=rs4[0:P, 0:1], scalar2=None, op0=ALU.mult),
                                 reads=[iacc, rs4], writes=[imp_t])
                        else:
                            c.op("dve", lambda e: e.scalar_tensor_tensor(out=imp_t[0:P, :], in0=iacc[0:P, r, :], scalar=rs4[0:P, r:r + 1], in1=imp_t[0:P, :],
                                                                          op0=ALU.mult, op1=ALU.add), reads=[iacc, rs4, imp_t], writes=[imp_t])
                    c.op("dve", lambda e: e.tensor_tensor(out=sc_t[0:P, :], in0=imp_t[0:P, :], in1=fm_s[0:P, :], op=ALU.add), reads=[imp_t, fm_s], writes=[sc_t])
                    c.op("dve", lambda e: e.max(out=m16[0:P, 0:8], in_=sc_t[0:P, :]), reads=[sc_t], writes=[m16])
                    c.op("dve", lambda e: e.match_replace(out=sc2_t[0:P, :], in_to_replace=m16[0:P, 0:8], in_values=sc_t[0:P, :], imm_value=-3.0e38),
                         reads=[sc_t, m16], writes=[sc2_t])
                    c.op("dve", lambda e: e.max(out=m16[0:P, 8:16], in_=sc2_t[0:P, :]), reads=[sc2_t], writes=[m16])
                    c.op("dve", lambda e: e.tensor_scalar(out=sel_b[0:P, :], in0=sc_t[0:P, :], scalar1=m16[0:P, 14:15], scalar2=None, op0=ALU.is_ge),
                         reads=[sc_t, m16], writes=[sel_b])
                    for sl in range(4):
                        c.op("pe", lambda e: e.transpose(out=pT[0][0:32, sl, 0:P], in_=sel_b[0:P, sl * 32:(sl + 1) * 32], identity=idb[0:P, 0:P]),
                             reads=[sel_b, idb], writes=[pT[0]])
                    c.op("dve", lambda e: e.tensor_copy(out=selT[:, :, 0:P], in_=pT[0][0:32, 0:4, 0:P]), reads=[pT[0]], writes=[selT])
                    negS = negT[:].rearrange("p a r q -> p (a r q)")[:, 0:64].rearrange("p (a r q) -> p a r q", a=4, r=4)
                    for r_ in range(4):
                        c.op("dve", lambda e: e.tensor_scalar(out=negS[:, :, r_, :], in0=selT[:, :, 0:P], scalar1=-1.0, scalar2=30000.0,
                                                              op0=ALU.add, op1=ALU.mult), reads=[selT], writes=[negT])
                    zero_bank(pT1f[:, :], pT1f)
                    for G8 in range(8):
                        kb, vb_ = kbuf[G8 % 2], vbuf[G8 % 2]
                        c.dma("sp", lambda e: e.dma_start(out=kb[p0:p0 + 64, 0:1024], in_=kTs_s[b, p0:p0 + 64, gp, G8 * 1024:(G8 + 1) * 1024]),
                              reads=[kv_res], writes=[kb])
                        c.dma("sp", lambda e: e.dma_start(out=vb_[:, 0:8, :], in_=vs_s[b, g, :, G8 * 8:(G8 + 1) * 8, :]), reads=[kv_res], writes=[vb_])
                        for k8 in range(8):
                            kc = G8 * 8 + k8
                            attn_unit(P, kb[p0:p0 + 64, k8 * 128:(k8 + 1) * 128], kb, qrhs, vb_[:, k8, :], vb_, None, None, None, None, False,
                                      bias=(e32_t[:, kc % 16, :], negS[:, kc // 16, :, :].rearrange("p r q -> p (r q)"), negT))
                    attn_unit(P, kTn[p0:p0 + 64, gp, :], kTn, qrhs, vn[:, 0, g, :], vn, nm_s[:, b, :], nm_s, None, None, True, NK=16)
                    finish_branch(P, g, 1, False, gsrc=gq)
                    zero_bank(pT1f[:, :], pT1f)
                    kb, vb_ = kbuf[0], vbuf[0]
                    c.dma("sp", lambda e: e.dma_start(out=kb[p0:p0 + 64, 0:512], in_=kTw_s[b, p0:p0 + 64, gp, :]), reads=[kv_res], writes=[kb])
                    c.dma("sp", lambda e: e.dma_start(out=vb_[:, 0:4, :], in_=vw_s[b, g]), reads=[kv_res], writes=[vb_])
                    for wc in range(4):
                        attn_unit(P, kb[p0:p0 + 64, wc * 128:(wc + 1) * 128], kb, qrhs, vb_[:, wc, :], vb_, wm_s[:, wc, :], wm_s, None, None, False)
                    attn_unit(P, kTn[p0:p0 + 64, 2 + gp, :], kTn, qrhs, vn[:, 1, g, :], vn, nm_s[:, b, :], nm_s, None, None, True, NK=16)
                    finish_branch(P, g, 2, False, gsrc=gq)
                c.op("act", lambda e: e.copy(out=attn_b[0:P, :], in_=attn_f[0:P, :, :].rearrange("p h d -> p (h d)")), reads=[attn_f], writes=[attn_b])
                for k in range(8):
                    c.op("pe", lambda e: e.transpose(out=pT[0][:, k, 0:P], in_=attn_b[0:P, k * 128:(k + 1) * 128], identity=idb[0:P, 0:P]),
                         reads=[attn_b, idb], writes=[pT[0]])
                c.op("dve", lambda e: e.tensor_copy(out=catT[:, 8:16, 4 * b:4 * b + 4], in_=pT[0][:, :, 0:P]), reads=[pT[0]], writes=[catT])

        gen_mod(modA, 1, 16, 128, "scale", gain=g1)
        gen_mod(modB, 0, 16, 128, "plain")
        gen_mod(modG, 2, 16, 128, "plain")
        xin = xt[0]
        c.dma("sp", lambda e: e.dma_start(out=xin[0:16, :], in_=xs), writes=[xin])
        c.dma("sp", lambda e: e.dma_start(out=cs_t[0:16, :], in_=css), writes=[cs_t])
        c.dma("sp", lambda e: e.dma_start(out=scv_t[:], in_=scv), writes=[scv_t])
        c.dma("sp", lambda e: e.dma_start(out=win_s[:, 0:508, :], in_=swin[:, 4:512, :]), is_output=True)
        pa = next_pq()
        for j in range(8):
            c.op("pe", lambda e: e.transpose(out=pa[:, j * 8:(j + 1) * 8], in_=scv_t[:, j * 128:(j + 1) * 128], identity=idf[0:8, 0:8]),
                 reads=[scv_t, idf], writes=[pa])
        c.op("dve", lambda e: e.tensor_copy(out=sctx[:, :, :, 0:2], in_=pa[:, 0:64].rearrange("p (c s t) -> p c s t", c=8, s=NS)),
             reads=[pa], writes=[sctx])
        norm_mod(16, xin, hb, st1, modA, modB)
        transpose_h(16, hb, 0)
        proc_tile(16, 16, xin, True, 0)

        for i in range(NT):
            gen_mod(modA, 1, 128, 0, "scale", gain=g1)
            gen_mod(modB, 0, 128, 0, "plain")
            gen_mod(modG, 2, 128, 0, "plain")
            xin = xt[i % 2]
            c.dma("sp", lambda e: e.dma_start(out=xin[:], in_=xo[i]), writes=[xin])
            c.dma("sp", lambda e: e.dma_start(out=xp2[0:2, :], in_=xpv[i]), writes=[xp2])
            c.dma("sp", lambda e: e.dma_start(out=cs_t[:], in_=cso[i]), writes=[cs_t])
            norm_mod(2, xp2, hb2, st2, modA, modB)
            transpose_h(2, hb2, 0)
            norm_mod(128, xin, hb, st1, modA, modB)
            transpose_h(128, hb, 2)
            proc_tile(128, 130, xin, False, i)

        c.finish()
        print("instructions (approx):", c.ninst)
    return nc


def _tiles_of(core):
    return [core, 15 - core, 16 + core, 31 - core, 32 + core, 47 - core, 48 + core, 63 - core]


def _rope_table(pos):
    half = 32
    inv = (10000.0 ** (-np.arange(half, dtype=np.float32) / half)).astype(np.float32)
    ang = pos.astype(np.float32)[:, None] * inv[None, :]
    cos = np.cos(ang).astype(np.float32)
    sin = np.sin(ang).astype(np.float32)
    return np.concatenate([cos, cos, -sin, sin], axis=1).astype(np.float32)


_NC_CACHE = {}


def kernel(x_prompt, x_sample, c_prompt, c_sample, cache_kv, state_win, state_conv, page_table,
           w_ada, b_ada, norm1_g, norm2_g, w_in, conv_w, conv_b, cmp_pe, cmp_w1, cmp_b1, cmp_w2, cmp_b2,
           w_out, peer_wq, peer_keys, peer_u, peer_v, final_g):
    f32 = np.float32
    x_prompt = np.asarray(x_prompt, f32)
    x_sample = np.asarray(x_sample, f32)
    xp_t = x_prompt[0].reshape(64, 128, D)
    idn = np.eye(128, dtype=f32)
    css = _rope_table(SEQ + (np.arange(16) % 4))
    csa = np.stack([_rope_table(128 * t + np.arange(128)) for t in range(64)]).astype(f32)
    cscmp = np.stack([_rope_table(16 * (128 * H + np.arange(128)) + 31) for H in range(4)]).astype(f32)
    jj = np.arange(512)[:, None]
    sb_ = np.arange(128)[None, :]
    ov = ((16 * jj < 64 * (sb_ + 1)) & (16 * jj + 32 > 64 * sb_) & (jj <= 510)).astype(f32)
    ovm = np.ascontiguousarray(ov.reshape(4, 128, 128).transpose(1, 0, 2))
    piota = np.arange(128, dtype=f32).reshape(128, 1)
    cmask_sm = np.ones((128, 4, 4), f32)
    cmask_sm[127, 3, :] = 0.0
    fmask_sm = np.zeros((4, 128), f32)
    fmask_sm[:, 0] = 1.0e4
    fmask_sm[:, 127] = 1.0e4
    wmask_sm = np.ones((128, 4, 4), f32)
    for tq in range(4):
        wmask_sm[0:tq + 1, 0, tq] = 0.0
    nmask = np.zeros((16, NS, 4), f32)
    for b_ in range(NS):
        for t_ in range(4):
            for tq in range(4):
                if t_ <= tq:
                    nmask[4 * b_ + t_, b_, tq] = 1.0
    e32 = np.zeros((32, 16, 128), f32)
    for c_ in range(16):
        for k_ in range(128):
            e32[2 * c_ + k_ // 64, c_, k_] = 1.0
    common = {
        "piota": piota, "cmask_sm": cmask_sm, "fmask_sm": fmask_sm, "wmask_sm": wmask_sm, "nmask": nmask,
        "cache": np.ascontiguousarray(np.asarray(cache_kv, f32)[0].reshape(2560 * 128, 1024)),
        "idn": idn, "css": css, "xp": np.ascontiguousarray(xp_t), "csa": csa, "cscmp": cscmp, "ovm": ovm, "e32": e32,
        "cmp_pe": np.asarray(cmp_pe[0], f32), "cmp_w1": np.asarray(cmp_w1[0], f32), "cmp_b1": np.asarray(cmp_b1[0], f32),
        "cmp_w2": np.asarray(cmp_w2[0], f32), "cmp_b2": np.asarray(cmp_b2[0], f32),
        "w_ada": np.asarray(w_ada[0], f32), "b_ada": np.asarray(b_ada, f32).reshape(1, -1),
        "g1": np.asarray(norm1_g, f32).reshape(1, D), "g2": np.asarray(norm2_g, f32).reshape(1, D),
        "fg": np.asarray(final_g, f32).reshape(1, D),
        "w_in": np.asarray(w_in[0], f32), "conv_w": np.asarray(conv_w[0], f32), "conv_b": np.asarray(conv_b, f32).reshape(1, DC),
        "w_out": np.asarray(w_out[0], f32),
        "peer_wq": np.asarray(peer_wq[0], f32), "peer_keys": np.ascontiguousarray(np.asarray(peer_keys[0], f32).reshape(16, 128, 128)),
        "peer_u": np.asarray(peer_u[0], f32), "peer_v": np.asarray(peer_v[0], f32),
    }
    in_maps = []
    for core in range(NCORES):
        tl = _tiles_of(core)
        xo = np.ascontiguousarray(xp_t[tl])
        xpv = np.zeros((NT, 2, D), f32)
        pfl = np.ones((128, NT), f32)
        cso = np.zeros((NT, 128, 128), f32)
        for i, t in enumerate(tl):
            if t > 0:
                xpv[i] = x_prompt[0, 128 * t - 2:128 * t]
            else:
                pfl[:, i] = 0.0
            cso[i] = _rope_table(128 * t + np.arange(128))
        fmask = np.zeros((NT, 128, 128), f32)
        cmask = np.zeros((NT, 128, 4, 128), f32)
        dmask = np.zeros((NT, 128, 8, 128), f32)
        wmask = np.zeros((NT, 128, 12, 128), f32)
        qa = np.arange(128)
        for i, t in enumerate(tl):
            qpos = 128 * t + qa
            cur = qpos // 64
            blk = np.arange(128)[None, :]
            forced = (blk == 0) | (blk == cur[:, None]) | (blk == cur[:, None] - 1)
            valid = blk <= cur[:, None]
            fmask[i] = np.where(valid, np.where(forced, 1.0e4, 0.0), -1.0e30)
            for jc in range(4):
                j = 128 * jc + np.arange(128)
                cmask[i, :, jc, :] = ((16 * j[:, None] + 31 <= qpos[None, :]) & (j[:, None] <= 510))
            for d_ in range(8):
                kc = 8 * i + d_
                kpos = 128 * kc + np.arange(128)
                dmask[i, :, d_, :] = (kpos[:, None] <= qpos[None, :])
            for w_ in range(12):
                kc = 8 * i - 4 + w_
                if kc < 0:
                    continue
                kpos = 128 * kc + np.arange(128)
                dist = qpos[None, :] - kpos[:, None]
                wmask[i, :, w_, :] = ((dist >= 0) & (dist < 512))
        sq = slice(NS * core, NS * core + NS)
        m = dict(common)
        m.update({
            "xo": xo, "xpv": xpv, "pfl": pfl, "cso": cso, "fmask": fmask, "cmask": cmask, "dmask": dmask, "wmask": wmask,
            "xs": np.ascontiguousarray(x_sample[sq].reshape(16, D)),
            "ptab": np.ascontiguousarray(np.asarray(page_table)[sq].astype(np.int32)),
            "cc": np.ascontiguousarray(np.concatenate([np.asarray(c_prompt, f32), np.asarray(c_sample, f32)[sq]], axis=0)),
            "scv": np.ascontiguousarray(np.asarray(state_conv, f32)[0, sq].reshape(8, DC)),
            "swin": np.ascontiguousarray(np.asarray(state_win, f32)[0, sq].reshape(NS, 512, 512)),
        })
        in_maps.append(m)

    if "nc" not in _NC_CACHE:
        _NC_CACHE["nc"] = build_program()
    nc = _NC_CACHE["nc"]
    shp = getattr(nc, "_din_shapes", None)
    if shp is not None:
        in_maps = [{k: (m_[k] if tuple(m_[k].shape) == shp[k] else np.zeros(shp[k], f32)) for k in shp} for m_ in in_maps]
    res = run_bass_kernel_spmd(nc, in_maps, core_ids=list(range(NCORES)))
    R = res.results

    y_prompt = np.zeros((1, SEQ, D), f32)
    kv_rows_prompt = np.zeros((1, 1, SEQ, 4, 4, 64), f32)
    win_prompt = np.zeros((1, 1, 512, 2, 4, 64), f32)
    conv_prompt = np.zeros((1, 1, 2, DC), f32)
    y_sample = np.zeros((32, 4, D), f32)
    kv_rows_sample = np.zeros((1, 32, 4, 4, 4, 64), f32)
    win_sample = np.zeros((1, 32, 512, 2, 4, 64), f32)
    conv_sample = np.zeros((1, 32, 2, DC), f32)
    for core in range(NCORES):
        r = R[core]
        tl = _tiles_of(core)
        for i, t in enumerate(tl):
            y_prompt[0, 128 * t:128 * (t + 1)] = r["yo"][i]
            kv_rows_prompt[0, 0, 128 * t:128 * (t + 1)] = r["rows_o"][i].reshape(128, 4, 4, 64)
            if t >= 60:
                win_prompt[0, 0, 128 * (t - 60):128 * (t - 59)] = r["win_o"][i].reshape(128, 2, 4, 64)
            if t == 63:
                conv_prompt[0, 0] = r["conv_o"][i]
        sq = slice(NS * core, NS * core + NS)
        y_sample[sq] = r["ys"].reshape(NS, 4, D)
        kv_rows_sample[0, sq] = r["rows_s"].reshape(NS, 4, 4, 4, 64)
        win_sample[0, sq] = r["win_s"].reshape(NS, 512, 2, 4, 64)
        conv_sample[0, sq] = r["conv_s"]
    return (y_prompt, y_sample, kv_rows_prompt, kv_rows_sample, win_prompt, win_sample, conv_prompt, conv_sample)
```
